# Optimizing a Trainium2 kernel written in Bass

```python
import math
import jax, jax.numpy as jnp
from jax import lax
import numpy as np

D_MODEL = 1024
BATCH = 1
SEQ = 16384
DEPTH = 2

GRID_W = 64
ATT_HEADS = 8
ATT_KV_HEADS = 2
ATT_HEAD_DIM = 64
ATT_WIDTH = ATT_HEADS * ATT_HEAD_DIM
ATT_KV_WIDTH = ATT_KV_HEADS * ATT_HEAD_DIM
ROPE_AXIS_DIM = ATT_HEAD_DIM // 2
ROPE_THETA = 10000.0
Q_BLOCK = 128
HG_HEADS = 4
HG_F_DIM = 128
HG_I_DIM = 128
HG_F_WIDTH = HG_HEADS * HG_F_DIM
HG_WIDTH = HG_HEADS * HG_I_DIM
HG_CHUNK = 64
MIN_FORGET = 1e-6
N_BRANCHES = 2
N_EXPERTS = 16
EC_CAPACITY_FACTOR = 2
EXPERT_FF = 2048
NORM_EPS = 1e-6

IN_SPLITS = (ATT_WIDTH, ATT_KV_WIDTH, ATT_KV_WIDTH,
             HG_F_WIDTH, HG_F_WIDTH, HG_F_WIDTH,
             HG_WIDTH, HG_WIDTH,
             N_BRANCHES * D_MODEL)
IN_WIDTH = sum(IN_SPLITS)

kernel_name = "hybrid_gqa_hgrn2_ecmoe_encoder"


def rms_norm(x, w):
    xf = x.astype(jnp.float32)
    y = xf * lax.rsqrt(jnp.mean(xf * xf, axis=-1, keepdims=True) + NORM_EPS)
    return (y * w.astype(jnp.float32)).astype(x.dtype)


def axial_rope_tables(seq_len):
    rows = seq_len // GRID_W
    row_id = jnp.repeat(jnp.arange(rows, dtype=jnp.float32), GRID_W)
    col_id = jnp.tile(jnp.arange(GRID_W, dtype=jnp.float32), rows)
    inv_freq = ROPE_THETA ** (-jnp.arange(0, ROPE_AXIS_DIM, 2, dtype=jnp.float32) / ROPE_AXIS_DIM)
    ang = jnp.stack([row_id[:, None] * inv_freq, col_id[:, None] * inv_freq], axis=1)
    return jnp.cos(ang), jnp.sin(ang)


def apply_axial_rope(x, cos, sin):
    B, S, H, hd = x.shape
    xr = x.reshape(B, S, H, 2, 2, ROPE_AXIS_DIM // 2)
    x1, x2 = xr[..., 0, :], xr[..., 1, :]
    cs, sn = cos[None, :, None], sin[None, :, None]
    out = jnp.stack([x1 * cs - x2 * sn, x2 * cs + x1 * sn], axis=-2)
    return out.reshape(B, S, H, hd)


def blocked_bidirectional_gqa(q, k, v):
    B, S, _, _ = q.shape
    G = ATT_HEADS // ATT_KV_HEADS
    n_blk = S // Q_BLOCK
    qb = q.reshape(B, n_blk, Q_BLOCK, ATT_KV_HEADS, G, ATT_HEAD_DIM).transpose(1, 0, 3, 4, 2, 5)
    kt = k.transpose(0, 2, 1, 3)
    vt = v.transpose(0, 2, 1, 3)
    scale = ATT_HEAD_DIM ** -0.5

    def one_block(qblk):
        s = jnp.einsum('bhgqd,bhkd->bhgqk', qblk, kt) * scale
        p = jax.nn.softmax(s, axis=-1)
        return jnp.einsum('bhgqk,bhkd->bhgqd', p, vt)

    o = lax.map(one_block, qb)
    return o.transpose(1, 0, 4, 2, 3, 5).reshape(B, S, ATT_WIDTH)


def gla_chunk_scan(q, k, v, log_f):
    B, S, H, Dk = q.shape
    Dv = v.shape[-1]
    n = S // HG_CHUNK

    def to_chunks(a):
        return a.reshape(B, n, HG_CHUNK, H, a.shape[-1]).transpose(1, 0, 3, 2, 4)

    qc, kc, vc, gc = to_chunks(q), to_chunks(k), to_chunks(v), to_chunks(log_f)
    lower = jnp.tril(jnp.ones((HG_CHUNK, HG_CHUNK), dtype=bool))[:, :, None]
    lower_f = lower.astype(jnp.float32)

    def step(state, inp):
        qi, ki, vi, gi = inp
        b = jnp.cumsum(gi, axis=-2)
        o_inter = jnp.einsum('bhcd,bhde->bhce', qi * jnp.exp(b), state)
        diff = b[:, :, :, None, :] - b[:, :, None, :, :]
        decay = jnp.exp(jnp.where(lower, diff, 0.0)) * lower_f
        scores = jnp.einsum('bhid,bhjd,bhijd->bhij', qi, ki, decay)
        o_intra = jnp.einsum('bhij,bhje->bhie', scores, vi)
        b_last = b[:, :, -1:, :]
        new_state = (jnp.exp(b_last[:, :, 0, :])[..., None] * state
                     + jnp.einsum('bhcd,bhce->bhde', ki * jnp.exp(b_last - b), vi))
        return new_state, o_inter + o_intra

    init = jnp.zeros((B, H, Dk, Dv), jnp.float32)
    _, o = lax.scan(step, init, (qc, kc, vc, gc))
    return o.transpose(1, 0, 3, 2, 4).reshape(B, S, H, Dv)


def hgrn2_forget(z, lb):
    f = lb + (1.0 - lb) * jax.nn.sigmoid(z)
    log_f = jnp.log(jnp.maximum(f, MIN_FORGET))
    one_minus_f = (1.0 - lb) * jax.nn.sigmoid(-z)
    return log_f, one_minus_f


def token_mixer(h, lb, w_in, q_norm_w, k_norm_w, hg_norm_w, w_branch_att, w_branch_hg, w_out,
                rope_cos, rope_sin):
    B, S, D = h.shape
    f32 = jnp.float32
    split_idx = np.cumsum(IN_SPLITS)[:-1].tolist()
    proj = jnp.einsum('bsd,de->bse', h, w_in)
    aq, ak, av, hq, hz_f, hz_b, hi, hgate, gate_logits = jnp.split(proj, split_idx, axis=-1)

    q = aq.astype(f32).reshape(B, S, ATT_HEADS, ATT_HEAD_DIM)
    k = ak.astype(f32).reshape(B, S, ATT_KV_HEADS, ATT_HEAD_DIM)
    v = av.astype(f32).reshape(B, S, ATT_KV_HEADS, ATT_HEAD_DIM)
    q = apply_axial_rope(rms_norm(q, q_norm_w), rope_cos, rope_sin)
    k = apply_axial_rope(rms_norm(k, k_norm_w), rope_cos, rope_sin)
    o_att = blocked_bidirectional_gqa(q, k, v)

    lb = lb.astype(f32)
    hqh = jax.nn.silu(hq.astype(f32)).reshape(B, S, HG_HEADS, HG_F_DIM)
    hv = hi.astype(f32).reshape(B, S, HG_HEADS, HG_I_DIM)
    logf_f, kf = hgrn2_forget(hz_f.astype(f32), lb[0])
    logf_b, kb = hgrn2_forget(hz_b.astype(f32), lb[1])
    shp = (B, S, HG_HEADS, HG_F_DIM)
    o_fwd = gla_chunk_scan(hqh, kf.reshape(shp), hv, logf_f.reshape(shp))
    flip = lambda a: jnp.flip(a, axis=1)
    o_bwd = flip(gla_chunk_scan(flip(hqh), flip(kb.reshape(shp)), flip(hv), flip(logf_b.reshape(shp))))
    o_h = rms_norm(o_fwd + o_bwd, hg_norm_w) * jax.nn.silu(
        hgate.astype(f32).reshape(B, S, HG_HEADS, HG_I_DIM))
    o_hg = o_h.reshape(B, S, HG_WIDTH)

    ya = jnp.einsum('bsw,wd->bsd', o_att.astype(h.dtype), w_branch_att)
    yh = jnp.einsum('bsw,wd->bsd', o_hg.astype(h.dtype), w_branch_hg)
    g = jax.nn.sigmoid(gate_logits.astype(f32)).reshape(B, S, N_BRANCHES, D)
    merged = (g[:, :, 0] * ya.astype(f32) + g[:, :, 1] * yh.astype(f32)).astype(h.dtype)
    return jnp.einsum('bsd,de->bse', merged, w_out)


def expert_choice_ffn(h, w_router, w_gate, w_up, w_down):
    B, S, D = h.shape
    cap = EC_CAPACITY_FACTOR * S // N_EXPERTS
    logits = jnp.einsum('bsd,de->bse', h.astype(jnp.float32), w_router.astype(jnp.float32))
    affinity = jax.nn.softmax(logits, axis=-1)
    top_w, top_idx = lax.top_k(affinity.transpose(0, 2, 1), cap)
    xe = jax.vmap(lambda hb, ib: hb[ib])(h, top_idx)
    hid = jax.nn.silu(jnp.einsum('becd,edf->becf', xe, w_gate)) * jnp.einsum('becd,edf->becf', xe, w_up)
    ye = jnp.einsum('becf,efd->becd', hid, w_down) * top_w[..., None].astype(h.dtype)
    combine = lambda yb, ib: jnp.zeros((S, D), h.dtype).at[ib.reshape(-1)].add(yb.reshape(-1, D))
    return jax.vmap(combine)(ye, top_idx)


def setup_inputs(seed: int = 0) -> dict:
    key = jax.random.key(seed)
    ks = jax.random.split(key, 20)
    L, D = DEPTH, D_MODEL
    f32 = jnp.float32

    def nrm(k, shape, fan_in, scale=1.0):
        return jax.random.normal(k, shape, f32) * (scale * fan_in ** -0.5)

    def gain(k, shape):
        return 1.0 + 0.02 * jax.random.normal(k, shape, f32)

    return {
        "x": jax.random.normal(ks[0], (BATCH, SEQ, D), f32),
        "c": jax.random.normal(ks[1], (BATCH, D), f32),
        "ada_w": nrm(ks[2], (L, D, 6 * D), D, 0.5),
        "ada_b": 0.02 * jax.random.normal(ks[3], (L, 6 * D), f32),
        "norm_mix_w": gain(ks[4], (L, D)),
        "norm_ffn_w": gain(ks[5], (L, D)),
        "w_in": nrm(ks[6], (L, D, IN_WIDTH), D),
        "q_norm_w": gain(ks[7], (L, ATT_HEAD_DIM)),
        "k_norm_w": gain(ks[8], (L, ATT_HEAD_DIM)),
        "hg_lower_bounds": jax.random.normal(ks[9], (L, 2, HG_F_WIDTH), f32),
        "hg_norm_w": gain(ks[10], (L, HG_I_DIM)),
        "w_branch_att": nrm(ks[11], (L, ATT_WIDTH, D), ATT_WIDTH),
        "w_branch_hg": nrm(ks[12], (L, HG_WIDTH, D), HG_WIDTH),
        "w_out": nrm(ks[13], (L, D, D), D),
        "w_router": nrm(ks[14], (L, D, N_EXPERTS), D),
        "w_exp_gate": nrm(ks[15], (L, N_EXPERTS, D, EXPERT_FF), D),
        "w_exp_up": nrm(ks[16], (L, N_EXPERTS, D, EXPERT_FF), D),
        "w_exp_down": nrm(ks[17], (L, N_EXPERTS, EXPERT_FF, D), EXPERT_FF),
    }


def reference(x, c, ada_w, ada_b, norm_mix_w, norm_ffn_w, w_in, q_norm_w, k_norm_w,
              hg_lower_bounds, hg_norm_w, w_branch_att, w_branch_hg, w_out, w_router,
              w_exp_gate, w_exp_up, w_exp_down):
    rope_cos, rope_sin = axial_rope_tables(x.shape[1])
    lb_soft = jax.nn.softmax(hg_lower_bounds.astype(jnp.float32), axis=0)
    lb_all = jnp.cumsum(lb_soft, axis=0) - lb_soft[0]
    c_act = jax.nn.silu(c)
    for l in range(DEPTH):
        mod = (jnp.einsum('bd,de->be', c_act, ada_w[l]) + ada_b[l])[:, None, :]
        shift1, scale1, gate1, shift2, scale2, gate2 = jnp.split(mod, 6, axis=-1)
        h = rms_norm(x, norm_mix_w[l]) * (1.0 + scale1) + shift1
        x = x + gate1 * token_mixer(h, lb_all[l], w_in[l], q_norm_w[l], k_norm_w[l], hg_norm_w[l],
                                    w_branch_att[l], w_branch_hg[l], w_out[l], rope_cos, rope_sin)
        h = rms_norm(x, norm_ffn_w[l]) * (1.0 + scale2) + shift2
        x = x + gate2 * expert_choice_ffn(h, w_router[l], w_exp_gate[l], w_exp_up[l], w_exp_down[l])
    return x
```

```python
import numpy as np
import ml_dtypes
from concourse.bass_utils import run_bass_kernel_spmd

from contextlib import ExitStack
import concourse.bass as bass
import concourse.mybir as mybir

F32 = mybir.dt.float32
BF16 = mybir.dt.bfloat16
I32 = mybir.dt.int32
U32 = mybir.dt.uint32
AF = mybir.ActivationFunctionType
ALU = mybir.AluOpType
AX = mybir.AxisListType

SEM_ROT = 30000
SAME_ENGINE_SYNC = True


class Tl:
    def __init__(self, k, t, name, space):
        self.k = k
        self.t = t
        self.name = name
        self.space = space
        self.w = {}
        self.r = {}
        self.dsem = {}
        self.dcnt = {}
        k.tiles.append(self)

    def __getitem__(self, idx):
        return self.t[idx]

    def ap(self):
        return self.t.ap() if hasattr(self.t, "ap") else self.t[:]


class K:
    ENG = ("pe", "dve", "act", "pool", "sp")

    def __init__(self, nc):
        self.nc = nc
        self.es = ExitStack()
        self.eng = {"pe": nc.tensor, "dve": nc.vector, "act": nc.scalar,
                    "pool": nc.gpsimd, "sp": nc.sync}
        self.gen = {e: 0 for e in self.ENG}
        self.sem = {}
        self.cnt = {e: 0 for e in self.ENG}
        for e in self.ENG:
            self.sem[(e, 0)] = self.es.enter_context(nc.semaphore(f"s_{e}_0"))
        self.seen = {e: {} for e in self.ENG}
        self.seenD = {e: {} for e in self.ENG}
        self.nsem = len(self.ENG)
        self.uid = 0
        self.tiles = []
        self.stacks = []
        self.dsem_scopes = []
        self.free_dsems = {}

    def push(self):
        self.stacks.append(ExitStack())
        self.dsem_scopes.append([])

    def pop(self):
        self.barrier()
        for t, q in self.dsem_scopes.pop():
            if q in t.dsem:
                self.free_dsems.setdefault(q, []).append((t.dsem.pop(q), t.dcnt.pop(q)))
        for e in self.ENG:
            self.seenD[e] = {}
        self.stacks.pop().close()

    def _st(self):
        return self.stacks[-1] if self.stacks else self.es

    def sb(self, name, shape, dt):
        self.uid += 1
        t = self._st().enter_context(self.nc.sbuf_tensor(f"{name}_{self.uid}", list(shape), dt))
        return Tl(self, t, name, "sb")

    def ps(self, name, shape, dt=F32):
        self.uid += 1
        t = self._st().enter_context(self.nc.psum_tensor(f"{name}_{self.uid}", list(shape), dt))
        return Tl(self, t, name, "ps")

    def dram(self, name, shape, dt, kind="Internal"):
        t = self.nc.dram_tensor(name, list(shape), dt, kind=kind)
        return Tl(self, t, name, "dram")

    def close(self):
        while self.stacks:
            self.stacks.pop().close()
        self.es.close()


    def _wait(self, e, ev):
        if ev is None:
            return
        if ev[0] == "E":
            _, src, gen, c = ev
            if src == e and (e == "pe" or not SAME_ENGINE_SYNC):
                return
            key = (src, gen)
            if self.seen[e].get(key, 0) >= c:
                return
            self.eng[e].wait_ge(self.sem[key], c)
            self.seen[e][key] = c
        else:
            _, tl, q = ev
            if q not in tl.dsem:
                return
            tgt = tl.dcnt[q] * 16
            if self.seenD[e].get((id(tl), q), 0) >= tgt:
                return
            self.eng[e].wait_ge(tl.dsem[q], tgt)
            self.seenD[e][(id(tl), q)] = tgt

    @staticmethod
    def _key(ev):
        return ev[:3] if ev[0] == "E" else ("D", id(ev[1]), ev[2])

    def _deps(self, e, reads, writes, disjoint=False):
        for t in reads:
            for ev in t.w.values():
                self._wait(e, ev)
        if disjoint:
            return
        for t in writes:
            for ev in t.w.values():
                self._wait(e, ev)
            for ev in t.r.values():
                self._wait(e, ev)

    def _commit(self, ev, reads, writes, disjoint=False):
        for t in reads:
            if t in writes:
                continue
            t.r[self._key(ev)] = ev
        for t in writes:
            if disjoint:
                t.w[self._key(ev)] = ev
            else:
                t.w = {self._key(ev): ev}
                t.r = {}

    def barrier(self):
        for t in self.tiles:
            for ev in list(t.w.values()) + list(t.r.values()):
                self._wait("sp", ev)
            for q in list(t.dsem):
                self._wait("sp", ("D", t, q))
        for e in self.ENG:
            if e != "sp" and self.cnt[e] > 0:
                self._wait("sp", ("E", e, self.gen[e], self.cnt[e]))
        self.op("sp", lambda e: e.nop())
        ev = ("E", "sp", self.gen["sp"], self.cnt["sp"])
        for e in self.ENG:
            if e != "sp":
                self._wait(e, ev)
        for t in self.tiles:
            t.w = {}
            t.r = {}

    def op(self, e, fn, reads=(), writes=()):
        self._deps(e, reads, writes)
        if self.cnt[e] >= SEM_ROT:
            self.gen[e] += 1
            self.cnt[e] = 0
            self.sem[(e, self.gen[e])] = self.es.enter_context(
                self.nc.semaphore(f"s_{e}_{self.gen[e]}"))
            self.nsem += 1
        ins = fn(self.eng[e])
        self.cnt[e] += 1
        g = self.gen[e]
        ins.then_inc(self.sem[(e, g)], 1)
        ev = ("E", e, g, self.cnt[e])
        self._commit(ev, reads, writes)
        return ins

    def dma(self, q, fn, sbt, reads=(), writes=(), disjoint=False):
        self._deps(q, reads, writes, disjoint)
        if q not in sbt.dsem:
            self.uid += 1
            if self.free_dsems.get(q):
                sbt.dsem[q], sbt.dcnt[q] = self.free_dsems[q].pop()
            else:
                sbt.dsem[q] = self.es.enter_context(self.nc.semaphore(f"d_{q}_{sbt.name}_{self.uid}"))
                sbt.dcnt[q] = 0
                self.nsem += 1
            if self.dsem_scopes:
                self.dsem_scopes[-1].append((sbt, q))
        ins = fn(self.eng[q])
        ins.then_inc(sbt.dsem[q], 16)
        sbt.dcnt[q] += 1
        ev = ("D", sbt, q)
        self._commit(ev, reads, writes, disjoint)
        return ins

    def load(self, q, dst, dst_ap, src, src_ap, disjoint=False, **kw):
        return self.dma(q, lambda e: e.dma_start(out=dst_ap, in_=src_ap, **kw), dst,
                        reads=[src], writes=[dst], disjoint=disjoint)

    def store(self, q, dst, dst_ap, src, src_ap, disjoint=True, **kw):
        return self.dma(q, lambda e: e.dma_start(out=dst_ap, in_=src_ap, **kw), src,
                        reads=[src], writes=[dst], disjoint=disjoint)

    def finish(self, tiles, e="sp"):
        for t in tiles:
            for ev in list(t.w.values()) + list(t.r.values()):
                self._wait(e, ev)


class Pool:
    def __init__(self, k, name, shape, dt, n, space="sb"):
        mk = k.sb if space == "sb" else k.ps
        self.t = [mk(f"{name}{i}", shape, dt) for i in range(n)]
        self.i = 0

    def next(self):
        t = self.t[self.i % len(self.t)]
        self.i += 1
        return t

class _Dummy:
    pass
    pass

S = 16384
D = 1024
NBLK = S // 512
NTILE = S // 128
INW = 5376
EPS = 1e-6
O_Q, O_K, O_V, O_HQ, O_ZF, O_ZB, O_HI, O_HG, O_GL = 0, 512, 640, 768, 1280, 1792, 2304, 2816, 3328
NEXP = 16
FF = 2048
CAP = 2048


def mm(k, out, oap, lt, lap, rt, rap, start, stop):
    k.op("pe", lambda e: e.matmul(oap, lhsT=lap, rhs=rap, start=start, stop=stop),
         [lt, rt], [out])


def tp(k, out, oap, it, iap, ident):
    k.op("pe", lambda e: e.transpose(oap, iap, ident[:]), [it, ident], [out])


def colload(k, name, src, ap):
    t = k.sb(name, [128, 8], F32)
    k.load("sp", t, t[:], src, ap.rearrange("o (c p) -> p (o c)", p=128),
           allow_slow_non_contiguous=True)
    return t


def rowload(k, name, src, ap, n):
    t = k.sb(name, [128, n], F32)
    k.load("sp", t, t[:], src, ap.partition_broadcast(128))
    return t


def phase_mod(k, T):
    k.push()
    ccol = colload(k, "ccol", T.c, T.c.ap())
    cact = k.sb("cact", [128, 8], F32)
    k.op("act", lambda e: e.activation(out=cact[:], in_=ccol[:], func=AF.Silu), [ccol], [cact])
    wpool = Pool(k, "adaw", [128, 8, 512], F32, 2)
    brow = k.sb("brow", [1, 6144], F32)
    mrow = k.sb("mrow", [1, 6144], F32)
    pp = Pool(k, "pmod", [1, 512], F32, 2, "ps")
    for l in range(2):
        k.load("sp", brow, brow[:], T.ada_b, T.ada_b.ap()[l:l + 1, :])
        for nb in range(12):
            w = wpool.next()
            k.load("sp", w, w[:], T.ada_w,
                   T.ada_w.ap()[l].rearrange("(kc p) n -> p kc n", p=128)[:, :, nb * 512:(nb + 1) * 512])
            ps = pp.next()
            for kc in range(8):
                mm(k, ps, ps[:], cact, cact[:, kc:kc + 1], w, w[:, kc, :], kc == 0, kc == 7)
            k.op("dve", lambda e: e.tensor_tensor(out=mrow[:, nb * 512:(nb + 1) * 512], in0=ps[:],
                                                  in1=brow[:, nb * 512:(nb + 1) * 512], op=ALU.add),
                 [ps, brow], [mrow])
        k.store("sp", T.modd, T.modd.ap()[l:l + 1, :], mrow, mrow[:], disjoint=False)
    k.pop()


def layer_consts(k, T, l):
    C = _Dummy()
    md = T.modd.ap()
    sh1 = colload(k, "sh1", T.modd, md[l:l + 1, 0:1024])
    sc1 = colload(k, "sc1", T.modd, md[l:l + 1, 1024:2048])
    nw1 = colload(k, "nw1", T.norm_mix_w, T.norm_mix_w.ap()[l:l + 1, :])
    C.a1 = k.sb("a1", [128, 8], F32)
    C.b1 = sh1
    k.op("dve", lambda e: e.scalar_tensor_tensor(out=C.a1[:], in0=sc1[:], scalar=1.0, in1=nw1[:],
                                                 op0=ALU.add, op1=ALU.mult), [sc1, nw1], [C.a1])
    C.a2 = k.sb("a2", [128, 1024], F32)
    k.push()
    sc2 = rowload(k, "sc2", T.modd, md[l:l + 1, 4096:5120], 1024)
    nw2 = rowload(k, "nw2", T.norm_ffn_w, T.norm_ffn_w.ap()[l:l + 1, :], 1024)
    k.op("dve", lambda e: e.scalar_tensor_tensor(out=C.a2[:], in0=sc2[:], scalar=1.0, in1=nw2[:],
                                                 op0=ALU.add, op1=ALU.mult), [sc2, nw2], [C.a2])
    k.pop()
    C.b2 = rowload(k, "sh2", T.modd, md[l:l + 1, 3072:4096], 1024)
    C.g1 = rowload(k, "g1", T.modd, md[l:l + 1, 2048:3072], 1024)
    C.g2 = rowload(k, "g2", T.modd, md[l:l + 1, 5120:6144], 1024)
    C.qkw = k.sb("qkw", [128, 10, 64], F32)
    for h in range(10):
        src = T.q_norm_w if h < 8 else T.k_norm_w
        k.load("sp", C.qkw, C.qkw[:, h, :], src, src.ap()[l:l + 1, :].partition_broadcast(128))
    C.hgw = k.sb("hgw", [128, 4, 128], F32)
    for h in range(4):
        k.load("sp", C.hgw, C.hgw[:, h, :], T.hg_norm_w, T.hg_norm_w.ap()[l:l + 1, :].partition_broadcast(128))
    C.lb = k.sb("lb", [128, 8], F32)
    C.oml = k.sb("oml", [128, 8], F32)
    C.noml = k.sb("noml", [128, 8], F32)
    if l == 0:
        k.op("dve", lambda e: e.memset(C.lb[:], 0.0), [], [C.lb])
    else:
        a0 = k.sb("lba0", [128, 8], F32)
        a1 = k.sb("lba1", [128, 8], F32)
        hb = T.hg_lower_bounds.ap()
        k.load("sp", a0, a0[:], T.hg_lower_bounds, hb[0].rearrange("r (h d) -> d (r h)", d=128),
               allow_slow_non_contiguous=True)
        k.load("sp", a1, a1[:], T.hg_lower_bounds, hb[1].rearrange("r (h d) -> d (r h)", d=128),
               allow_slow_non_contiguous=True)
        k.op("dve", lambda e: e.tensor_tensor(out=a0[:], in0=a1[:], in1=a0[:], op=ALU.subtract), [a0, a1], [a0])
        k.op("act", lambda e: e.activation(out=C.lb[:], in_=a0[:], func=AF.Sigmoid), [a0], [C.lb])
    k.op("dve", lambda e: e.tensor_scalar(out=C.oml[:], in0=C.lb[:], scalar1=-1.0, scalar2=1.0,
                                          op0=ALU.mult, op1=ALU.add), [C.lb], [C.oml])
    k.op("dve", lambda e: e.tensor_scalar(out=C.noml[:], in0=C.oml[:], scalar1=-1.0, scalar2=None,
                                          op0=ALU.mult), [C.oml], [C.noml])
    return C


def phase_proj(k, T, C, l, xsrc, xsrc_ap):
    k.push()
    win = k.sb("win", [128, 8, INW], BF16)
    for kc in range(8):
        for cb in range(3):
            k.load("pool", win, win[:, kc, cb * 1792:(cb + 1) * 1792], T.w_in,
                   T.w_in.ap()[l, kc * 128:(kc + 1) * 128, cb * 1792:(cb + 1) * 1792], disjoint=True)
    xp = Pool(k, "xt", [128, 4, 1024], F32, 1)
    xnp = Pool(k, "xn", [128, 4, 1024], BF16, 1)
    hTp = Pool(k, "hT", [128, 8, 512], BF16, 1)
    junk = k.sb("junk", [128, 1024], BF16)
    ssp = Pool(k, "ss", [128, 4], F32, 2)
    rsp = Pool(k, "rs", [128, 4], F32, 2)
    pb = Pool(k, "pb", [128, 512], F32, 6, "ps")
    ptr = Pool(k, "ptr", [128, 1024], BF16, 2, "ps")
    qkp = Pool(k, "qk", [128, 10, 64], F32, 2)
    sqp = Pool(k, "sq", [128, 10, 64], F32, 2)
    t1p = Pool(k, "t1", [128, 10, 64], F32, 2)
    s10p = Pool(k, "s10", [128, 10], F32, 2)
    csp = Pool(k, "cs", [128, 2, 64], F32, 2)
    qbp = Pool(k, "qb", [128, 5, 128], BF16, 2)
    qTp = Pool(k, "qTs", [128, 5, 512], BF16, 1)
    vtp = Pool(k, "vt", [128, 4, 130], BF16, 2)
    hip = Pool(k, "hi", [128, 4, 512], BF16, 1)
    sgp = Pool(k, "sg", [128, 4, 512], BF16, 1)
    glp = Pool(k, "gl", [128, 2048], F32, 1)
    fmp = Pool(k, "fm", [128, 512], BF16, 3)
    f32p = Pool(k, "f32", [128, 512], F32, 4)
    sigp = Pool(k, "sig", [128, 512], F32, 2)
    for B in range(NBLK):
        r0 = B * 512
        xt = xp.next()
        k.load("sp", xt, xt[:], xsrc, xsrc_ap[r0:r0 + 512, :].rearrange("(t p) d -> p t d", p=128))
        ss = ssp.next()
        rs = rsp.next()
        xn = xnp.next()
        for t in range(4):
            k.op("act", lambda e: e.activation(out=junk[:], in_=xt[:, t, :], func=AF.Square,
                                               accum_out=ss[:, t:t + 1]), [xt], [junk, ss])
        k.op("act", lambda e: e.activation(out=rs[:], in_=ss[:], func=AF.Sqrt, scale=1.0 / D, bias=EPS),
             [ss], [rs])
        k.op("dve", lambda e: e.reciprocal(out=rs[:], in_=rs[:]), [rs], [rs])
        for t in range(4):
            k.op("act", lambda e: e.activation(out=xn[:, t, :], in_=xt[:, t, :], func=AF.Copy,
                                               scale=rs[:, t:t + 1]), [xt, rs], [xn])
        hT = hTp.next()
        for kc in range(8):
            p = ptr.next()
            for t in range(4):
                tp(k, p, p[:, t * 128:(t + 1) * 128], xn, xn[:, t, kc * 128:(kc + 1) * 128], T.ident_bf)
            k.op("dve", lambda e: e.tensor_scalar(out=hT[:, kc, :], in0=p[:, 0:512],
                                                  scalar1=C.a1[:, kc:kc + 1], scalar2=C.b1[:, kc:kc + 1],
                                                  op0=ALU.mult, op1=ALU.add), [p, C.a1, C.b1], [hT])

        def proj_tm(ps, t, c0, n):
            for kc in range(8):
                mm(k, ps, ps[:, 0:n], hT, hT[:, kc, t * 128:(t + 1) * 128], win, win[:, kc, c0:c0 + n],
                   kc == 0, kc == 7)

        def proj_fm(ps, c0):
            for kc in range(8):
                mm(k, ps, ps[:], win, win[:, kc, c0:c0 + 128], hT, hT[:, kc, :], kc == 0, kc == 7)

        qT = qTp.next()
        vt = vtp.next()
        k.op("pool", lambda e: e.memset(vt[:], 1.0), [], [vt])
        hi = hip.next()
        sg = sgp.next()
        for t in range(4):
            tr0 = r0 + t * 128
            cs = csp.next()
            k.load("sp", cs, cs[:, 0, :], T.cos64, T.cos64.ap()[tr0:tr0 + 128, :])
            k.load("sp", cs, cs[:, 1, :], T.sin64, T.sin64.ap()[tr0:tr0 + 128, :])
            ps1 = pb.next()
            proj_tm(ps1, t, O_Q, 512)
            ps2 = pb.next()
            proj_tm(ps2, t, O_K, 256)
            qk = qkp.next()
            k.op("act", lambda e: e.copy(out=qk[:, 0:8, :], in_=ps1[:].rearrange("p (h d) -> p h d", d=64)),
                 [ps1], [qk])
            k.op("act", lambda e: e.copy(out=qk[:, 8:10, :], in_=ps2[:, 0:128].rearrange("p (h d) -> p h d", d=64)),
                 [ps2], [qk])
            k.op("act", lambda e: e.copy(out=vt[:, t, :].rearrange("p (g d) -> p g d", d=65)[:, :, 0:64],
                                         in_=ps2[:, 128:256].rearrange("p (g d) -> p g d", d=64)), [ps2], [vt])
            sq = sqp.next()
            s10 = s10p.next()
            k.op("dve", lambda e: e.tensor_tensor(out=sq[:], in0=qk[:], in1=qk[:], op=ALU.mult), [qk], [sq])
            k.op("dve", lambda e: e.tensor_reduce(out=s10[:], in_=sq[:], axis=AX.X, op=ALU.add), [sq], [s10])
            k.op("act", lambda e: e.activation(out=s10[:], in_=s10[:], func=AF.Sqrt, scale=1.0 / 64, bias=EPS),
                 [s10], [s10])
            k.op("dve", lambda e: e.reciprocal(out=s10[:], in_=s10[:]), [s10], [s10])
            k.op("dve", lambda e: e.tensor_tensor(out=qk[:], in0=qk[:],
                                                  in1=s10[:].unsqueeze(2).to_broadcast([128, 10, 64]),
                                                  op=ALU.mult), [qk, s10], [qk])
            k.op("pool", lambda e: e.tensor_tensor(out=qk[:], in0=qk[:], in1=C.qkw[:], op=ALU.mult),
                 [qk, C.qkw], [qk])
            t1 = t1p.next()
            k.op("pool", lambda e: e.tensor_tensor(out=t1[:], in0=qk[:],
                                                   in1=cs[:, 0:1, :].to_broadcast([128, 10, 64]), op=ALU.mult),
                 [qk, cs], [t1])
            qv = qk[:].rearrange("p h (a f j) -> p h a f j", a=2, f=2)
            sv = cs[:, 1, :].rearrange("p (a f j) -> p a f j", a=2, f=2)
            sqv = sq[:].rearrange("p h (a f j) -> p h a f j", a=2, f=2)
            for f in range(2):
                k.op("dve", lambda e: e.tensor_tensor(
                    out=sqv[:, :, :, f, :], in0=qv[:, :, :, 1 - f, :],
                    in1=sv[:, :, f, :].unsqueeze(1).to_broadcast([128, 10, 2, 16]), op=ALU.mult),
                    [qk, cs], [sq])
            qb = qbp.next()
            k.op("dve", lambda e: e.tensor_tensor(
                out=qb[:, 0:4, :].rearrange("p j (g d) -> p g j d", g=2),
                in0=t1[:, 0:8, :].rearrange("p (g j) d -> p g j d", g=2),
                in1=sq[:, 0:8, :].rearrange("p (g j) d -> p g j d", g=2), op=ALU.add), [t1, sq], [qb])
            k.op("pool", lambda e: e.tensor_tensor(
                out=qb[:, 4, :].rearrange("p (g d) -> p g d", g=2),
                in0=t1[:, 8:10, :], in1=sq[:, 8:10, :], op=ALU.add), [t1, sq], [qb])
            p = ptr.next()
            for j in range(5):
                tp(k, p, p[:, j * 128:(j + 1) * 128], qb, qb[:, j, :], T.ident_bf)
            k.op("act", lambda e: e.copy(out=qT[:, :, t * 128:(t + 1) * 128],
                                         in_=p[:, 0:640].rearrange("p (j q) -> p j q", q=128)), [p], [qT])
            ps = pb.next()
            proj_tm(ps, t, O_HI, 512)
            k.op("dve", lambda e: e.tensor_copy(out=hi[:, t, :], in_=ps[:]), [ps], [hi])
            ps = pb.next()
            proj_tm(ps, t, O_HG, 512)
            k.op("act", lambda e: e.activation(out=sg[:, t, :], in_=ps[:], func=AF.Silu), [ps], [sg])
            gl = glp.next()
            for c in range(4):
                ps = pb.next()
                proj_tm(ps, t, O_GL + c * 512, 512)
                k.op("act", lambda e: e.activation(out=gl[:, c * 512:(c + 1) * 512], in_=ps[:], func=AF.Sigmoid),
                     [ps], [gl])
            k.store("pool", T.gl_d, T.gl_d.ap()[tr0:tr0 + 128, :], gl, gl[:])
        k.store("pool", T.qT_d, T.qT_d.ap()[:, :, r0:r0 + 512], qT, qT[:, 0:4, :])
        k.store("pool", T.kT_d, T.kT_d.ap()[:, r0:r0 + 512], qT, qT[:, 4, :])
        k.store("pool", T.va_d, T.va_d.ap()[r0:r0 + 512, :].rearrange("(t p) c -> p t c", p=128), vt, vt[:])
        k.store("pool", T.vh_d, T.vh_d.ap()[r0:r0 + 512, :].rearrange("(t p) c -> p t c", p=128), hi, hi[:])
        k.store("pool", T.sg_d, T.sg_d.ap()[r0:r0 + 512, :].rearrange("(t p) c -> p t c", p=128), sg, sg[:])
        for h in range(4):
            ps = pb.next()
            proj_fm(ps, O_HQ + h * 128)
            o = fmp.next()
            k.op("act", lambda e: e.activation(out=o[:], in_=ps[:], func=AF.Silu), [ps], [o])
            k.store("pool", T.hq_d, T.hq_d.ap()[h, :, r0:r0 + 512], o, o[:])
        for dr in range(2):
            for h in range(4):
                ci = dr * 4 + h
                ps = pb.next()
                proj_fm(ps, (O_ZF if dr == 0 else O_ZB) + h * 128)
                sig = sigp.next()
                k.op("act", lambda e: e.activation(out=sig[:], in_=ps[:], func=AF.Sigmoid), [ps], [sig])
                f = f32p.next()
                k.op("dve", lambda e: e.tensor_scalar(out=f[:], in0=sig[:], scalar1=C.oml[:, ci:ci + 1],
                                                      scalar2=C.lb[:, ci:ci + 1], op0=ALU.mult, op1=ALU.add),
                     [sig, C.oml, C.lb], [f])
                k.op("pool", lambda e: e.tensor_scalar(out=f[:], in0=f[:], scalar1=1e-6, scalar2=None,
                                                       op0=ALU.max), [f], [f])
                k.op("act", lambda e: e.activation(out=f[:], in_=f[:], func=AF.Ln), [f], [f])
                k.store("pool", T.g_d, T.g_d.ap()[dr, h, :, r0:r0 + 512], f, f[:])
                kk = f32p.next()
                k.op("dve", lambda e: e.tensor_scalar(out=kk[:], in0=sig[:], scalar1=C.noml[:, ci:ci + 1],
                                                      scalar2=C.oml[:, ci:ci + 1], op0=ALU.mult, op1=ALU.add),
                     [sig, C.noml, C.oml], [kk])
                k.store("pool", T.kk_d, T.kk_d.ap()[dr, h, :, r0:r0 + 512], kk, kk[:])
    k.pop()


def phase_attn(k, T):
    k.push()
    kT = k.sb("kT", [128, S], BF16)
    va = k.sb("va", [128, NTILE, 130], BF16)
    for i in range(4):
        k.load("sp", kT, kT[:, i * 4096:(i + 1) * 4096], T.kT_d, T.kT_d.ap()[:, i * 4096:(i + 1) * 4096],
               disjoint=True)
        k.load("sp", va, va[:, i * 32:(i + 1) * 32, :], T.va_d,
               T.va_d.ap()[i * 4096:(i + 1) * 4096, :].rearrange("(t p) c -> p t c", p=128), disjoint=True)
    qp = Pool(k, "qblk", [128, 4, 512], BF16, 2)
    psc = Pool(k, "psc", [128, 512], F32, 3, "ps")
    pac = Pool(k, "pac", [128, 512], F32, 4, "ps")
    ppx = k.ps("ppx", [128, 8, 128], BF16)
    ptp = Pool(k, "pT", [128, 512], BF16, 4)
    ohp = Pool(k, "ohi", [65, 512], BF16, 2)
    olp = Pool(k, "olo", [65, 512], BF16, 2)
    otp = Pool(k, "otm", [128, 4, 65], F32, 2)
    rcp = Pool(k, "rc", [128, 4], F32, 2)
    oap = Pool(k, "oa", [128, 4, 512], BF16, 2)
    for B in range(NBLK):
        r0 = B * 512
        qb = qp.next()
        k.load("sp", qb, qb[:], T.qT_d, T.qT_d.ap()[:, :, r0:r0 + 512])
        oa = oap.next()
        for g in range(2):
            acc = [pac.next() for _ in range(4)]
            for kc in range(NTILE):
                for j in range(4):
                    ps = psc.next()
                    mm(k, ps, ps[:], kT, kT[64 * g:64 * g + 64, kc * 128:(kc + 1) * 128],
                       qb, qb[64 * g:64 * g + 64, j, :], True, True)
                    pT = ptp.next()
                    k.op("act", lambda e: e.activation(out=pT[:], in_=ps[:], func=AF.Exp, scale=0.125), [ps], [pT])
                    mm(k, acc[j], acc[j][0:65, :], va, va[:, kc, g * 65:(g + 1) * 65], pT, pT[:],
                       kc == 0, kc == NTILE - 1)
            for j in range(4):
                h = g * 4 + j
                ohi = ohp.next(); olo = olp.next()
                k.op("act", lambda e: e.copy(out=ohi[:], in_=acc[j][0:65, :]), [acc[j]], [ohi])
                k.op("dve", lambda e: e.tensor_tensor(out=olo[:], in0=acc[j][0:65, :], in1=ohi[:], op=ALU.subtract),
                     [acc[j], ohi], [olo])
                for t in range(4):
                    tp(k, ppx, ppx[:, t, 0:65], ohi, ohi[:, t * 128:(t + 1) * 128], T.ident65)
                    tp(k, ppx, ppx[:, 4 + t, 0:65], olo, olo[:, t * 128:(t + 1) * 128], T.ident65)
                otm = otp.next()
                k.op("act", lambda e: e.copy(out=otm[:], in_=ppx[:, 0:4, 0:65]), [ppx], [otm])
                k.op("dve", lambda e: e.tensor_tensor(out=otm[:], in0=otm[:], in1=ppx[:, 4:8, 0:65], op=ALU.add),
                     [otm, ppx], [otm])
                rc = rcp.next()
                k.op("dve", lambda e: e.reciprocal(out=rc[:], in_=otm[:, :, 64]), [otm], [rc])
                k.op("dve", lambda e: e.tensor_tensor(out=oa[:, :, h * 64:(h + 1) * 64], in0=otm[:, :, 0:64],
                                                      in1=rc[:].unsqueeze(2).to_broadcast([128, 4, 64]),
                                                      op=ALU.mult), [otm, rc], [oa])
        k.store("pool", T.oatt_d, T.oatt_d.ap()[r0:r0 + 512, :].rearrange("(t p) c -> p t c", p=128), oa, oa[:])
    k.pop()


def phase_hgrn(k, T):
    k.push()
    HD = [(h, dr) for dr in range(2) for h in range(4)]
    S32 = {hd: k.sb(f"S32_{hd[0]}{hd[1]}", [128, 128], F32) for hd in HD}
    S16 = {hd: k.sb(f"S16_{hd[0]}{hd[1]}", [128, 128], BF16) for hd in HD}
    for hd in HD:
        k.op("pool", lambda e: e.memset(S32[hd][:], 0.0), [], [S32[hd]])
        k.op("pool", lambda e: e.memset(S16[hd][:], 0.0), [], [S16[hd]])
    N = 8
    hqp = Pool(k, "hq", [128, 128], BF16, 2 * N)
    gp = Pool(k, "g", [128, 128], F32, 2 * N)
    kkp = Pool(k, "kk", [128, 128], F32, 2 * N)
    vp = Pool(k, "v", [128, 128], BF16, 2 * N)
    for i_ in range(2 * N):
        for p_ in (gp, kkp, vp):
            p_.t[i_].dsem = hqp.t[i_].dsem
            p_.t[i_].dcnt = hqp.t[i_].dcnt
    bp = Pool(k, "b", [128, 128], F32, N)
    b2p = Pool(k, "b2", [128, 128], F32, N)
    ebp = Pool(k, "eb", [128, 128], F32, N)
    tmp = Pool(k, "tmp", [128, 128], F32, N)
    qtp = Pool(k, "qt", [128, 128], BF16, N)
    ktp = Pool(k, "kt", [128, 128], BF16, N)
    khtp = Pool(k, "kht", [128, 128], BF16, N)
    scp = Pool(k, "sc", [128, 128], BF16, N)
    osp = Pool(k, "os", [128, 128], F32, N)
    po = [k.ps(f"po{i}", [128, 4, 128], F32) for i in range(2)]
    psb = k.ps("psb", [128, 4, 128], F32)
    ptbs = [k.ps(f"ptb{i}", [128, 2, 4, 128], BF16) for i in range(2)]
    pub = [k.ps(f"pu{i}", [128, 4, 128], F32) for i in range(2)]
    qpp = Pool(k, "qpad", [128, 4, 128], BF16, N)
    khpp = Pool(k, "khpad", [128, 4, 128], BF16, N)
    khtpp = Pool(k, "khtpad", [128, 4, 128], BF16, N)
    for step in range(NTILE):
        st = {}
        for i, (h, dr) in enumerate(HD):
            Tt = step if dr == 0 else NTILE - 1 - step
            c0 = Tt * 128
            hq = hqp.next(); g = gp.next(); kk = kkp.next(); v = vp.next()
            k.load("sp", hq, hq[:], T.hq_d, T.hq_d.ap()[h, :, c0:c0 + 128])
            k.load("sp", g, g[:], T.g_d, T.g_d.ap()[dr, h, :, c0:c0 + 128])
            k.load("sp", kk, kk[:], T.kk_d, T.kk_d.ap()[dr, h, :, c0:c0 + 128])
            k.load("sp", v, v[:], T.vh_d, T.vh_d.ap()[c0:c0 + 128, h * 128:(h + 1) * 128])
            b = bp.next()
            k.op("dve", lambda e: e.tensor_tensor_scan(out=b[:], data0=T.rst[:], data1=g[:], initial=0.0,
                                                       op0=ALU.mult, op1=ALU.add), [T.rst, g], [b])
            b3 = b[:].rearrange("p (c i) -> p c i", i=32)
            if dr == 1:
                b2 = b2p.next()
                k.op("pool", lambda e: e.tensor_tensor(out=b2[:].rearrange("p (c i) -> p c i", i=32),
                                                       in0=b3[:, :, 31:32].to_broadcast([128, 4, 32]),
                                                       in1=b3, op=ALU.subtract), [b], [b2])
                k.op("pool", lambda e: e.tensor_tensor(out=b2[:], in0=b2[:], in1=g[:], op=ALU.add), [b2, g], [b2])
                b = b2
                b3 = b[:].rearrange("p (c i) -> p c i", i=32)
            le = 31 if dr == 0 else 0
            eb = ebp.next()
            k.op("act", lambda e: e.activation(out=eb[:], in_=b[:], func=AF.Exp), [b], [eb])
            qt = qtp.next()
            k.op("dve", lambda e: e.tensor_tensor(out=qt[:], in0=hq[:], in1=eb[:], op=ALU.mult), [hq, eb], [qt])
            t1 = tmp.next()
            k.op("act", lambda e: e.activation(out=t1[:], in_=b[:], func=AF.Exp, scale=-1.0), [b], [t1])
            kt = ktp.next()
            k.op("dve", lambda e: e.tensor_tensor(out=kt[:], in0=kk[:], in1=t1[:], op=ALU.mult), [kk, t1], [kt])
            t2 = tmp.next()
            k.op("pool", lambda e: e.tensor_tensor(out=t2[:].rearrange("p (c i) -> p c i", i=32),
                                                   in0=b3[:, :, le:le + 1].to_broadcast([128, 4, 32]),
                                                   in1=b3, op=ALU.subtract), [b], [t2])
            k.op("act", lambda e: e.activation(out=t2[:], in_=t2[:], func=AF.Exp), [t2], [t2])
            kht = khtp.next()
            k.op("pool", lambda e: e.tensor_tensor(out=kht[:], in0=kk[:], in1=t2[:], op=ALU.mult), [kk, t2], [kht])
            khtpad = khtpp.next()
            k.op("pool", lambda e: e.tensor_tensor(out=khtpad[:], in0=kht[:].unsqueeze(1).to_broadcast([128, 4, 128]),
                                                   in1=T.cmask[:], op=ALU.mult), [kht, T.cmask], [khtpad])
            ptb = ptbs[(i // 2) % 2]
            for c in range(4):
                tp(k, ptb, ptb[:, i % 2, c, :], khtpad, khtpad[:, c, :], T.ident_bf)
            kh = khpp.next()
            k.op("act", lambda e: e.copy(out=kh[:], in_=ptb[:, i % 2, :, :]), [ptb], [kh])
            qpad = qpp.next()
            k.op("dve", lambda e: e.tensor_tensor(out=qpad[:], in0=qt[:].unsqueeze(1).to_broadcast([128, 4, 128]),
                                                  in1=T.cmask[:], op=ALU.mult), [qt, T.cmask], [qpad])
            mm(k, psb, psb[:, i % 4, :], kt, kt[:], qt, qt[:], True, True)
            sc = scp.next()
            mk = T.mask_f if dr == 0 else T.mask_b
            k.op("dve", lambda e: e.tensor_tensor(out=sc[:], in0=psb[:, i % 4, :], in1=mk[:], op=ALU.mult),
                 [psb, mk], [sc])
            o = po[i // 4]
            k.op("pe", lambda e: e.matmul(o[:, i % 4, :], lhsT=sc[:], rhs=v[:], start=(i % 4 == 0), stop=False,
                                           skip_group_check=True), [sc, v], [o])
            st[(h, dr)] = (qpad, kh, v, eb, le, Tt)
        for ci in range(4):
            for i, (h, dr) in enumerate(HD):
                qt, kh, v, eb, le, Tt = st[(h, dr)]
                c = ci if dr == 0 else 3 - ci
                o = po[i // 4]
                k.op("pe", lambda e: e.matmul(o[:, i % 4, :], lhsT=qt[:, c, :],
                                               rhs=S16[(h, dr)][:], start=False, stop=(ci == 3),
                                               skip_group_check=True), [qt, S16[(h, dr)]], [o])
                pu = pub[i // 4]
                k.op("pe", lambda e: e.matmul(pu[:, i % 4, :], lhsT=kh[:, c, :],
                                               rhs=v[:], start=True, stop=True,
                                               skip_group_check=True), [kh, v], [pu])
                s32 = S32[(h, dr)]
                dcol = eb[:, 32 * c + le:32 * c + le + 1]
                k.op("dve", lambda e: e.scalar_tensor_tensor(out=s32[:], in0=s32[:], scalar=dcol,
                                                             in1=pu[:, i % 4, :], op0=ALU.mult, op1=ALU.add),
                     [s32, eb, pu], [s32])
                k.op("act", lambda e: e.copy(out=S16[(h, dr)][:], in_=s32[:]), [s32], [S16[(h, dr)]])
        for i, (h, dr) in enumerate(HD):
            Tt = st[(h, dr)][5]
            os_ = osp.next()
            k.op("act", lambda e: e.copy(out=os_[:], in_=po[i // 4][:, i % 4, :]), [po[i // 4]], [os_])
            dst = T.of_d if dr == 0 else T.ob_d
            k.store("pool", dst, dst.ap()[Tt * 128:(Tt + 1) * 128, h * 128:(h + 1) * 128], os_, os_[:])
    k.pop()


def phase_merge(k, T, C, l, xsrc, xsrc_ap, acc, acc_ap):
    import os
    CUT = float(os.environ.get('MCUT', '99'))
    k.push()
    wba = k.sb("wba", [128, 4, 1024], BF16)
    wbh = k.sb("wbh", [128, 4, 1024], BF16)
    wout = k.sb("wout", [128, 8, 1024], BF16)
    wr = k.sb("wr", [128, 8, 16], F32)
    stg = Pool(k, "stg", [128, 1024], F32, 2)
    for kc in range(4):
        k.load("pool", wba, wba[:, kc, :], T.w_branch_att, T.w_branch_att.ap()[l, kc * 128:(kc + 1) * 128, :], disjoint=True)
        k.load("pool", wbh, wbh[:, kc, :], T.w_branch_hg, T.w_branch_hg.ap()[l, kc * 128:(kc + 1) * 128, :], disjoint=True)
    for kc in range(8):
        s_ = stg.next()
        k.load("sp", s_, s_[:], T.w_out, T.w_out.ap()[l, kc * 128:(kc + 1) * 128, :])
        k.op("dve", lambda e: e.tensor_tensor(out=wout[:, kc, :], in0=s_[:], in1=C.g1[:], op=ALU.mult),
             [s_, C.g1], [wout])
    k.load("sp", wr, wr[:], T.w_router, T.w_router.ap()[l].rearrange("(kc p) e -> p kc e", p=128))
    wr3 = [k.sb(f"wr3_{i}", [128, 8, 16], BF16) for i in range(3)]
    wrr = k.sb("wrr", [128, 8, 16], F32)
    k.op("act", lambda e: e.copy(out=wr3[0][:], in_=wr[:]), [wr], [wr3[0]])
    k.op("dve", lambda e: e.tensor_tensor(out=wrr[:], in0=wr[:], in1=wr3[0][:], op=ALU.subtract), [wr, wr3[0]], [wrr])
    k.op("act", lambda e: e.copy(out=wr3[1][:], in_=wrr[:]), [wrr], [wr3[1]])
    k.op("dve", lambda e: e.tensor_tensor(out=wrr[:], in0=wrr[:], in1=wr3[1][:], op=ALU.subtract), [wrr, wr3[1]], [wrr])
    k.op("act", lambda e: e.copy(out=wr3[2][:], in_=wrr[:]), [wrr], [wr3[2]])
    oap = Pool(k, "oat", [128, 512], BF16, 2)
    ofp = Pool(k, "of", [128, 4, 128], F32, 2)
    obp = Pool(k, "ob", [128, 4, 128], F32, 2)
    sgp = Pool(k, "sgm", [128, 512], BF16, 2)
    glp = Pool(k, "glm", [128, 2048], F32, 2)
    xp = Pool(k, "xm", [128, 1024], F32, 2)
    sqp = Pool(k, "sqm", [128, 4, 128], F32, 2)
    s4p = Pool(k, "s4", [128, 4], F32, 2)
    ohp = Pool(k, "ohb", [128, 512], BF16, 2)
    lTp = Pool(k, "lT", [128, 8, 128], BF16, 2)
    m1p = Pool(k, "m1", [128, 1024], F32, 2)
    m2p = Pool(k, "m2", [128, 1024], F32, 2)
    mbp = Pool(k, "mb", [128, 1024], BF16, 2)
    mTp = Pool(k, "mT", [128, 8, 128], BF16, 2)
    x1p = Pool(k, "x1", [128, 1024], F32, 2)
    junk = k.sb("junkm", [128, 1024], F32)
    s1p = Pool(k, "s1", [128, 1], F32, 4)
    h2p = Pool(k, "h2", [128, 1024], F32, 2)
    h2bp = Pool(k, "h2b", [128, 8, 128], BF16, 6)
    hbp = [Pool(k, f"hb{i}", [128, 1024], BF16, 2 if i == 0 else 1) for i in range(3)]
    r1p = Pool(k, "r1", [128, 1024], F32, 1)
    lgp = Pool(k, "lg", [128, 16], F32, 2)
    pbk = Pool(k, "pbk", [128, 512], F32, 6, "ps")
    ptb = k.ps("ptbm", [128, 1024], BF16)
    plg = k.ps("plg", [128, 16], F32)
    for Tt in range(int(os.environ.get('MTILES', NTILE))):
        r0 = Tt * 128
        oat = oap.next(); of = ofp.next(); ob = obp.next(); sg = sgp.next(); gl = glp.next(); xm = xp.next()
        k.load("sp", oat, oat[:], T.oatt_d, T.oatt_d.ap()[r0:r0 + 128, :])
        k.load("sp", of, of[:], T.of_d, T.of_d.ap()[r0:r0 + 128, :].rearrange("p (h e) -> p h e", e=128))
        k.load("sp", ob, ob[:], T.ob_d, T.ob_d.ap()[r0:r0 + 128, :].rearrange("p (h e) -> p h e", e=128))
        k.load("sp", sg, sg[:], T.sg_d, T.sg_d.ap()[r0:r0 + 128, :])
        k.load("sp", gl, gl[:], T.gl_d, T.gl_d.ap()[r0:r0 + 128, :])
        k.load("sp", xm, xm[:], xsrc, xsrc_ap[r0:r0 + 128, :])
        k.op("pool", lambda e: e.tensor_tensor(out=of[:], in0=of[:], in1=ob[:], op=ALU.add), [of, ob], [of])
        sq = sqp.next(); s4 = s4p.next()
        k.op("dve", lambda e: e.tensor_tensor(out=sq[:], in0=of[:], in1=of[:], op=ALU.mult), [of], [sq])
        k.op("dve", lambda e: e.tensor_reduce(out=s4[:], in_=sq[:], axis=AX.X, op=ALU.add), [sq], [s4])
        k.op("act", lambda e: e.activation(out=s4[:], in_=s4[:], func=AF.Sqrt, scale=1.0 / 128, bias=EPS), [s4], [s4])
        k.op("dve", lambda e: e.reciprocal(out=s4[:], in_=s4[:]), [s4], [s4])
        k.op("dve", lambda e: e.tensor_tensor(out=of[:], in0=of[:], in1=s4[:].unsqueeze(2).to_broadcast([128, 4, 128]),
                                              op=ALU.mult), [of, s4], [of])
        k.op("pool", lambda e: e.tensor_tensor(out=of[:], in0=of[:], in1=C.hgw[:], op=ALU.mult), [of, C.hgw], [of])
        ohb = ohp.next()
        k.op("dve", lambda e: e.tensor_tensor(out=ohb[:].rearrange("p (h e) -> p h e", e=128), in0=of[:],
                                              in1=sg[:].rearrange("p (h e) -> p h e", e=128), op=ALU.mult),
             [of, sg], [ohb])
        if CUT < 1:
            continue
        for j in range(4):
            tp(k, ptb, ptb[:, j * 128:(j + 1) * 128], oat, oat[:, j * 128:(j + 1) * 128], T.ident_bf)
            tp(k, ptb, ptb[:, (4 + j) * 128:(5 + j) * 128], ohb, ohb[:, j * 128:(j + 1) * 128], T.ident_bf)
        lT = lTp.next()
        k.op("act", lambda e: e.copy(out=lT[:], in_=ptb[:].rearrange("p (j q) -> p j q", q=128)), [ptb], [lT])
        if CUT < 2:
            continue
        m1 = m1p.next(); m2 = m2p.next()
        for hf in range(2):
            pa = pbk.next()
            for kc in range(4):
                mm(k, pa, pa[:], lT, lT[:, kc, :], wba, wba[:, kc, hf * 512:(hf + 1) * 512], kc == 0, kc == 3)
            k.op("dve", lambda e: e.tensor_tensor(out=m1[:, hf * 512:(hf + 1) * 512], in0=pa[:],
                                                  in1=gl[:, hf * 512:(hf + 1) * 512], op=ALU.mult), [pa, gl], [m1])
            ph = pbk.next()
            for kc in range(4):
                mm(k, ph, ph[:], lT, lT[:, 4 + kc, :], wbh, wbh[:, kc, hf * 512:(hf + 1) * 512], kc == 0, kc == 3)
            k.op("dve", lambda e: e.tensor_tensor(out=m2[:, hf * 512:(hf + 1) * 512], in0=ph[:],
                                                  in1=gl[:, 1024 + hf * 512:1024 + (hf + 1) * 512], op=ALU.mult),
                 [ph, gl], [m2])
        mb = mbp.next()
        k.op("pool", lambda e: e.tensor_tensor(out=mb[:], in0=m1[:], in1=m2[:], op=ALU.add), [m1, m2], [mb])
        for j in range(8):
            tp(k, ptb, ptb[:, j * 128:(j + 1) * 128], mb, mb[:, j * 128:(j + 1) * 128], T.ident_bf)
        mT = mTp.next()
        k.op("act", lambda e: e.copy(out=mT[:], in_=ptb[:].rearrange("p (j q) -> p j q", q=128)), [ptb], [mT])
        x1 = x1p.next()
        for hf in range(2):
            po = pbk.next()
            for kc in range(8):
                mm(k, po, po[:], mT, mT[:, kc, :], wout, wout[:, kc, hf * 512:(hf + 1) * 512], kc == 0, kc == 7)
            k.op("dve", lambda e: e.tensor_tensor(out=x1[:, hf * 512:(hf + 1) * 512], in0=po[:],
                                                  in1=xm[:, hf * 512:(hf + 1) * 512], op=ALU.add), [po, xm], [x1])
        if CUT < 3:
            continue
        k.store("pool", T.xs1, T.xs1.ap()[r0:r0 + 128, :], x1, x1[:])
        k.store("pool", acc, acc_ap[r0:r0 + 128, :], x1, x1[:])
        if CUT < 3.1:
            continue
        s1 = s1p.next()
        k.op("act", lambda e: e.activation(out=junk[:], in_=x1[:], func=AF.Square, accum_out=s1[:]), [x1], [junk, s1])
        if CUT < 3.2:
            continue
        k.op("act", lambda e: e.activation(out=s1[:], in_=s1[:], func=AF.Sqrt, scale=1.0 / D, bias=EPS), [s1], [s1])
        k.op("dve", lambda e: e.reciprocal(out=s1[:], in_=s1[:]), [s1], [s1])
        if CUT < 3.3:
            continue
        h2 = h2p.next()
        k.op("act", lambda e: e.activation(out=h2[:], in_=x1[:], func=AF.Copy, scale=s1[:]), [x1, s1], [h2])
        if CUT < 3.4:
            continue
        k.op("dve", lambda e: e.tensor_tensor(out=h2[:], in0=h2[:], in1=C.a2[:], op=ALU.mult), [h2, C.a2], [h2])
        if CUT < 3.5:
            continue
        k.op("dve", lambda e: e.tensor_tensor(out=h2[:], in0=h2[:], in1=C.b2[:], op=ALU.add), [h2, C.b2], [h2])
        if CUT < 4:
            continue
        hb = [hbp[i].next() for i in range(3)]
        r1 = r1p.next()
        k.op("act", lambda e: e.copy(out=hb[0][:], in_=h2[:]), [h2], [hb[0]])
        k.op("dve", lambda e: e.tensor_tensor(out=r1[:], in0=h2[:], in1=hb[0][:], op=ALU.subtract), [h2, hb[0]], [r1])
        k.op("act", lambda e: e.copy(out=hb[1][:], in_=r1[:]), [r1], [hb[1]])
        k.op("dve", lambda e: e.tensor_tensor(out=r1[:], in0=r1[:], in1=hb[1][:], op=ALU.subtract), [r1, hb[1]], [r1])
        k.op("act", lambda e: e.copy(out=hb[2][:], in_=r1[:]), [r1], [hb[2]])
        hT3 = [h2bp.next() for _ in range(3)]
        for i in range(3):
            for j in range(8):
                tp(k, ptb, ptb[:, j * 128:(j + 1) * 128], hb[i], hb[i][:, j * 128:(j + 1) * 128], T.ident_bf)
            k.op("act" if i != 1 else "dve", lambda e: e.tensor_copy(out=hT3[i][:], in_=ptb[:].rearrange("p (j q) -> p j q", q=128))
                 if i == 1 else e.copy(out=hT3[i][:], in_=ptb[:].rearrange("p (j q) -> p j q", q=128)), [ptb], [hT3[i]])
        k.store("pool", T.h2b_d, T.h2b_d.ap()[r0:r0 + 128, :], hb[0], hb[0][:])
        if CUT < 5:
            continue
        terms = [(0, 0), (0, 1), (1, 0), (0, 2), (2, 0), (1, 1)]
        for ti, (a_, b_) in enumerate(terms):
            for kc in range(8):
                mm(k, plg, plg[:], hT3[a_], hT3[a_][:, kc, :], wr3[b_], wr3[b_][:, kc, :],
                   ti == 0 and kc == 0, ti == len(terms) - 1 and kc == 7)
        if CUT < 6:
            continue
        lg = lgp.next()
        mx = s1p.next(); se = s1p.next()
        k.op("dve", lambda e: e.tensor_reduce(out=mx[:], in_=plg[:], axis=AX.X, op=ALU.max), [plg], [mx])
        k.op("dve", lambda e: e.tensor_scalar(out=mx[:], in0=mx[:], scalar1=-1.0, scalar2=None, op0=ALU.mult), [mx], [mx])
        k.op("act", lambda e: e.activation(out=lg[:], in_=plg[:], func=AF.Exp, bias=mx[:], accum_out=se[:]),
             [plg, mx], [lg, se])
        k.op("dve", lambda e: e.reciprocal(out=se[:], in_=se[:]), [se], [se])
        k.op("dve", lambda e: e.tensor_scalar(out=lg[:], in0=lg[:], scalar1=se[:], scalar2=None, op0=ALU.mult),
             [lg, se], [lg])
        k.store("pool", T.aff_d, T.aff_d.ap()[r0:r0 + 128, :], lg, lg[:])
    k.pop()


def phase_route(k, T, idx_all):
    k.push()
    for nm, shp, dt in (("iota", [128, CAP], F32), ("rst16", [128, CAP], F32), ("U_bf", [128, 128], BF16), ("tcol", [128, 2], BF16)):
        t = k.sb(nm, shp, dt)
        src = getattr(T, "c_" + nm)
        k.load("sp", t, t[:], src, src.ap())
        setattr(T, nm, t)
    aff = k.sb("affall", [128, 128, NEXP], F32)
    k.load("sp", aff, aff[:], T.aff_d, T.aff_d.ap().rearrange("(t p) e -> t p e", p=128))
    cmp = k.sb("cmp", [128, 128, NEXP], F32)
    ones = k.sb("ones", [128, 128], F32)
    k.op("pool", lambda e: e.memset(ones[:], 1.0), [], [ones])
    lo = k.sb("lo", [128, NEXP], F32); hi = k.sb("hi", [128, NEXP], F32)
    mid = k.sb("mid", [128, NEXP], F32); cnt = k.sb("cnt", [128, NEXP], F32)
    ge = k.sb("ge", [128, NEXP], F32); d1 = k.sb("d1", [128, NEXP], F32)
    pt = k.ps("ptot", [128, NEXP], F32)
    k.op("dve", lambda e: e.memset(lo[:], 0.0), [], [lo])
    k.op("dve", lambda e: e.memset(hi[:], 1.0), [], [hi])
    for it in range(40):
        k.op("dve", lambda e: e.tensor_tensor(out=mid[:], in0=lo[:], in1=hi[:], op=ALU.add), [lo, hi], [mid])
        k.op("dve", lambda e: e.tensor_scalar(out=mid[:], in0=mid[:], scalar1=0.5, scalar2=None, op0=ALU.mult), [mid], [mid])
        k.op("dve", lambda e: e.tensor_tensor(out=cmp[:], in0=aff[:],
                                              in1=mid[:].unsqueeze(1).to_broadcast([128, 128, NEXP]),
                                              op=ALU.is_gt), [aff, mid], [cmp])
        k.op("dve", lambda e: e.tensor_reduce(out=cnt[:], in_=cmp[:].rearrange("t p e -> t e p"), axis=AX.X,
                                              op=ALU.add), [cmp], [cnt])
        mm(k, pt, pt[:], ones, ones[:], cnt, cnt[:], True, True)
        k.op("dve", lambda e: e.tensor_scalar(out=ge[:], in0=pt[:], scalar1=float(CAP) - 0.5, scalar2=None,
                                              op0=ALU.is_ge), [pt], [ge])
        k.op("dve", lambda e: e.tensor_tensor(out=d1[:], in0=mid[:], in1=lo[:], op=ALU.subtract), [mid, lo], [d1])
        k.op("dve", lambda e: e.tensor_tensor(out=d1[:], in0=d1[:], in1=ge[:], op=ALU.mult), [d1, ge], [d1])
        k.op("dve", lambda e: e.tensor_tensor(out=lo[:], in0=lo[:], in1=d1[:], op=ALU.add), [lo, d1], [lo])
        k.op("dve", lambda e: e.tensor_tensor(out=d1[:], in0=hi[:], in1=mid[:], op=ALU.subtract), [hi, mid], [d1])
        k.op("dve", lambda e: e.tensor_tensor(out=d1[:], in0=d1[:], in1=ge[:], op=ALU.mult), [d1, ge], [d1])
        k.op("dve", lambda e: e.tensor_tensor(out=hi[:], in0=mid[:], in1=d1[:], op=ALU.add), [mid, d1], [hi])
    msk = k.sb("msk", [128, NEXP, 128], F32)
    scn = k.sb("scn", [128, NEXP, 128], F32)
    scb = k.sb("scb", [128, NEXP, 129], BF16)
    k.op("dve", lambda e: e.tensor_tensor(out=msk[:].rearrange("t e p -> t p e"), in0=aff[:],
                                          in1=lo[:].unsqueeze(1).to_broadcast([128, 128, NEXP]), op=ALU.is_gt),
         [aff, lo], [msk])
    k.op("dve", lambda e: e.tensor_tensor_scan(out=scn[:].rearrange("t e p -> t (e p)"),
                                               data0=T.rst16[:], data1=msk[:].rearrange("t e p -> t (e p)"),
                                               initial=0.0, op0=ALU.mult, op1=ALU.add), [T.rst16, msk], [scn])
    k.op("act", lambda e: e.copy(out=scb[:, :, 0:128], in_=scn[:]), [scn], [scb])
    k.op("act", lambda e: e.copy(out=scb[:, :, 128], in_=T.tcol[:, 0:1].to_broadcast([128, NEXP])), [T.tcol], [scb])
    totb = k.sb("totb", [128, NEXP], BF16)
    tot = k.sb("tot", [128, NEXP], F32)
    k.op("dve", lambda e: e.tensor_copy(out=totb[:], in_=scn[:, :, 127]), [scn], [totb])
    k.op("dve", lambda e: e.tensor_copy(out=tot[:], in_=scn[:, :, 127]), [scn], [tot])
    pof = k.ps("pof", [128, NEXP], F32)
    mm(k, pof, pof[:], T.U_bf, T.U_bf[:], totb, totb[:], True, True)
    offs = k.sb("offs", [128, NEXP], F32)
    incl = k.sb("incl", [128, NEXP], F32)
    k.op("dve", lambda e: e.tensor_copy(out=offs[:], in_=pof[:]), [pof], [offs])
    k.op("dve", lambda e: e.tensor_tensor(out=incl[:], in0=offs[:], in1=tot[:], op=ALU.add), [offs, tot], [incl])
    t1p = Pool(k, "rt1", [128, CAP], F32, 2)
    ohp = Pool(k, "oh", [128, CAP], BF16, 2)
    wp = Pool(k, "wrk", [128, CAP], BF16, 2)
    pa = Pool(k, "pa", [128, 129], F32, 2, "ps")
    pr = Pool(k, "pr", [128, 1], F32, 2, "ps")
    rcp = Pool(k, "rcol", [128, 1], F32, 4)
    fnp = Pool(k, "fine", [128, 1], F32, 4)
    jk = k.sb("jk", [128, 128], F32)
    idxf = k.sb("idxf", [128, NEXP, 16], F32)
    for ex in range(NEXP):
        t1 = t1p.next(); oh = ohp.next(); w_ = wp.next(); t2 = t1p.next()
        k.op("dve", lambda e: e.tensor_scalar(out=t1[:], in0=T.iota[:], scalar1=offs[:, ex:ex + 1], scalar2=None,
                                              op0=ALU.is_ge), [T.iota, offs], [t1])
        k.op("dve", lambda e: e.scalar_tensor_tensor(out=oh[:], in0=T.iota[:], scalar=incl[:, ex:ex + 1], in1=t1[:],
                                                     op0=ALU.is_lt, op1=ALU.mult), [T.iota, incl, t1], [oh])
        k.op("dve", lambda e: e.tensor_scalar(out=t2[:], in0=T.iota[:], scalar1=offs[:, ex:ex + 1], scalar2=None,
                                              op0=ALU.subtract), [T.iota, offs], [t2])
        k.op("dve", lambda e: e.tensor_tensor(out=w_[:], in0=t2[:], in1=oh[:], op=ALU.mult), [t2, oh], [w_])
        for kq in range(16):
            pA = pa.next(); pR = pr.next()
            mm(k, pA, pA[:], oh, oh[:, kq * 128:(kq + 1) * 128], scb, scb[:, ex, :], True, True)
            mm(k, pR, pR[:], w_, w_[:, kq * 128:(kq + 1) * 128], T.tcol, T.tcol[:, 1:2], True, True)
            rc = rcp.next(); fn = fnp.next()
            k.op("act", lambda e: e.copy(out=rc[:], in_=pR[:]), [pR], [rc])
            k.op("dve", lambda e: e.tensor_scalar(out=jk[:], in0=pA[:, 0:128], scalar1=rc[:], scalar2=0.0,
                                                  op0=ALU.is_le, op1=ALU.add, accum_out=fn[:]), [pA, rc], [jk, fn])
            k.op("dve", lambda e: e.scalar_tensor_tensor(out=idxf[:, ex, kq:kq + 1], in0=pA[:, 128:129], scalar=128.0,
                                                         in1=fn[:], op0=ALU.mult, op1=ALU.add), [pA, fn], [idxf])
    k.op("dve", lambda e: e.tensor_copy(out=idx_all[:], in_=idxf[:]), [idxf], [idx_all])
    k.pop()


def phase_ffn(k, T, C, l, idx_all, acc, acc_ap):
    k.push()
    wg = k.sb("wg", [128, 8, FF], BF16)
    wu = k.sb("wu", [128, 8, FF], BF16)
    wd = k.sb("wd", [128, 16, 1024], BF16)
    stg = Pool(k, "stgd", [128, 1024], F32, 2)
    xep = Pool(k, "xe", [128, 1024], BF16, 3)
    twp = Pool(k, "tw", [128, NEXP], F32, 8)
    hTp = Pool(k, "xeT", [128, 8, 512], BF16, 2)
    hid = k.sb("hid", [128, 16, 512], BF16)
    sgp = Pool(k, "sgf", [128, 512], F32, 2)
    yop = Pool(k, "yo", [128, 1024], F32, 3)
    pgu = Pool(k, "pgu", [128, 512], F32, 4, "ps")
    pdn = Pool(k, "pdn", [128, 512], F32, 2, "ps")
    ptx = Pool(k, "ptx", [128, 1024], BF16, 2, "ps")
    for ex in range(NEXP):
        for kc in range(8):
            k.load("pool", wg, wg[:, kc, :], T.w_exp_gate, T.w_exp_gate.ap()[l, ex, kc * 128:(kc + 1) * 128, :])
            k.load("pool", wu, wu[:, kc, :], T.w_exp_up, T.w_exp_up.ap()[l, ex, kc * 128:(kc + 1) * 128, :])
        for fc in range(16):
            s_ = stg.next()
            k.load("sp", s_, s_[:], T.w_exp_down, T.w_exp_down.ap()[l, ex, fc * 128:(fc + 1) * 128, :])
            k.op("dve", lambda e: e.tensor_tensor(out=wd[:, fc, :], in0=s_[:], in1=C.g2[:], op=ALU.mult),
                 [s_, C.g2], [wd])
        for B in range(CAP // 512):
            hT = hTp.next()
            tws = []
            for t in range(4):
                kq = B * 4 + t
                xe = xep.next(); tw = twp.next()
                ix = idx_all[:, ex, kq:kq + 1]
                k.dma("pool", lambda e: e.indirect_dma_start(out=xe[:], out_offset=None, in_=T.h2b_d.ap(),
                                                              in_offset=bass.IndirectOffsetOnAxis(ap=ix, axis=0)),
                      xe, reads=[T.h2b_d, idx_all], writes=[xe])
                k.dma("pool", lambda e: e.indirect_dma_start(out=tw[:], out_offset=None, in_=T.aff_d.ap(),
                                                              in_offset=bass.IndirectOffsetOnAxis(ap=ix, axis=0)),
                      tw, reads=[T.aff_d, idx_all], writes=[tw])
                tws.append(tw)
                px = ptx.next()
                for j in range(8):
                    tp(k, px, px[:, j * 128:(j + 1) * 128], xe, xe[:, j * 128:(j + 1) * 128], T.ident_bf)
                k.op("act", lambda e: e.copy(out=hT[:, :, t * 128:(t + 1) * 128],
                                             in_=px[:].rearrange("p (j q) -> p j q", q=128)), [px], [hT])
            for fc in range(16):
                pg = pgu.next()
                for kc in range(8):
                    mm(k, pg, pg[:], wg, wg[:, kc, fc * 128:(fc + 1) * 128], hT, hT[:, kc, :], kc == 0, kc == 7)
                pu = pgu.next()
                for kc in range(8):
                    mm(k, pu, pu[:], wu, wu[:, kc, fc * 128:(fc + 1) * 128], hT, hT[:, kc, :], kc == 0, kc == 7)
                sg = sgp.next()
                k.op("act", lambda e: e.activation(out=sg[:], in_=pg[:], func=AF.Silu), [pg], [sg])
                k.op("dve", lambda e: e.tensor_tensor(out=hid[:, fc, :], in0=sg[:], in1=pu[:], op=ALU.mult),
                     [sg, pu], [hid])
            for t in range(4):
                kq = B * 4 + t
                yo = yop.next()
                for hf in range(2):
                    pd = pdn.next()
                    for fc in range(16):
                        mm(k, pd, pd[:], hid, hid[:, fc, t * 128:(t + 1) * 128], wd, wd[:, fc, hf * 512:(hf + 1) * 512],
                           fc == 0, fc == 15)
                    k.op("act", lambda e: e.activation(out=yo[:, hf * 512:(hf + 1) * 512], in_=pd[:], func=AF.Copy,
                                                       scale=tws[t][:, ex:ex + 1]), [pd, tws[t]], [yo])
                ix = idx_all[:, ex, kq:kq + 1]
                k.dma("pool", lambda e: e.indirect_dma_start(out=acc_ap, out_offset=bass.IndirectOffsetOnAxis(ap=ix, axis=0),
                                                              in_=yo[:], in_offset=None, compute_op=ALU.add),
                      yo, reads=[yo, idx_all], writes=[acc])
    k.pop()


def build(stop_after=99, dbg=(), nlayers=2, skip=()):
    nc = bass.Bass("TRN2", target_bir_lowering=False)
    k = K(nc)
    T = _Dummy()

    def inp(name, shape, dt=F32):
        setattr(T, name, k.dram(name, shape, dt, kind="ExternalInput"))

    inp("x", [1, S, D]); inp("c", [1, D]); inp("ada_w", [2, D, 6 * D]); inp("ada_b", [2, 6 * D])
    inp("norm_mix_w", [2, D]); inp("norm_ffn_w", [2, D]); inp("w_in", [2, D, INW])
    inp("q_norm_w", [2, 64]); inp("k_norm_w", [2, 64]); inp("hg_lower_bounds", [2, 2, 512])
    inp("hg_norm_w", [2, 128]); inp("w_branch_att", [2, 512, D]); inp("w_branch_hg", [2, 512, D])
    inp("w_out", [2, D, D]); inp("w_router", [2, D, NEXP])
    if stop_after >= 7:
        inp("w_exp_gate", [2, NEXP, D, FF]); inp("w_exp_up", [2, NEXP, D, FF]); inp("w_exp_down", [2, NEXP, FF, D])
    inp("cos64", [S, 64]); inp("sin64", [S, 64])
    inp("c_ident_bf", [128, 128], BF16); inp("c_ident_f", [128, 128]); inp("c_mask_f", [128, 128], BF16)
    inp("c_mask_b", [128, 128], BF16); inp("c_rst", [128, 128]); inp("c_cmask", [128, 4, 128], BF16)
    inp("c_iota", [128, CAP]); inp("c_rst16", [128, CAP]); inp("c_U_bf", [128, 128], BF16); inp("c_tcol", [128, 2], BF16)
    T.y = k.dram("y", [S, D], F32, kind="ExternalOutput")

    def scr(name, shape, dt):
        setattr(T, name, k.dram(name, shape, dt, kind="ExternalOutput" if name in dbg else "Internal"))

    scr("modd", [2, 6 * D], F32)
    scr("qT_d", [128, 4, S], BF16); scr("kT_d", [128, S], BF16); scr("va_d", [S, 130], BF16)
    scr("vh_d", [S, 512], BF16); scr("sg_d", [S, 512], BF16); scr("gl_d", [S, 2048], F32)
    scr("hq_d", [4, 128, S], BF16); scr("g_d", [2, 4, 128, S], F32); scr("kk_d", [2, 4, 128, S], F32)
    scr("oatt_d", [S, 512], BF16); scr("of_d", [S, 512], F32); scr("ob_d", [S, 512], F32)
    scr("xs1", [S, D], F32); scr("xs2", [S, D], F32); scr("h2T_d", [128, 8, S], BF16)
    scr("aff_d", [S, NEXP], F32); scr("h2b_d", [S, D], BF16)
    for nm, dt in (("ident_bf", BF16), ("ident_f", F32), ("mask_f", BF16), ("mask_b", BF16), ("rst", F32)):
        t = k.sb(nm, [128, 128], dt)
        src = getattr(T, "c_" + nm)
        k.load("sp", t, t[:], src, src.ap())
        setattr(T, nm, t)
    T.cmask = k.sb("cmask", [128, 4, 128], BF16)
    k.load("sp", T.cmask, T.cmask[:], T.c_cmask, T.c_cmask.ap())
    T.ident65 = k.sb("ident65", [65, 65], BF16)
    k.load("sp", T.ident65, T.ident65[:], T.c_ident_bf, T.c_ident_bf.ap()[0:65, 0:65])
    phase_mod(k, T)
    if stop_after >= 1:
        for l in range(nlayers):
            k.push()
            C = layer_consts(k, T, l)
            idx_all = k.sb("idx_all", [128, NEXP, 16], I32)
            xsrc, xap = (T.x, T.x.ap()[0]) if l == 0 else (T.xs2, T.xs2.ap())
            acc, aap = (T.xs2, T.xs2.ap()) if l == 0 else (T.y, T.y.ap())
            if 2 not in skip:
                phase_proj(k, T, C, l, xsrc, xap)
            if stop_after >= 3 and 3 not in skip:
                phase_attn(k, T)
            if stop_after >= 4 and 4 not in skip:
                phase_hgrn(k, T)
            if stop_after >= 5 and 5 not in skip:
                phase_merge(k, T, C, l, xsrc, xap, acc, aap)
            if stop_after >= 6:
                phase_route(k, T, idx_all)
            if stop_after >= 7:
                phase_ffn(k, T, C, l, idx_all, acc, aap)
            k.pop()
            if stop_after < 7:
                break
    k.barrier()
    k.close()
    return nc


def host_consts():
    bf = ml_dtypes.bfloat16
    t = np.arange(S)
    inv = (10000.0 ** (-np.arange(0, 32, 2, dtype=np.float32) / 32)).astype(np.float32)
    ang_r = ((t // 64).astype(np.float32)[:, None] * inv).astype(np.float32)
    ang_c = ((t % 64).astype(np.float32)[:, None] * inv).astype(np.float32)
    cr, sr, cc, sc = np.cos(ang_r), np.sin(ang_r), np.cos(ang_c), np.sin(ang_c)
    cos64 = np.concatenate([cr, cr, cc, cc], 1).astype(np.float32)
    sin64 = np.concatenate([-sr, sr, -sc, sc], 1).astype(np.float32)
    j = np.arange(128)[:, None]
    i = np.arange(128)[None, :]
    same = (j // 32) == (i // 32)
    return {
        "cos64": cos64, "sin64": sin64,
        "c_ident_bf": np.eye(128, dtype=np.float32).astype(bf), "c_ident_f": np.eye(128, dtype=np.float32),
        "c_mask_f": (same & (j <= i)).astype(np.float32).astype(bf),
        "c_mask_b": (same & (j >= i)).astype(np.float32).astype(bf),
        "c_cmask": np.broadcast_to(((np.arange(128)[None, :] // 32) == np.arange(4)[:, None]).astype(np.float32),
                                   (128, 4, 128)).astype(bf).copy(),
        "c_iota": np.broadcast_to(np.arange(CAP, dtype=np.float32), (128, CAP)).copy(),
        "c_rst16": np.broadcast_to(((np.arange(CAP) % 128) != 0).astype(np.float32), (128, CAP)).copy(),
        "c_U_bf": (np.arange(128)[:, None] < np.arange(128)[None, :]).astype(np.float32).astype(bf),
        "c_tcol": np.stack([np.arange(128, dtype=np.float32), np.ones(128, np.float32)], 1).astype(bf),
        "c_rst": np.broadcast_to(((np.arange(128) % 32) != 0).astype(np.float32), (128, 128)).copy(),
    }


def kernel(**inputs):
    nc = build()
    m = {kk_: np.ascontiguousarray(np.asarray(v, dtype=np.float32)) for kk_, v in inputs.items()}
    m.update(host_consts())
    res = run_bass_kernel_spmd(nc, [m], core_ids=[0])
    return np.asarray(res.results[0]["y"], dtype=np.float32).reshape(1, S, D)
```

```python
import numpy as np
import ml_dtypes
from concourse.bass_utils import run_bass_kernel_spmd

from contextlib import ExitStack
import concourse.bass as bass
import concourse.mybir as mybir

F32 = mybir.dt.float32
BF16 = mybir.dt.bfloat16
I32 = mybir.dt.int32
U32 = mybir.dt.uint32
AF = mybir.ActivationFunctionType
ALU = mybir.AluOpType
AX = mybir.AxisListType

SEM_ROT = 30000
SAME_ENGINE_SYNC = True


class Tl:
    def __init__(self, k, t, name, space):
        self.k = k
        self.t = t
        self.name = name
        self.space = space
        self.w = {}
        self.r = {}
        self.dsem = {}
        self.dcnt = {}
        k.tiles.append(self)

    def __getitem__(self, idx):
        return self.t[idx]

    def ap(self):
        return self.t.ap() if hasattr(self.t, "ap") else self.t[:]


class K:
    ENG = ("pe", "dve", "act", "pool", "sp")

    def __init__(self, nc):
        self.nc = nc
        self.es = ExitStack()
        self.eng = {"pe": nc.tensor, "dve": nc.vector, "act": nc.scalar,
                    "pool": nc.gpsimd, "sp": nc.sync}
        self.gen = {e: 0 for e in self.ENG}
        self.sem = {}
        self.cnt = {e: 0 for e in self.ENG}
        for e in self.ENG:
            self.sem[(e, 0)] = self.es.enter_context(nc.semaphore(f"s_{e}_0"))
        self.seen = {e: {} for e in self.ENG}
        self.seenD = {e: {} for e in self.ENG}
        self.nsem = len(self.ENG)
        self.uid = 0
        self.tiles = []
        self.stacks = []
        self.dsem_scopes = []
        self.free_dsems = {}

    def push(self):
        self.stacks.append(ExitStack())
        self.dsem_scopes.append([])

    def pop(self):
        self.barrier()
        for t, q in self.dsem_scopes.pop():
            if q in t.dsem:
                self.free_dsems.setdefault(q, []).append((t.dsem.pop(q), t.dcnt.pop(q)))
        for e in self.ENG:
            self.seenD[e] = {}
        self.stacks.pop().close()

    def _st(self):
        return self.stacks[-1] if self.stacks else self.es

    def sb(self, name, shape, dt):
        self.uid += 1
        t = self._st().enter_context(self.nc.sbuf_tensor(f"{name}_{self.uid}", list(shape), dt))
        return Tl(self, t, name, "sb")

    def ps(self, name, shape, dt=F32):
        self.uid += 1
        t = self._st().enter_context(self.nc.psum_tensor(f"{name}_{self.uid}", list(shape), dt))
        return Tl(self, t, name, "ps")

    def dram(self, name, shape, dt, kind="Internal"):
        t = self.nc.dram_tensor(name, list(shape), dt, kind=kind)
        return Tl(self, t, name, "dram")

    def close(self):
        while self.stacks:
            self.stacks.pop().close()
        self.es.close()


    def _wait(self, e, ev):
        if ev is None:
            return
        if ev[0] == "E":
            _, src, gen, c = ev
            if src == e and (e == "pe" or not SAME_ENGINE_SYNC):
                return
            key = (src, gen)
            if self.seen[e].get(key, 0) >= c:
                return
            self.eng[e].wait_ge(self.sem[key], c)
            self.seen[e][key] = c
        else:
            _, tl, q = ev
            if q not in tl.dsem:
                return
            tgt = tl.dcnt[q] * 16
            if self.seenD[e].get((id(tl), q), 0) >= tgt:
                return
            self.eng[e].wait_ge(tl.dsem[q], tgt)
            self.seenD[e][(id(tl), q)] = tgt

    @staticmethod
    def _key(ev):
        return ev[:3] if ev[0] == "E" else ("D", id(ev[1]), ev[2])

    def _deps(self, e, reads, writes, disjoint=False):
        for t in reads:
            for ev in t.w.values():
                self._wait(e, ev)
        if disjoint:
            return
        for t in writes:
            for ev in t.w.values():
                self._wait(e, ev)
            for ev in t.r.values():
                self._wait(e, ev)

    def _commit(self, ev, reads, writes, disjoint=False):
        for t in reads:
            if t in writes:
                continue
            t.r[self._key(ev)] = ev
        for t in writes:
            if disjoint:
                t.w[self._key(ev)] = ev
            else:
                t.w = {self._key(ev): ev}
                t.r = {}

    def barrier(self):
        for t in self.tiles:
            for ev in list(t.w.values()) + list(t.r.values()):
                self._wait("sp", ev)
            for q in list(t.dsem):
                self._wait("sp", ("D", t, q))
        for e in self.ENG:
            if e != "sp" and self.cnt[e] > 0:
                self._wait("sp", ("E", e, self.gen[e], self.cnt[e]))
        self.op("sp", lambda e: e.nop())
        ev = ("E", "sp", self.gen["sp"], self.cnt["sp"])
        for e in self.ENG:
            if e != "sp":
                self._wait(e, ev)
        for t in self.tiles:
            t.w = {}
            t.r = {}

    def op(self, e, fn, reads=(), writes=()):
        self._deps(e, reads, writes)
        if self.cnt[e] >= SEM_ROT:
            self.gen[e] += 1
            self.cnt[e] = 0
            self.sem[(e, self.gen[e])] = self.es.enter_context(
                self.nc.semaphore(f"s_{e}_{self.gen[e]}"))
            self.nsem += 1
        ins = fn(self.eng[e])
        self.cnt[e] += 1
        g = self.gen[e]
        ins.then_inc(self.sem[(e, g)], 1)
        ev = ("E", e, g, self.cnt[e])
        self._commit(ev, reads, writes)
        return ins

    def dma(self, q, fn, sbt, reads=(), writes=(), disjoint=False):
        self._deps(q, reads, writes, disjoint)
        if q not in sbt.dsem:
            self.uid += 1
            if self.free_dsems.get(q):
                sbt.dsem[q], sbt.dcnt[q] = self.free_dsems[q].pop()
            else:
                sbt.dsem[q] = self.es.enter_context(self.nc.semaphore(f"d_{q}_{sbt.name}_{self.uid}"))
                sbt.dcnt[q] = 0
                self.nsem += 1
            if self.dsem_scopes:
                self.dsem_scopes[-1].append((sbt, q))
        ins = fn(self.eng[q])
        ins.then_inc(sbt.dsem[q], 16)
        sbt.dcnt[q] += 1
        ev = ("D", sbt, q)
        self._commit(ev, reads, writes, disjoint)
        return ins

    def load(self, q, dst, dst_ap, src, src_ap, disjoint=False, **kw):
        return self.dma(q, lambda e: e.dma_start(out=dst_ap, in_=src_ap, **kw), dst,
                        reads=[src], writes=[dst], disjoint=disjoint)

    def store(self, q, dst, dst_ap, src, src_ap, disjoint=True, **kw):
        return self.dma(q, lambda e: e.dma_start(out=dst_ap, in_=src_ap, **kw), src,
                        reads=[src], writes=[dst], disjoint=disjoint)

    def finish(self, tiles, e="sp"):
        for t in tiles:
            for ev in list(t.w.values()) + list(t.r.values()):
                self._wait(e, ev)


class Pool:
    def __init__(self, k, name, shape, dt, n, space="sb"):
        mk = k.sb if space == "sb" else k.ps
        self.t = [mk(f"{name}{i}", shape, dt) for i in range(n)]
        self.i = 0

    def next(self):
        t = self.t[self.i % len(self.t)]
        self.i += 1
        return t

class _Dummy:
    pass
    pass

S = 16384
D = 1024
NBLK = S // 512
NTILE = S // 128
INW = 5376
EPS = 1e-6
O_Q, O_K, O_V, O_HQ, O_ZF, O_ZB, O_HI, O_HG, O_GL = 0, 512, 640, 768, 1280, 1792, 2304, 2816, 3328
NEXP = 16
FF = 2048
CAP = 2048


def mm(k, out, oap, lt, lap, rt, rap, start, stop):
    k.op("pe", lambda e: e.matmul(oap, lhsT=lap, rhs=rap, start=start, stop=stop),
         [lt, rt], [out])


def tp(k, out, oap, it, iap, ident):
    k.op("pe", lambda e: e.transpose(oap, iap, ident[:]), [it, ident], [out])


def colload(k, name, src, ap):
    t = k.sb(name, [128, 8], F32)
    k.load("sp", t, t[:], src, ap.rearrange("o (c p) -> p (o c)", p=128),
           allow_slow_non_contiguous=True)
    return t


def rowload(k, name, src, ap, n):
    t = k.sb(name, [128, n], F32)
    k.load("sp", t, t[:], src, ap.partition_broadcast(128))
    return t


def phase_mod(k, T):
    k.push()
    ccol = colload(k, "ccol", T.c, T.c.ap())
    cact = k.sb("cact", [128, 8], F32)
    k.op("act", lambda e: e.activation(out=cact[:], in_=ccol[:], func=AF.Silu), [ccol], [cact])
    wpool = Pool(k, "adaw", [128, 8, 512], F32, 2)
    brow = k.sb("brow", [1, 6144], F32)
    mrow = k.sb("mrow", [1, 6144], F32)
    pp = Pool(k, "pmod", [1, 512], F32, 2, "ps")
    for l in range(2):
        k.load("sp", brow, brow[:], T.ada_b, T.ada_b.ap()[l:l + 1, :])
        for nb in range(12):
            w = wpool.next()
            k.load("sp", w, w[:], T.ada_w,
                   T.ada_w.ap()[l].rearrange("(kc p) n -> p kc n", p=128)[:, :, nb * 512:(nb + 1) * 512])
            ps = pp.next()
            for kc in range(8):
                mm(k, ps, ps[:], cact, cact[:, kc:kc + 1], w, w[:, kc, :], kc == 0, kc == 7)
            k.op("dve", lambda e: e.tensor_tensor(out=mrow[:, nb * 512:(nb + 1) * 512], in0=ps[:],
                                                  in1=brow[:, nb * 512:(nb + 1) * 512], op=ALU.add),
                 [ps, brow], [mrow])
        k.store("sp", T.modd, T.modd.ap()[l:l + 1, :], mrow, mrow[:], disjoint=False)
    k.pop()


def layer_consts(k, T, l):
    C = _Dummy()
    md = T.modd.ap()
    sh1 = colload(k, "sh1", T.modd, md[l:l + 1, 0:1024])
    sc1 = colload(k, "sc1", T.modd, md[l:l + 1, 1024:2048])
    nw1 = colload(k, "nw1", T.norm_mix_w, T.norm_mix_w.ap()[l:l + 1, :])
    C.a1 = k.sb("a1", [128, 8], F32)
    C.b1 = sh1
    k.op("dve", lambda e: e.scalar_tensor_tensor(out=C.a1[:], in0=sc1[:], scalar=1.0, in1=nw1[:],
                                                 op0=ALU.add, op1=ALU.mult), [sc1, nw1], [C.a1])
    C.a2 = k.sb("a2", [128, 1024], F32)
    k.push()
    sc2 = rowload(k, "sc2", T.modd, md[l:l + 1, 4096:5120], 1024)
    nw2 = rowload(k, "nw2", T.norm_ffn_w, T.norm_ffn_w.ap()[l:l + 1, :], 1024)
    k.op("dve", lambda e: e.scalar_tensor_tensor(out=C.a2[:], in0=sc2[:], scalar=1.0, in1=nw2[:],
                                                 op0=ALU.add, op1=ALU.mult), [sc2, nw2], [C.a2])
    k.pop()
    C.b2 = rowload(k, "sh2", T.modd, md[l:l + 1, 3072:4096], 1024)
    C.g1 = rowload(k, "g1", T.modd, md[l:l + 1, 2048:3072], 1024)
    C.g2 = rowload(k, "g2", T.modd, md[l:l + 1, 5120:6144], 1024)
    C.qkw = k.sb("qkw", [128, 10, 64], F32)
    for h in range(10):
        src = T.q_norm_w if h < 8 else T.k_norm_w
        k.load("sp", C.qkw, C.qkw[:, h, :], src, src.ap()[l:l + 1, :].partition_broadcast(128))
    C.hgw = k.sb("hgw", [128, 4, 128], F32)
    for h in range(4):
        k.load("sp", C.hgw, C.hgw[:, h, :], T.hg_norm_w, T.hg_norm_w.ap()[l:l + 1, :].partition_broadcast(128))
    C.lb = k.sb("lb", [128, 8], F32)
    C.oml = k.sb("oml", [128, 8], F32)
    C.noml = k.sb("noml", [128, 8], F32)
    if l == 0:
        k.op("dve", lambda e: e.memset(C.lb[:], 0.0), [], [C.lb])
    else:
        a0 = k.sb("lba0", [128, 8], F32)
        a1 = k.sb("lba1", [128, 8], F32)
        hb = T.hg_lower_bounds.ap()
        k.load("sp", a0, a0[:], T.hg_lower_bounds, hb[0].rearrange("r (h d) -> d (r h)", d=128),
               allow_slow_non_contiguous=True)
        k.load("sp", a1, a1[:], T.hg_lower_bounds, hb[1].rearrange("r (h d) -> d (r h)", d=128),
               allow_slow_non_contiguous=True)
        k.op("dve", lambda e: e.tensor_tensor(out=a0[:], in0=a1[:], in1=a0[:], op=ALU.subtract), [a0, a1], [a0])
        k.op("act", lambda e: e.activation(out=C.lb[:], in_=a0[:], func=AF.Sigmoid), [a0], [C.lb])
    k.op("dve", lambda e: e.tensor_scalar(out=C.oml[:], in0=C.lb[:], scalar1=-1.0, scalar2=1.0,
                                          op0=ALU.mult, op1=ALU.add), [C.lb], [C.oml])
    k.op("dve", lambda e: e.tensor_scalar(out=C.noml[:], in0=C.oml[:], scalar1=-1.0, scalar2=None,
                                          op0=ALU.mult), [C.oml], [C.noml])
    return C


def phase_proj(k, T, C, l, xsrc, xsrc_ap):
    k.push()
    win = k.sb("win", [128, 8, INW], BF16)
    for kc in range(8):
        for cb in range(3):
            k.load("pool", win, win[:, kc, cb * 1792:(cb + 1) * 1792], T.w_in,
                   T.w_in.ap()[l, kc * 128:(kc + 1) * 128, cb * 1792:(cb + 1) * 1792], disjoint=True)
    xp = Pool(k, "xt", [128, 4, 1024], F32, 1)
    xnp = Pool(k, "xn", [128, 4, 1024], BF16, 1)
    hTp = Pool(k, "hT", [128, 8, 512], BF16, 1)
    junk = k.sb("junk", [128, 1024], BF16)
    ssp = Pool(k, "ss", [128, 4], F32, 2)
    rsp = Pool(k, "rs", [128, 4], F32, 2)
    pb = Pool(k, "pb", [128, 512], F32, 6, "ps")
    ptr = Pool(k, "ptr", [128, 1024], BF16, 2, "ps")
    qkp = Pool(k, "qk", [128, 10, 64], F32, 2)
    sqp = Pool(k, "sq", [128, 10, 64], F32, 2)
    t1p = Pool(k, "t1", [128, 10, 64], F32, 2)
    s10p = Pool(k, "s10", [128, 10], F32, 2)
    csp = Pool(k, "cs", [128, 2, 64], F32, 2)
    qbp = Pool(k, "qb", [128, 5, 128], BF16, 2)
    qTp = Pool(k, "qTs", [128, 5, 512], BF16, 1)
    vtp = Pool(k, "vt", [128, 4, 256], BF16, 2)
    hip = Pool(k, "hi", [128, 4, 512], BF16, 1)
    sgp = Pool(k, "sg", [128, 4, 512], BF16, 1)
    glp = Pool(k, "gl", [128, 2048], F32, 1)
    fmp = Pool(k, "fm", [128, 512], BF16, 3)
    f32p = Pool(k, "f32", [128, 512], F32, 4)
    sigp = Pool(k, "sig", [128, 512], F32, 2)
    for B in range(NBLK):
        r0 = B * 512
        xt = xp.next()
        k.load("sp", xt, xt[:], xsrc, xsrc_ap[r0:r0 + 512, :].rearrange("(t p) d -> p t d", p=128))
        ss = ssp.next()
        rs = rsp.next()
        xn = xnp.next()
        for t in range(4):
            k.op("act", lambda e: e.activation(out=junk[:], in_=xt[:, t, :], func=AF.Square,
                                               accum_out=ss[:, t:t + 1]), [xt], [junk, ss])
        k.op("act", lambda e: e.activation(out=rs[:], in_=ss[:], func=AF.Sqrt, scale=1.0 / D, bias=EPS),
             [ss], [rs])
        k.op("dve", lambda e: e.reciprocal(out=rs[:], in_=rs[:]), [rs], [rs])
        for t in range(4):
            k.op("act", lambda e: e.activation(out=xn[:, t, :], in_=xt[:, t, :], func=AF.Copy,
                                               scale=rs[:, t:t + 1]), [xt, rs], [xn])
        hT = hTp.next()
        for kc in range(8):
            p = ptr.next()
            for t in range(4):
                tp(k, p, p[:, t * 128:(t + 1) * 128], xn, xn[:, t, kc * 128:(kc + 1) * 128], T.ident_bf)
            k.op("dve", lambda e: e.tensor_scalar(out=hT[:, kc, :], in0=p[:, 0:512],
                                                  scalar1=C.a1[:, kc:kc + 1], scalar2=C.b1[:, kc:kc + 1],
                                                  op0=ALU.mult, op1=ALU.add), [p, C.a1, C.b1], [hT])

        def proj_tm(ps, t, c0, n):
            for kc in range(8):
                mm(k, ps, ps[:, 0:n], hT, hT[:, kc, t * 128:(t + 1) * 128], win, win[:, kc, c0:c0 + n],
                   kc == 0, kc == 7)

        def proj_fm(ps, c0):
            for kc in range(8):
                mm(k, ps, ps[:], win, win[:, kc, c0:c0 + 128], hT, hT[:, kc, :], kc == 0, kc == 7)

        qT = qTp.next()
        vt = vtp.next()
        k.op("pool", lambda e: e.memset(vt[:], 0.0), [], [vt])
        k.op("pool", lambda e: e.memset(vt[:, :, 64:65], 1.0), [], [vt])
        k.op("pool", lambda e: e.memset(vt[:, :, 192:193], 1.0), [], [vt])
        hi = hip.next()
        sg = sgp.next()
        for t in range(4):
            tr0 = r0 + t * 128
            cs = csp.next()
            k.load("sp", cs, cs[:, 0, :], T.cos64, T.cos64.ap()[tr0:tr0 + 128, :])
            k.load("sp", cs, cs[:, 1, :], T.sin64, T.sin64.ap()[tr0:tr0 + 128, :])
            ps1 = pb.next()
            proj_tm(ps1, t, O_Q, 512)
            ps2 = pb.next()
            proj_tm(ps2, t, O_K, 256)
            qk = qkp.next()
            k.op("act", lambda e: e.copy(out=qk[:, 0:8, :], in_=ps1[:].rearrange("p (h d) -> p h d", d=64)),
                 [ps1], [qk])
            k.op("act", lambda e: e.copy(out=qk[:, 8:10, :], in_=ps2[:, 0:128].rearrange("p (h d) -> p h d", d=64)),
                 [ps2], [qk])
            k.op("act", lambda e: e.copy(out=vt[:, t, :].rearrange("p (g d) -> p g d", d=128)[:, :, 0:64],
                                         in_=ps2[:, 128:256].rearrange("p (g d) -> p g d", d=64)), [ps2], [vt])
            sq = sqp.next()
            s10 = s10p.next()
            k.op("dve", lambda e: e.tensor_tensor(out=sq[:], in0=qk[:], in1=qk[:], op=ALU.mult), [qk], [sq])
            k.op("dve", lambda e: e.tensor_reduce(out=s10[:], in_=sq[:], axis=AX.X, op=ALU.add), [sq], [s10])
            k.op("act", lambda e: e.activation(out=s10[:], in_=s10[:], func=AF.Sqrt, scale=1.0 / 64, bias=EPS),
                 [s10], [s10])
            k.op("dve", lambda e: e.reciprocal(out=s10[:], in_=s10[:]), [s10], [s10])
            k.op("dve", lambda e: e.tensor_tensor(out=qk[:], in0=qk[:],
                                                  in1=s10[:].unsqueeze(2).to_broadcast([128, 10, 64]),
                                                  op=ALU.mult), [qk, s10], [qk])
            k.op("pool", lambda e: e.tensor_tensor(out=qk[:], in0=qk[:], in1=C.qkw[:], op=ALU.mult),
                 [qk, C.qkw], [qk])
            t1 = t1p.next()
            k.op("pool", lambda e: e.tensor_tensor(out=t1[:], in0=qk[:],
                                                   in1=cs[:, 0:1, :].to_broadcast([128, 10, 64]), op=ALU.mult),
                 [qk, cs], [t1])
            qv = qk[:].rearrange("p h (a f j) -> p h a f j", a=2, f=2)
            sv = cs[:, 1, :].rearrange("p (a f j) -> p a f j", a=2, f=2)
            sqv = sq[:].rearrange("p h (a f j) -> p h a f j", a=2, f=2)
            for f in range(2):
                k.op("dve", lambda e: e.tensor_tensor(
                    out=sqv[:, :, :, f, :], in0=qv[:, :, :, 1 - f, :],
                    in1=sv[:, :, f, :].unsqueeze(1).to_broadcast([128, 10, 2, 16]), op=ALU.mult),
                    [qk, cs], [sq])
            qb = qbp.next()
            k.op("dve", lambda e: e.tensor_tensor(
                out=qb[:, 0:4, :].rearrange("p j (g d) -> p g j d", g=2),
                in0=t1[:, 0:8, :].rearrange("p (g j) d -> p g j d", g=2),
                in1=sq[:, 0:8, :].rearrange("p (g j) d -> p g j d", g=2), op=ALU.add), [t1, sq], [qb])
            k.op("pool", lambda e: e.tensor_tensor(
                out=qb[:, 4, :].rearrange("p (g d) -> p g d", g=2),
                in0=t1[:, 8:10, :], in1=sq[:, 8:10, :], op=ALU.add), [t1, sq], [qb])
            p = ptr.next()
            for j in range(5):
                tp(k, p, p[:, j * 128:(j + 1) * 128], qb, qb[:, j, :], T.ident_bf)
            k.op("act", lambda e: e.copy(out=qT[:, :, t * 128:(t + 1) * 128],
                                         in_=p[:, 0:640].rearrange("p (j q) -> p j q", q=128)), [p], [qT])
            ps = pb.next()
            proj_tm(ps, t, O_HI, 512)
            k.op("dve", lambda e: e.tensor_copy(out=hi[:, t, :], in_=ps[:]), [ps], [hi])
            ps = pb.next()
            proj_tm(ps, t, O_HG, 512)
            k.op("act", lambda e: e.activation(out=sg[:, t, :], in_=ps[:], func=AF.Silu), [ps], [sg])
            gl = glp.next()
            for c in range(4):
                ps = pb.next()
                proj_tm(ps, t, O_GL + c * 512, 512)
                k.op("act", lambda e: e.activation(out=gl[:, c * 512:(c + 1) * 512], in_=ps[:], func=AF.Sigmoid),
                     [ps], [gl])
            k.store("pool", T.gl_d, T.gl_d.ap()[tr0:tr0 + 128, :], gl, gl[:])
        k.store("pool", T.qT_d, T.qT_d.ap()[:, :, r0:r0 + 512], qT, qT[:, 0:4, :])
        k.store("pool", T.kT_d, T.kT_d.ap()[:, r0:r0 + 512], qT, qT[:, 4, :])
        k.store("pool", T.va_d, T.va_d.ap()[r0:r0 + 512, :].rearrange("(t p) c -> p t c", p=128), vt, vt[:])
        k.store("pool", T.vh_d, T.vh_d.ap()[r0:r0 + 512, :].rearrange("(t p) c -> p t c", p=128), hi, hi[:])
        k.store("pool", T.sg_d, T.sg_d.ap()[r0:r0 + 512, :].rearrange("(t p) c -> p t c", p=128), sg, sg[:])
        for h in range(4):
            ps = pb.next()
            proj_fm(ps, O_HQ + h * 128)
            o = fmp.next()
            k.op("act", lambda e: e.activation(out=o[:], in_=ps[:], func=AF.Silu), [ps], [o])
            k.store("pool", T.hq_d, T.hq_d.ap()[h, :, r0:r0 + 512], o, o[:])
        for dr in range(2):
            for h in range(4):
                ci = dr * 4 + h
                ps = pb.next()
                proj_fm(ps, (O_ZF if dr == 0 else O_ZB) + h * 128)
                sig = sigp.next()
                k.op("act", lambda e: e.activation(out=sig[:], in_=ps[:], func=AF.Sigmoid), [ps], [sig])
                f = f32p.next()
                k.op("dve", lambda e: e.tensor_scalar(out=f[:], in0=sig[:], scalar1=C.oml[:, ci:ci + 1],
                                                      scalar2=C.lb[:, ci:ci + 1], op0=ALU.mult, op1=ALU.add),
                     [sig, C.oml, C.lb], [f])
                k.op("pool", lambda e: e.tensor_scalar(out=f[:], in0=f[:], scalar1=1e-6, scalar2=None,
                                                       op0=ALU.max), [f], [f])
                k.op("act", lambda e: e.activation(out=f[:], in_=f[:], func=AF.Ln), [f], [f])
                k.store("pool", T.g_d, T.g_d.ap()[dr, h, :, r0:r0 + 512], f, f[:])
                kk = f32p.next()
                k.op("dve", lambda e: e.tensor_scalar(out=kk[:], in0=sig[:], scalar1=C.noml[:, ci:ci + 1],
                                                      scalar2=C.oml[:, ci:ci + 1], op0=ALU.mult, op1=ALU.add),
                     [sig, C.noml, C.oml], [kk])
                k.store("pool", T.kk_d, T.kk_d.ap()[dr, h, :, r0:r0 + 512], kk, kk[:])
    k.pop()


def phase_attn(k, T):
    k.push()
    kT = k.sb("kT", [128, S], BF16)
    va = k.sb("va", [128, NTILE, 256], BF16)
    for i in range(4):
        k.load("sp", kT, kT[:, i * 4096:(i + 1) * 4096], T.kT_d, T.kT_d.ap()[:, i * 4096:(i + 1) * 4096],
               disjoint=True)
        k.load("sp", va, va[:, i * 32:(i + 1) * 32, :], T.va_d,
               T.va_d.ap()[i * 4096:(i + 1) * 4096, :].rearrange("(t p) c -> p t c", p=128), disjoint=True)
    qz = [Pool(k, f"qz{g}", [128, 4, 512], BF16, 2) for g in range(2)]
    for g in range(2):
        for t_ in qz[g].t:
            k.op("pool", lambda e: e.memset(t_[:], 0.0), [], [t_])
    psc = Pool(k, "psc", [128, 512], F32, 3, "ps")
    pac = Pool(k, "pac", [128, 512], F32, 4, "ps")
    ppx = k.ps("ppx", [128, 8, 128], BF16)
    ptp = Pool(k, "pT", [128, 512], BF16, 4)
    ohp = Pool(k, "ohi", [65, 512], BF16, 2)
    olp = Pool(k, "olo", [65, 512], BF16, 2)
    otp = Pool(k, "otm", [128, 4, 65], F32, 2)
    rcp = Pool(k, "rc", [128, 4], F32, 2)
    oap = Pool(k, "oa", [128, 4, 512], BF16, 2)
    for B in range(NBLK):
        r0 = B * 512
        qzb = [qz[g].next() for g in range(2)]
        for g in range(2):
            k.load("sp", qzb[g], qzb[g][64 * g:64 * g + 64, :, :], T.qT_d, T.qT_d.ap()[64 * g:64 * g + 64, :, r0:r0 + 512])
        oa = oap.next()
        for g in range(2):
            acc = [pac.next() for _ in range(4)]
            for kc in range(NTILE):
                for j in range(4):
                    ps = psc.next()
                    mm(k, ps, ps[:], kT, kT[:, kc * 128:(kc + 1) * 128], qzb[g], qzb[g][:, j, :], True, True)
                    pT = ptp.next()
                    k.op("act", lambda e: e.activation(out=pT[:], in_=ps[:], func=AF.Exp, scale=0.125), [ps], [pT])
                    mm(k, acc[j], acc[j][:], va, va[:, kc, g * 128:(g + 1) * 128], pT, pT[:],
                       kc == 0, kc == NTILE - 1)
            for j in range(4):
                h = g * 4 + j
                ohi = ohp.next(); olo = olp.next()
                k.op("act", lambda e: e.copy(out=ohi[:], in_=acc[j][0:65, :]), [acc[j]], [ohi])
                k.op("dve", lambda e: e.tensor_tensor(out=olo[:], in0=acc[j][0:65, :], in1=ohi[:], op=ALU.subtract),
                     [acc[j], ohi], [olo])
                for t in range(4):
                    tp(k, ppx, ppx[:, t, 0:65], ohi, ohi[:, t * 128:(t + 1) * 128], T.ident65)
                    tp(k, ppx, ppx[:, 4 + t, 0:65], olo, olo[:, t * 128:(t + 1) * 128], T.ident65)
                otm = otp.next()
                k.op("act", lambda e: e.copy(out=otm[:], in_=ppx[:, 0:4, 0:65]), [ppx], [otm])
                k.op("dve", lambda e: e.tensor_tensor(out=otm[:], in0=otm[:], in1=ppx[:, 4:8, 0:65], op=ALU.add),
                     [otm, ppx], [otm])
                rc = rcp.next()
                k.op("dve", lambda e: e.reciprocal(out=rc[:], in_=otm[:, :, 64]), [otm], [rc])
                k.op("dve", lambda e: e.tensor_tensor(out=oa[:, :, h * 64:(h + 1) * 64], in0=otm[:, :, 0:64],
                                                      in1=rc[:].unsqueeze(2).to_broadcast([128, 4, 64]),
                                                      op=ALU.mult), [otm, rc], [oa])
        k.store("pool", T.oatt_d, T.oatt_d.ap()[r0:r0 + 512, :].rearrange("(t p) c -> p t c", p=128), oa, oa[:])
    k.pop()


def phase_hgrn(k, T):
    k.push()
    HD = [(h, dr) for dr in range(2) for h in range(4)]
    S32 = {hd: k.sb(f"S32_{hd[0]}{hd[1]}", [128, 128], F32) for hd in HD}
    S16 = {hd: k.sb(f"S16_{hd[0]}{hd[1]}", [128, 128], BF16) for hd in HD}
    for hd in HD:
        k.op("pool", lambda e: e.memset(S32[hd][:], 0.0), [], [S32[hd]])
        k.op("pool", lambda e: e.memset(S16[hd][:], 0.0), [], [S16[hd]])
    N = 8
    hqp = Pool(k, "hq", [128, 128], BF16, 2 * N)
    gp = Pool(k, "g", [128, 128], F32, 2 * N)
    kkp = Pool(k, "kk", [128, 128], F32, 2 * N)
    vp = Pool(k, "v", [128, 128], BF16, 2 * N)
    for i_ in range(2 * N):
        for p_ in (gp, kkp, vp):
            p_.t[i_].dsem = hqp.t[i_].dsem
            p_.t[i_].dcnt = hqp.t[i_].dcnt
    bp = Pool(k, "b", [128, 128], F32, N)
    b2p = Pool(k, "b2", [128, 128], F32, N)
    ebp = Pool(k, "eb", [128, 128], F32, N)
    tmp = Pool(k, "tmp", [128, 128], F32, N)
    qtp = Pool(k, "qt", [128, 128], BF16, N)
    ktp = Pool(k, "kt", [128, 128], BF16, N)
    khtp = Pool(k, "kht", [128, 128], BF16, N)
    scp = Pool(k, "sc", [128, 128], BF16, N)
    osp = Pool(k, "os", [128, 128], F32, N)
    po = [k.ps(f"po{i}", [128, 4, 128], F32) for i in range(2)]
    psb = k.ps("psb", [128, 4, 128], F32)
    ptbs = [k.ps(f"ptb{i}", [128, 2, 4, 128], BF16) for i in range(2)]
    pub = [k.ps(f"pu{i}", [128, 4, 128], F32) for i in range(2)]
    qpp = Pool(k, "qpad", [128, 4, 128], BF16, N)
    khpp = Pool(k, "khpad", [128, 4, 128], BF16, N)
    khtpp = Pool(k, "khtpad", [128, 4, 128], BF16, N)
    for step in range(NTILE):
        st = {}
        for i, (h, dr) in enumerate(HD):
            Tt = step if dr == 0 else NTILE - 1 - step
            c0 = Tt * 128
            hq = hqp.next(); g = gp.next(); kk = kkp.next(); v = vp.next()
            k.load("sp", hq, hq[:], T.hq_d, T.hq_d.ap()[h, :, c0:c0 + 128])
            k.load("sp", g, g[:], T.g_d, T.g_d.ap()[dr, h, :, c0:c0 + 128])
            k.load("sp", kk, kk[:], T.kk_d, T.kk_d.ap()[dr, h, :, c0:c0 + 128])
            k.load("sp", v, v[:], T.vh_d, T.vh_d.ap()[c0:c0 + 128, h * 128:(h + 1) * 128])
            b = bp.next()
            k.op("dve", lambda e: e.tensor_tensor_scan(out=b[:], data0=T.rst[:], data1=g[:], initial=0.0,
                                                       op0=ALU.mult, op1=ALU.add), [T.rst, g], [b])
            b3 = b[:].rearrange("p (c i) -> p c i", i=32)
            if dr == 1:
                b2 = b2p.next()
                k.op("pool", lambda e: e.tensor_tensor(out=b2[:].rearrange("p (c i) -> p c i", i=32),
                                                       in0=b3[:, :, 31:32].to_broadcast([128, 4, 32]),
                                                       in1=b3, op=ALU.subtract), [b], [b2])
                k.op("pool", lambda e: e.tensor_tensor(out=b2[:], in0=b2[:], in1=g[:], op=ALU.add), [b2, g], [b2])
                b = b2
                b3 = b[:].rearrange("p (c i) -> p c i", i=32)
            le = 31 if dr == 0 else 0
            eb = ebp.next()
            k.op("act", lambda e: e.activation(out=eb[:], in_=b[:], func=AF.Exp), [b], [eb])
            qt = qtp.next()
            k.op("dve", lambda e: e.tensor_tensor(out=qt[:], in0=hq[:], in1=eb[:], op=ALU.mult), [hq, eb], [qt])
            t1 = tmp.next()
            k.op("act", lambda e: e.activation(out=t1[:], in_=b[:], func=AF.Exp, scale=-1.0), [b], [t1])
            kt = ktp.next()
            k.op("dve", lambda e: e.tensor_tensor(out=kt[:], in0=kk[:], in1=t1[:], op=ALU.mult), [kk, t1], [kt])
            t2 = tmp.next()
            k.op("pool", lambda e: e.tensor_tensor(out=t2[:].rearrange("p (c i) -> p c i", i=32),
                                                   in0=b3[:, :, le:le + 1].to_broadcast([128, 4, 32]),
                                                   in1=b3, op=ALU.subtract), [b], [t2])
            k.op("act", lambda e: e.activation(out=t2[:], in_=t2[:], func=AF.Exp), [t2], [t2])
            kht = khtp.next()
            k.op("pool", lambda e: e.tensor_tensor(out=kht[:], in0=kk[:], in1=t2[:], op=ALU.mult), [kk, t2], [kht])
            khtpad = khtpp.next()
            k.op("pool", lambda e: e.tensor_tensor(out=khtpad[:], in0=kht[:].unsqueeze(1).to_broadcast([128, 4, 128]),
                                                   in1=T.cmask[:], op=ALU.mult), [kht, T.cmask], [khtpad])
            ptb = ptbs[(i // 2) % 2]
            for c in range(4):
                tp(k, ptb, ptb[:, i % 2, c, :], khtpad, khtpad[:, c, :], T.ident_bf)
            kh = khpp.next()
            k.op("act", lambda e: e.copy(out=kh[:], in_=ptb[:, i % 2, :, :]), [ptb], [kh])
            qpad = qpp.next()
            k.op("dve", lambda e: e.tensor_tensor(out=qpad[:], in0=qt[:].unsqueeze(1).to_broadcast([128, 4, 128]),
                                                  in1=T.cmask[:], op=ALU.mult), [qt, T.cmask], [qpad])
            mm(k, psb, psb[:, i % 4, :], kt, kt[:], qt, qt[:], True, True)
            sc = scp.next()
            mk = T.mask_f if dr == 0 else T.mask_b
            k.op("dve", lambda e: e.tensor_tensor(out=sc[:], in0=psb[:, i % 4, :], in1=mk[:], op=ALU.mult),
                 [psb, mk], [sc])
            o = po[i // 4]
            k.op("pe", lambda e: e.matmul(o[:, i % 4, :], lhsT=sc[:], rhs=v[:], start=(i % 4 == 0), stop=False,
                                           skip_group_check=True), [sc, v], [o])
            st[(h, dr)] = (qpad, kh, v, eb, le, Tt)
        for ci in range(4):
            for i, (h, dr) in enumerate(HD):
                qt, kh, v, eb, le, Tt = st[(h, dr)]
                c = ci if dr == 0 else 3 - ci
                o = po[i // 4]
                k.op("pe", lambda e: e.matmul(o[:, i % 4, :], lhsT=qt[:, c, :],
                                               rhs=S16[(h, dr)][:], start=False, stop=(ci == 3),
                                               skip_group_check=True), [qt, S16[(h, dr)]], [o])
                pu = pub[i // 4]
                k.op("pe", lambda e: e.matmul(pu[:, i % 4, :], lhsT=kh[:, c, :],
                                               rhs=v[:], start=True, stop=True,
                                               skip_group_check=True), [kh, v], [pu])
                s32 = S32[(h, dr)]
                dcol = eb[:, 32 * c + le:32 * c + le + 1]
                k.op("dve", lambda e: e.scalar_tensor_tensor(out=s32[:], in0=s32[:], scalar=dcol,
                                                             in1=pu[:, i % 4, :], op0=ALU.mult, op1=ALU.add),
                     [s32, eb, pu], [s32])
                k.op("act", lambda e: e.copy(out=S16[(h, dr)][:], in_=s32[:]), [s32], [S16[(h, dr)]])
        for i, (h, dr) in enumerate(HD):
            Tt = st[(h, dr)][5]
            os_ = osp.next()
            k.op("act", lambda e: e.copy(out=os_[:], in_=po[i // 4][:, i % 4, :]), [po[i // 4]], [os_])
            dst = T.of_d if dr == 0 else T.ob_d
            k.store("pool", dst, dst.ap()[Tt * 128:(Tt + 1) * 128, h * 128:(h + 1) * 128], os_, os_[:])
    k.pop()


def phase_merge(k, T, C, l, xsrc, xsrc_ap, acc, acc_ap):
    import os
    CUT = float(os.environ.get('MCUT', '99'))
    k.push()
    wba = k.sb("wba", [128, 4, 1024], BF16)
    wbh = k.sb("wbh", [128, 4, 1024], BF16)
    wout = k.sb("wout", [128, 8, 1024], BF16)
    wr = k.sb("wr", [128, 8, 16], F32)
    stg = Pool(k, "stg", [128, 1024], F32, 2)
    for kc in range(4):
        k.load("pool", wba, wba[:, kc, :], T.w_branch_att, T.w_branch_att.ap()[l, kc * 128:(kc + 1) * 128, :], disjoint=True)
        k.load("pool", wbh, wbh[:, kc, :], T.w_branch_hg, T.w_branch_hg.ap()[l, kc * 128:(kc + 1) * 128, :], disjoint=True)
    for kc in range(8):
        s_ = stg.next()
        k.load("sp", s_, s_[:], T.w_out, T.w_out.ap()[l, kc * 128:(kc + 1) * 128, :])
        k.op("dve", lambda e: e.tensor_tensor(out=wout[:, kc, :], in0=s_[:], in1=C.g1[:], op=ALU.mult),
             [s_, C.g1], [wout])
    k.load("sp", wr, wr[:], T.w_router, T.w_router.ap()[l].rearrange("(kc p) e -> p kc e", p=128))
    wr3 = [k.sb(f"wr3_{i}", [128, 8, 16], BF16) for i in range(3)]
    wrr = k.sb("wrr", [128, 8, 16], F32)
    k.op("act", lambda e: e.copy(out=wr3[0][:], in_=wr[:]), [wr], [wr3[0]])
    k.op("dve", lambda e: e.tensor_tensor(out=wrr[:], in0=wr[:], in1=wr3[0][:], op=ALU.subtract), [wr, wr3[0]], [wrr])
    k.op("act", lambda e: e.copy(out=wr3[1][:], in_=wrr[:]), [wrr], [wr3[1]])
    k.op("dve", lambda e: e.tensor_tensor(out=wrr[:], in0=wrr[:], in1=wr3[1][:], op=ALU.subtract), [wrr, wr3[1]], [wrr])
    k.op("act", lambda e: e.copy(out=wr3[2][:], in_=wrr[:]), [wrr], [wr3[2]])
    oap = Pool(k, "oat", [128, 512], BF16, 2)
    ofp = Pool(k, "of", [128, 4, 128], F32, 2)
    obp = Pool(k, "ob", [128, 4, 128], F32, 2)
    sgp = Pool(k, "sgm", [128, 512], BF16, 2)
    glp = Pool(k, "glm", [128, 2048], F32, 2)
    xp = Pool(k, "xm", [128, 1024], F32, 2)
    sqp = Pool(k, "sqm", [128, 4, 128], F32, 2)
    s4p = Pool(k, "s4", [128, 4], F32, 2)
    ohp = Pool(k, "ohb", [128, 512], BF16, 2)
    lTp = Pool(k, "lT", [128, 8, 128], BF16, 2)
    m1p = Pool(k, "m1", [128, 1024], F32, 2)
    m2p = Pool(k, "m2", [128, 1024], F32, 2)
    mbp = Pool(k, "mb", [128, 1024], BF16, 2)
    mTp = Pool(k, "mT", [128, 8, 128], BF16, 2)
    x1p = Pool(k, "x1", [128, 1024], F32, 2)
    junk = k.sb("junkm", [128, 1024], F32)
    s1p = Pool(k, "s1", [128, 1], F32, 4)
    h2p = Pool(k, "h2", [128, 1024], F32, 2)
    h2bp = Pool(k, "h2b", [128, 8, 128], BF16, 6)
    hbp = [Pool(k, f"hb{i}", [128, 1024], BF16, 2 if i == 0 else 1) for i in range(3)]
    r1p = Pool(k, "r1", [128, 1024], F32, 1)
    lgp = Pool(k, "lg", [128, 16], F32, 2)
    pbk = Pool(k, "pbk", [128, 512], F32, 6, "ps")
    ptb = k.ps("ptbm", [128, 1024], BF16)
    plg = k.ps("plg", [128, 16], F32)
    for Tt in range(int(os.environ.get('MTILES', NTILE))):
        r0 = Tt * 128
        oat = oap.next(); of = ofp.next(); ob = obp.next(); sg = sgp.next(); gl = glp.next(); xm = xp.next()
        k.load("sp", oat, oat[:], T.oatt_d, T.oatt_d.ap()[r0:r0 + 128, :])
        k.load("sp", of, of[:], T.of_d, T.of_d.ap()[r0:r0 + 128, :].rearrange("p (h e) -> p h e", e=128))
        k.load("sp", ob, ob[:], T.ob_d, T.ob_d.ap()[r0:r0 + 128, :].rearrange("p (h e) -> p h e", e=128))
        k.load("sp", sg, sg[:], T.sg_d, T.sg_d.ap()[r0:r0 + 128, :])
        k.load("sp", gl, gl[:], T.gl_d, T.gl_d.ap()[r0:r0 + 128, :])
        k.load("sp", xm, xm[:], xsrc, xsrc_ap[r0:r0 + 128, :])
        k.op("pool", lambda e: e.tensor_tensor(out=of[:], in0=of[:], in1=ob[:], op=ALU.add), [of, ob], [of])
        sq = sqp.next(); s4 = s4p.next()
        k.op("dve", lambda e: e.tensor_tensor(out=sq[:], in0=of[:], in1=of[:], op=ALU.mult), [of], [sq])
        k.op("dve", lambda e: e.tensor_reduce(out=s4[:], in_=sq[:], axis=AX.X, op=ALU.add), [sq], [s4])
        k.op("act", lambda e: e.activation(out=s4[:], in_=s4[:], func=AF.Sqrt, scale=1.0 / 128, bias=EPS), [s4], [s4])
        k.op("dve", lambda e: e.reciprocal(out=s4[:], in_=s4[:]), [s4], [s4])
        k.op("dve", lambda e: e.tensor_tensor(out=of[:], in0=of[:], in1=s4[:].unsqueeze(2).to_broadcast([128, 4, 128]),
                                              op=ALU.mult), [of, s4], [of])
        k.op("pool", lambda e: e.tensor_tensor(out=of[:], in0=of[:], in1=C.hgw[:], op=ALU.mult), [of, C.hgw], [of])
        ohb = ohp.next()
        k.op("dve", lambda e: e.tensor_tensor(out=ohb[:].rearrange("p (h e) -> p h e", e=128), in0=of[:],
                                              in1=sg[:].rearrange("p (h e) -> p h e", e=128), op=ALU.mult),
             [of, sg], [ohb])
        if CUT < 1:
            continue
        for j in range(4):
            tp(k, ptb, ptb[:, j * 128:(j + 1) * 128], oat, oat[:, j * 128:(j + 1) * 128], T.ident_bf)
            tp(k, ptb, ptb[:, (4 + j) * 128:(5 + j) * 128], ohb, ohb[:, j * 128:(j + 1) * 128], T.ident_bf)
        lT = lTp.next()
        k.op("act", lambda e: e.copy(out=lT[:], in_=ptb[:].rearrange("p (j q) -> p j q", q=128)), [ptb], [lT])
        if CUT < 2:
            continue
        m1 = m1p.next(); m2 = m2p.next()
        for hf in range(2):
            pa = pbk.next()
            for kc in range(4):
                mm(k, pa, pa[:], lT, lT[:, kc, :], wba, wba[:, kc, hf * 512:(hf + 1) * 512], kc == 0, kc == 3)
            k.op("dve", lambda e: e.tensor_tensor(out=m1[:, hf * 512:(hf + 1) * 512], in0=pa[:],
                                                  in1=gl[:, hf * 512:(hf + 1) * 512], op=ALU.mult), [pa, gl], [m1])
            ph = pbk.next()
            for kc in range(4):
                mm(k, ph, ph[:], lT, lT[:, 4 + kc, :], wbh, wbh[:, kc, hf * 512:(hf + 1) * 512], kc == 0, kc == 3)
            k.op("dve", lambda e: e.tensor_tensor(out=m2[:, hf * 512:(hf + 1) * 512], in0=ph[:],
                                                  in1=gl[:, 1024 + hf * 512:1024 + (hf + 1) * 512], op=ALU.mult),
                 [ph, gl], [m2])
        mb = mbp.next()
        k.op("pool", lambda e: e.tensor_tensor(out=mb[:], in0=m1[:], in1=m2[:], op=ALU.add), [m1, m2], [mb])
        for j in range(8):
            tp(k, ptb, ptb[:, j * 128:(j + 1) * 128], mb, mb[:, j * 128:(j + 1) * 128], T.ident_bf)
        mT = mTp.next()
        k.op("act", lambda e: e.copy(out=mT[:], in_=ptb[:].rearrange("p (j q) -> p j q", q=128)), [ptb], [mT])
        x1 = x1p.next()
        for hf in range(2):
            po = pbk.next()
            for kc in range(8):
                mm(k, po, po[:], mT, mT[:, kc, :], wout, wout[:, kc, hf * 512:(hf + 1) * 512], kc == 0, kc == 7)
            k.op("dve", lambda e: e.tensor_tensor(out=x1[:, hf * 512:(hf + 1) * 512], in0=po[:],
                                                  in1=xm[:, hf * 512:(hf + 1) * 512], op=ALU.add), [po, xm], [x1])
        if CUT < 3:
            continue
        k.store("pool", T.xs1, T.xs1.ap()[r0:r0 + 128, :], x1, x1[:])
        k.store("pool", acc, acc_ap[r0:r0 + 128, :], x1, x1[:])
        if CUT < 3.1:
            continue
        s1 = s1p.next()
        k.op("act", lambda e: e.activation(out=junk[:], in_=x1[:], func=AF.Square, accum_out=s1[:]), [x1], [junk, s1])
        if CUT < 3.2:
            continue
        k.op("act", lambda e: e.activation(out=s1[:], in_=s1[:], func=AF.Sqrt, scale=1.0 / D, bias=EPS), [s1], [s1])
        k.op("dve", lambda e: e.reciprocal(out=s1[:], in_=s1[:]), [s1], [s1])
        if CUT < 3.3:
            continue
        h2 = h2p.next()
        k.op("act", lambda e: e.activation(out=h2[:], in_=x1[:], func=AF.Copy, scale=s1[:]), [x1, s1], [h2])
        if CUT < 3.4:
            continue
        k.op("dve", lambda e: e.tensor_tensor(out=h2[:], in0=h2[:], in1=C.a2[:], op=ALU.mult), [h2, C.a2], [h2])
        if CUT < 3.5:
            continue
        k.op("dve", lambda e: e.tensor_tensor(out=h2[:], in0=h2[:], in1=C.b2[:], op=ALU.add), [h2, C.b2], [h2])
        if CUT < 4:
            continue
        hb = [hbp[i].next() for i in range(3)]
        r1 = r1p.next()
        k.op("act", lambda e: e.copy(out=hb[0][:], in_=h2[:]), [h2], [hb[0]])
        k.op("dve", lambda e: e.tensor_tensor(out=r1[:], in0=h2[:], in1=hb[0][:], op=ALU.subtract), [h2, hb[0]], [r1])
        k.op("act", lambda e: e.copy(out=hb[1][:], in_=r1[:]), [r1], [hb[1]])
        k.op("dve", lambda e: e.tensor_tensor(out=r1[:], in0=r1[:], in1=hb[1][:], op=ALU.subtract), [r1, hb[1]], [r1])
        k.op("act", lambda e: e.copy(out=hb[2][:], in_=r1[:]), [r1], [hb[2]])
        hT3 = [h2bp.next() for _ in range(3)]
        for i in range(3):
            for j in range(8):
                tp(k, ptb, ptb[:, j * 128:(j + 1) * 128], hb[i], hb[i][:, j * 128:(j + 1) * 128], T.ident_bf)
            k.op("act" if i != 1 else "dve", lambda e: e.tensor_copy(out=hT3[i][:], in_=ptb[:].rearrange("p (j q) -> p j q", q=128))
                 if i == 1 else e.copy(out=hT3[i][:], in_=ptb[:].rearrange("p (j q) -> p j q", q=128)), [ptb], [hT3[i]])
        k.store("pool", T.h2b_d, T.h2b_d.ap()[r0:r0 + 128, :], hb[0], hb[0][:])
        if CUT < 5:
            continue
        terms = [(0, 0), (0, 1), (1, 0), (0, 2), (2, 0), (1, 1)]
        for ti, (a_, b_) in enumerate(terms):
            for kc in range(8):
                mm(k, plg, plg[:], hT3[a_], hT3[a_][:, kc, :], wr3[b_], wr3[b_][:, kc, :],
                   ti == 0 and kc == 0, ti == len(terms) - 1 and kc == 7)
        if CUT < 6:
            continue
        lg = lgp.next()
        mx = s1p.next(); se = s1p.next()
        k.op("dve", lambda e: e.tensor_reduce(out=mx[:], in_=plg[:], axis=AX.X, op=ALU.max), [plg], [mx])
        k.op("dve", lambda e: e.tensor_scalar(out=mx[:], in0=mx[:], scalar1=-1.0, scalar2=None, op0=ALU.mult), [mx], [mx])
        k.op("act", lambda e: e.activation(out=lg[:], in_=plg[:], func=AF.Exp, bias=mx[:], accum_out=se[:]),
             [plg, mx], [lg, se])
        k.op("dve", lambda e: e.reciprocal(out=se[:], in_=se[:]), [se], [se])
        k.op("dve", lambda e: e.tensor_scalar(out=lg[:], in0=lg[:], scalar1=se[:], scalar2=None, op0=ALU.mult),
             [lg, se], [lg])
        k.store("pool", T.aff_d, T.aff_d.ap()[r0:r0 + 128, :], lg, lg[:])
    k.pop()


def phase_route(k, T, idx_all):
    k.push()
    for nm, shp, dt in (("iota", [128, CAP], F32), ("rst16", [128, CAP], F32), ("U_bf", [128, 128], BF16), ("tcol", [128, 2], BF16)):
        t = k.sb(nm, shp, dt)
        src = getattr(T, "c_" + nm)
        k.load("sp", t, t[:], src, src.ap())
        setattr(T, nm, t)
    aff = k.sb("affall", [128, 128, NEXP], F32)
    k.load("sp", aff, aff[:], T.aff_d, T.aff_d.ap().rearrange("(t p) e -> t p e", p=128))
    cmp = k.sb("cmp", [128, 128, NEXP], F32)
    ones = k.sb("ones", [128, 128], F32)
    k.op("pool", lambda e: e.memset(ones[:], 1.0), [], [ones])
    lo = k.sb("lo", [128, NEXP], F32); hi = k.sb("hi", [128, NEXP], F32)
    mid = k.sb("mid", [128, NEXP], F32); cnt = k.sb("cnt", [128, NEXP], F32)
    ge = k.sb("ge", [128, NEXP], F32); d1 = k.sb("d1", [128, NEXP], F32)
    pt = k.ps("ptot", [128, NEXP], F32)
    k.op("dve", lambda e: e.memset(lo[:], 0.0), [], [lo])
    k.op("dve", lambda e: e.memset(hi[:], 1.0), [], [hi])
    for it in range(40):
        k.op("dve", lambda e: e.tensor_tensor(out=mid[:], in0=lo[:], in1=hi[:], op=ALU.add), [lo, hi], [mid])
        k.op("dve", lambda e: e.tensor_scalar(out=mid[:], in0=mid[:], scalar1=0.5, scalar2=None, op0=ALU.mult), [mid], [mid])
        k.op("dve", lambda e: e.tensor_tensor(out=cmp[:], in0=aff[:],
                                              in1=mid[:].unsqueeze(1).to_broadcast([128, 128, NEXP]),
                                              op=ALU.is_gt), [aff, mid], [cmp])
        k.op("dve", lambda e: e.tensor_reduce(out=cnt[:], in_=cmp[:].rearrange("t p e -> t e p"), axis=AX.X,
                                              op=ALU.add), [cmp], [cnt])
        mm(k, pt, pt[:], ones, ones[:], cnt, cnt[:], True, True)
        k.op("dve", lambda e: e.tensor_scalar(out=ge[:], in0=pt[:], scalar1=float(CAP) - 0.5, scalar2=None,
                                              op0=ALU.is_ge), [pt], [ge])
        k.op("dve", lambda e: e.tensor_tensor(out=d1[:], in0=mid[:], in1=lo[:], op=ALU.subtract), [mid, lo], [d1])
        k.op("dve", lambda e: e.tensor_tensor(out=d1[:], in0=d1[:], in1=ge[:], op=ALU.mult), [d1, ge], [d1])
        k.op("dve", lambda e: e.tensor_tensor(out=lo[:], in0=lo[:], in1=d1[:], op=ALU.add), [lo, d1], [lo])
        k.op("dve", lambda e: e.tensor_tensor(out=d1[:], in0=hi[:], in1=mid[:], op=ALU.subtract), [hi, mid], [d1])
        k.op("dve", lambda e: e.tensor_tensor(out=d1[:], in0=d1[:], in1=ge[:], op=ALU.mult), [d1, ge], [d1])
        k.op("dve", lambda e: e.tensor_tensor(out=hi[:], in0=mid[:], in1=d1[:], op=ALU.add), [mid, d1], [hi])
    msk = k.sb("msk", [128, NEXP, 128], F32)
    scn = k.sb("scn", [128, NEXP, 128], F32)
    scb = k.sb("scb", [128, NEXP, 129], BF16)
    k.op("dve", lambda e: e.tensor_tensor(out=msk[:].rearrange("t e p -> t p e"), in0=aff[:],
                                          in1=lo[:].unsqueeze(1).to_broadcast([128, 128, NEXP]), op=ALU.is_gt),
         [aff, lo], [msk])
    k.op("dve", lambda e: e.tensor_tensor_scan(out=scn[:].rearrange("t e p -> t (e p)"),
                                               data0=T.rst16[:], data1=msk[:].rearrange("t e p -> t (e p)"),
                                               initial=0.0, op0=ALU.mult, op1=ALU.add), [T.rst16, msk], [scn])
    k.op("act", lambda e: e.copy(out=scb[:, :, 0:128], in_=scn[:]), [scn], [scb])
    k.op("act", lambda e: e.copy(out=scb[:, :, 128], in_=T.tcol[:, 0:1].to_broadcast([128, NEXP])), [T.tcol], [scb])
    totb = k.sb("totb", [128, NEXP], BF16)
    tot = k.sb("tot", [128, NEXP], F32)
    k.op("dve", lambda e: e.tensor_copy(out=totb[:], in_=scn[:, :, 127]), [scn], [totb])
    k.op("dve", lambda e: e.tensor_copy(out=tot[:], in_=scn[:, :, 127]), [scn], [tot])
    pof = k.ps("pof", [128, NEXP], F32)
    mm(k, pof, pof[:], T.U_bf, T.U_bf[:], totb, totb[:], True, True)
    offs = k.sb("offs", [128, NEXP], F32)
    incl = k.sb("incl", [128, NEXP], F32)
    k.op("dve", lambda e: e.tensor_copy(out=offs[:], in_=pof[:]), [pof], [offs])
    k.op("dve", lambda e: e.tensor_tensor(out=incl[:], in0=offs[:], in1=tot[:], op=ALU.add), [offs, tot], [incl])
    t1p = Pool(k, "rt1", [128, CAP], F32, 2)
    ohp = Pool(k, "oh", [128, CAP], BF16, 2)
    wp = Pool(k, "wrk", [128, CAP], BF16, 2)
    pa = Pool(k, "pa", [128, 129], F32, 2, "ps")
    pr = Pool(k, "pr", [128, 1], F32, 2, "ps")
    rcp = Pool(k, "rcol", [128, 1], F32, 4)
    fnp = Pool(k, "fine", [128, 1], F32, 4)
    jk = k.sb("jk", [128, 128], F32)
    idxf = k.sb("idxf", [128, NEXP, 16], F32)
    for ex in range(NEXP):
        t1 = t1p.next(); oh = ohp.next(); w_ = wp.next(); t2 = t1p.next()
        k.op("dve", lambda e: e.tensor_scalar(out=t1[:], in0=T.iota[:], scalar1=offs[:, ex:ex + 1], scalar2=None,
                                              op0=ALU.is_ge), [T.iota, offs], [t1])
        k.op("dve", lambda e: e.scalar_tensor_tensor(out=oh[:], in0=T.iota[:], scalar=incl[:, ex:ex + 1], in1=t1[:],
                                                     op0=ALU.is_lt, op1=ALU.mult), [T.iota, incl, t1], [oh])
        k.op("dve", lambda e: e.tensor_scalar(out=t2[:], in0=T.iota[:], scalar1=offs[:, ex:ex + 1], scalar2=None,
                                              op0=ALU.subtract), [T.iota, offs], [t2])
        k.op("dve", lambda e: e.tensor_tensor(out=w_[:], in0=t2[:], in1=oh[:], op=ALU.mult), [t2, oh], [w_])
        for kq in range(16):
            pA = pa.next(); pR = pr.next()
            mm(k, pA, pA[:], oh, oh[:, kq * 128:(kq + 1) * 128], scb, scb[:, ex, :], True, True)
            mm(k, pR, pR[:], w_, w_[:, kq * 128:(kq + 1) * 128], T.tcol, T.tcol[:, 1:2], True, True)
            rc = rcp.next(); fn = fnp.next()
            k.op("act", lambda e: e.copy(out=rc[:], in_=pR[:]), [pR], [rc])
            k.op("dve", lambda e: e.tensor_scalar(out=jk[:], in0=pA[:, 0:128], scalar1=rc[:], scalar2=0.0,
                                                  op0=ALU.is_le, op1=ALU.add, accum_out=fn[:]), [pA, rc], [jk, fn])
            k.op("dve", lambda e: e.scalar_tensor_tensor(out=idxf[:, ex, kq:kq + 1], in0=pA[:, 128:129], scalar=128.0,
                                                         in1=fn[:], op0=ALU.mult, op1=ALU.add), [pA, fn], [idxf])
    k.op("dve", lambda e: e.tensor_copy(out=idx_all[:], in_=idxf[:]), [idxf], [idx_all])
    k.pop()


def phase_ffn(k, T, C, l, idx_all, acc, acc_ap):
    k.push()
    wg = k.sb("wg", [128, 8, FF], BF16)
    wu = k.sb("wu", [128, 8, FF], BF16)
    wd = k.sb("wd", [128, 16, 1024], BF16)
    stg = Pool(k, "stgd", [128, 1024], F32, 2)
    xep = Pool(k, "xe", [128, 1024], BF16, 3)
    twp = Pool(k, "tw", [128, NEXP], F32, 8)
    hTp = Pool(k, "xeT", [128, 8, 512], BF16, 2)
    hid = k.sb("hid", [128, 16, 512], BF16)
    sgp = Pool(k, "sgf", [128, 512], F32, 2)
    yop = Pool(k, "yo", [128, 1024], F32, 3)
    pgu = Pool(k, "pgu", [128, 512], F32, 4, "ps")
    pdn = Pool(k, "pdn", [128, 512], F32, 2, "ps")
    ptx = Pool(k, "ptx", [128, 1024], BF16, 2, "ps")
    for ex in range(NEXP):
        for kc in range(8):
            k.load("pool", wg, wg[:, kc, :], T.w_exp_gate, T.w_exp_gate.ap()[l, ex, kc * 128:(kc + 1) * 128, :])
            k.load("pool", wu, wu[:, kc, :], T.w_exp_up, T.w_exp_up.ap()[l, ex, kc * 128:(kc + 1) * 128, :])
        for fc in range(16):
            s_ = stg.next()
            k.load("sp", s_, s_[:], T.w_exp_down, T.w_exp_down.ap()[l, ex, fc * 128:(fc + 1) * 128, :])
            k.op("dve", lambda e: e.tensor_tensor(out=wd[:, fc, :], in0=s_[:], in1=C.g2[:], op=ALU.mult),
                 [s_, C.g2], [wd])
        for B in range(CAP // 512):
            hT = hTp.next()
            tws = []
            for t in range(4):
                kq = B * 4 + t
                xe = xep.next(); tw = twp.next()
                ix = idx_all[:, ex, kq:kq + 1]
                k.dma("pool", lambda e: e.indirect_dma_start(out=xe[:], out_offset=None, in_=T.h2b_d.ap(),
                                                              in_offset=bass.IndirectOffsetOnAxis(ap=ix, axis=0)),
                      xe, reads=[T.h2b_d, idx_all], writes=[xe])
                k.dma("pool", lambda e: e.indirect_dma_start(out=tw[:], out_offset=None, in_=T.aff_d.ap(),
                                                              in_offset=bass.IndirectOffsetOnAxis(ap=ix, axis=0)),
                      tw, reads=[T.aff_d, idx_all], writes=[tw])
                tws.append(tw)
                px = ptx.next()
                for j in range(8):
                    tp(k, px, px[:, j * 128:(j + 1) * 128], xe, xe[:, j * 128:(j + 1) * 128], T.ident_bf)
                k.op("act", lambda e: e.copy(out=hT[:, :, t * 128:(t + 1) * 128],
                                             in_=px[:].rearrange("p (j q) -> p j q", q=128)), [px], [hT])
            for fc in range(16):
                pg = pgu.next()
                for kc in range(8):
                    mm(k, pg, pg[:], wg, wg[:, kc, fc * 128:(fc + 1) * 128], hT, hT[:, kc, :], kc == 0, kc == 7)
                pu = pgu.next()
                for kc in range(8):
                    mm(k, pu, pu[:], wu, wu[:, kc, fc * 128:(fc + 1) * 128], hT, hT[:, kc, :], kc == 0, kc == 7)
                sg = sgp.next()
                k.op("act", lambda e: e.activation(out=sg[:], in_=pg[:], func=AF.Silu), [pg], [sg])
                k.op("dve", lambda e: e.tensor_tensor(out=hid[:, fc, :], in0=sg[:], in1=pu[:], op=ALU.mult),
                     [sg, pu], [hid])
            for t in range(4):
                kq = B * 4 + t
                yo = yop.next()
                for hf in range(2):
                    pd = pdn.next()
                    for fc in range(16):
                        mm(k, pd, pd[:], hid, hid[:, fc, t * 128:(t + 1) * 128], wd, wd[:, fc, hf * 512:(hf + 1) * 512],
                           fc == 0, fc == 15)
                    k.op("act", lambda e: e.activation(out=yo[:, hf * 512:(hf + 1) * 512], in_=pd[:], func=AF.Copy,
                                                       scale=tws[t][:, ex:ex + 1]), [pd, tws[t]], [yo])
                ix = idx_all[:, ex, kq:kq + 1]
                k.dma("pool", lambda e: e.indirect_dma_start(out=acc_ap, out_offset=bass.IndirectOffsetOnAxis(ap=ix, axis=0),
                                                              in_=yo[:], in_offset=None, compute_op=ALU.add),
                      yo, reads=[yo, idx_all], writes=[acc])
    k.pop()


def build(stop_after=99, dbg=(), nlayers=2, skip=()):
    nc = bass.Bass("TRN2", target_bir_lowering=False)
    k = K(nc)
    T = _Dummy()

    def inp(name, shape, dt=F32):
        setattr(T, name, k.dram(name, shape, dt, kind="ExternalInput"))

    inp("x", [1, S, D]); inp("c", [1, D]); inp("ada_w", [2, D, 6 * D]); inp("ada_b", [2, 6 * D])
    inp("norm_mix_w", [2, D]); inp("norm_ffn_w", [2, D]); inp("w_in", [2, D, INW])
    inp("q_norm_w", [2, 64]); inp("k_norm_w", [2, 64]); inp("hg_lower_bounds", [2, 2, 512])
    inp("hg_norm_w", [2, 128]); inp("w_branch_att", [2, 512, D]); inp("w_branch_hg", [2, 512, D])
    inp("w_out", [2, D, D]); inp("w_router", [2, D, NEXP])
    if stop_after >= 7:
        inp("w_exp_gate", [2, NEXP, D, FF]); inp("w_exp_up", [2, NEXP, D, FF]); inp("w_exp_down", [2, NEXP, FF, D])
    inp("cos64", [S, 64]); inp("sin64", [S, 64])
    inp("c_ident_bf", [128, 128], BF16); inp("c_ident_f", [128, 128]); inp("c_mask_f", [128, 128], BF16)
    inp("c_mask_b", [128, 128], BF16); inp("c_rst", [128, 128]); inp("c_cmask", [128, 4, 128], BF16)
    inp("c_iota", [128, CAP]); inp("c_rst16", [128, CAP]); inp("c_U_bf", [128, 128], BF16); inp("c_tcol", [128, 2], BF16)
    T.y = k.dram("y", [S, D], F32, kind="ExternalOutput")

    def scr(name, shape, dt):
        setattr(T, name, k.dram(name, shape, dt, kind="ExternalOutput" if name in dbg else "Internal"))

    scr("modd", [2, 6 * D], F32)
    scr("qT_d", [128, 4, S], BF16); scr("kT_d", [128, S], BF16); scr("va_d", [S, 256], BF16)
    scr("vh_d", [S, 512], BF16); scr("sg_d", [S, 512], BF16); scr("gl_d", [S, 2048], F32)
    scr("hq_d", [4, 128, S], BF16); scr("g_d", [2, 4, 128, S], F32); scr("kk_d", [2, 4, 128, S], F32)
    scr("oatt_d", [S, 512], BF16); scr("of_d", [S, 512], F32); scr("ob_d", [S, 512], F32)
    scr("xs1", [S, D], F32); scr("xs2", [S, D], F32); scr("h2T_d", [128, 8, S], BF16)
    scr("aff_d", [S, NEXP], F32); scr("h2b_d", [S, D], BF16)
    for nm, dt in (("ident_bf", BF16), ("ident_f", F32), ("mask_f", BF16), ("mask_b", BF16), ("rst", F32)):
        t = k.sb(nm, [128, 128], dt)
        src = getattr(T, "c_" + nm)
        k.load("sp", t, t[:], src, src.ap())
        setattr(T, nm, t)
    T.cmask = k.sb("cmask", [128, 4, 128], BF16)
    k.load("sp", T.cmask, T.cmask[:], T.c_cmask, T.c_cmask.ap())
    T.ident65 = k.sb("ident65", [65, 65], BF16)
    k.load("sp", T.ident65, T.ident65[:], T.c_ident_bf, T.c_ident_bf.ap()[0:65, 0:65])
    phase_mod(k, T)
    if stop_after >= 1:
        for l in range(nlayers):
            k.push()
            C = layer_consts(k, T, l)
            idx_all = k.sb("idx_all", [128, NEXP, 16], I32)
            xsrc, xap = (T.x, T.x.ap()[0]) if l == 0 else (T.xs2, T.xs2.ap())
            acc, aap = (T.xs2, T.xs2.ap()) if l == 0 else (T.y, T.y.ap())
            if 2 not in skip:
                phase_proj(k, T, C, l, xsrc, xap)
            if stop_after >= 3 and 3 not in skip:
                phase_attn(k, T)
            if stop_after >= 4 and 4 not in skip:
                phase_hgrn(k, T)
            if stop_after >= 5 and 5 not in skip:
                phase_merge(k, T, C, l, xsrc, xap, acc, aap)
            if stop_after >= 6:
                phase_route(k, T, idx_all)
            if stop_after >= 7:
                phase_ffn(k, T, C, l, idx_all, acc, aap)
            k.pop()
            if stop_after < 7:
                break
    k.barrier()
    k.close()
    return nc


def host_consts():
    bf = ml_dtypes.bfloat16
    t = np.arange(S)
    inv = (10000.0 ** (-np.arange(0, 32, 2, dtype=np.float32) / 32)).astype(np.float32)
    ang_r = ((t // 64).astype(np.float32)[:, None] * inv).astype(np.float32)
    ang_c = ((t % 64).astype(np.float32)[:, None] * inv).astype(np.float32)
    cr, sr, cc, sc = np.cos(ang_r), np.sin(ang_r), np.cos(ang_c), np.sin(ang_c)
    cos64 = np.concatenate([cr, cr, cc, cc], 1).astype(np.float32)
    sin64 = np.concatenate([-sr, sr, -sc, sc], 1).astype(np.float32)
    j = np.arange(128)[:, None]
    i = np.arange(128)[None, :]
    same = (j // 32) == (i // 32)
    return {
        "cos64": cos64, "sin64": sin64,
        "c_ident_bf": np.eye(128, dtype=np.float32).astype(bf), "c_ident_f": np.eye(128, dtype=np.float32),
        "c_mask_f": (same & (j <= i)).astype(np.float32).astype(bf),
        "c_mask_b": (same & (j >= i)).astype(np.float32).astype(bf),
        "c_cmask": np.broadcast_to(((np.arange(128)[None, :] // 32) == np.arange(4)[:, None]).astype(np.float32),
                                   (128, 4, 128)).astype(bf).copy(),
        "c_iota": np.broadcast_to(np.arange(CAP, dtype=np.float32), (128, CAP)).copy(),
        "c_rst16": np.broadcast_to(((np.arange(CAP) % 128) != 0).astype(np.float32), (128, CAP)).copy(),
        "c_U_bf": (np.arange(128)[:, None] < np.arange(128)[None, :]).astype(np.float32).astype(bf),
        "c_tcol": np.stack([np.arange(128, dtype=np.float32), np.ones(128, np.float32)], 1).astype(bf),
        "c_rst": np.broadcast_to(((np.arange(128) % 32) != 0).astype(np.float32), (128, 128)).copy(),
    }


def kernel(**inputs):
    nc = build()
    m = {kk_: np.ascontiguousarray(np.asarray(v, dtype=np.float32)) for kk_, v in inputs.items()}
    m.update(host_consts())
    res = run_bass_kernel_spmd(nc, [m], core_ids=[0])
    return np.asarray(res.results[0]["y"], dtype=np.float32).reshape(1, S, D)
```

```python
import numpy as np
import ml_dtypes
from concourse.bass_utils import run_bass_kernel_spmd

from contextlib import ExitStack
import concourse.bass as bass
import concourse.mybir as mybir

F32 = mybir.dt.float32
BF16 = mybir.dt.bfloat16
I32 = mybir.dt.int32
U32 = mybir.dt.uint32
AF = mybir.ActivationFunctionType
ALU = mybir.AluOpType
AX = mybir.AxisListType

SEM_ROT = 30000
SAME_ENGINE_SYNC = True


class Tl:
    def __init__(self, k, t, name, space):
        self.k = k
        self.t = t
        self.name = name
        self.space = space
        self.w = {}
        self.r = {}
        self.dsem = {}
        self.dcnt = {}
        k.tiles.append(self)

    def __getitem__(self, idx):
        return self.t[idx]

    def ap(self):
        return self.t.ap() if hasattr(self.t, "ap") else self.t[:]


class K:
    ENG = ("pe", "dve", "act", "pool", "sp")

    def __init__(self, nc):
        self.nc = nc
        self.es = ExitStack()
        self.eng = {"pe": nc.tensor, "dve": nc.vector, "act": nc.scalar,
                    "pool": nc.gpsimd, "sp": nc.sync}
        self.gen = {e: 0 for e in self.ENG}
        self.sem = {}
        self.cnt = {e: 0 for e in self.ENG}
        for e in self.ENG:
            self.sem[(e, 0)] = self.es.enter_context(nc.semaphore(f"s_{e}_0"))
        self.seen = {e: {} for e in self.ENG}
        self.seenD = {e: {} for e in self.ENG}
        self.nsem = len(self.ENG)
        self.uid = 0
        self.tiles = []
        self.stacks = []
        self.dsem_scopes = []
        self.free_dsems = {}

    def push(self):
        self.stacks.append(ExitStack())
        self.dsem_scopes.append([])

    def pop(self):
        self.barrier()
        for t, q in self.dsem_scopes.pop():
            if q in t.dsem:
                self.free_dsems.setdefault(q, []).append((t.dsem.pop(q), t.dcnt.pop(q)))
        for e in self.ENG:
            self.seenD[e] = {}
        self.stacks.pop().close()

    def _st(self):
        return self.stacks[-1] if self.stacks else self.es

    def sb(self, name, shape, dt):
        self.uid += 1
        t = self._st().enter_context(self.nc.sbuf_tensor(f"{name}_{self.uid}", list(shape), dt))
        return Tl(self, t, name, "sb")

    def ps(self, name, shape, dt=F32):
        self.uid += 1
        t = self._st().enter_context(self.nc.psum_tensor(f"{name}_{self.uid}", list(shape), dt))
        return Tl(self, t, name, "ps")

    def dram(self, name, shape, dt, kind="Internal"):
        t = self.nc.dram_tensor(name, list(shape), dt, kind=kind)
        return Tl(self, t, name, "dram")

    def close(self):
        while self.stacks:
            self.stacks.pop().close()
        self.es.close()


    def _wait(self, e, ev):
        if ev is None:
            return
        if ev[0] == "E":
            _, src, gen, c = ev
            if src == e and (e == "pe" or not SAME_ENGINE_SYNC):
                return
            key = (src, gen)
            if self.seen[e].get(key, 0) >= c:
                return
            self.eng[e].wait_ge(self.sem[key], c)
            self.seen[e][key] = c
        else:
            _, tl, q = ev
            if q not in tl.dsem:
                return
            tgt = tl.dcnt[q] * 16
            if self.seenD[e].get((id(tl), q), 0) >= tgt:
                return
            self.eng[e].wait_ge(tl.dsem[q], tgt)
            self.seenD[e][(id(tl), q)] = tgt

    @staticmethod
    def _key(ev):
        return ev[:3] if ev[0] == "E" else ("D", id(ev[1]), ev[2])

    def _deps(self, e, reads, writes, disjoint=False):
        for t in reads:
            for ev in t.w.values():
                self._wait(e, ev)
        if disjoint:
            return
        for t in writes:
            for ev in t.w.values():
                self._wait(e, ev)
            for ev in t.r.values():
                self._wait(e, ev)

    def _commit(self, ev, reads, writes, disjoint=False):
        for t in reads:
            if t in writes:
                continue
            t.r[self._key(ev)] = ev
        for t in writes:
            if disjoint:
                t.w[self._key(ev)] = ev
            else:
                t.w = {self._key(ev): ev}
                t.r = {}

    def barrier(self):
        for t in self.tiles:
            for ev in list(t.w.values()) + list(t.r.values()):
                self._wait("sp", ev)
            for q in list(t.dsem):
                self._wait("sp", ("D", t, q))
        for e in self.ENG:
            if e != "sp" and self.cnt[e] > 0:
                self._wait("sp", ("E", e, self.gen[e], self.cnt[e]))
        self.op("sp", lambda e: e.nop())
        ev = ("E", "sp", self.gen["sp"], self.cnt["sp"])
        for e in self.ENG:
            if e != "sp":
                self._wait(e, ev)
        for t in self.tiles:
            t.w = {}
            t.r = {}

    def op(self, e, fn, reads=(), writes=()):
        self._deps(e, reads, writes)
        if self.cnt[e] >= SEM_ROT:
            self.gen[e] += 1
            self.cnt[e] = 0
            self.sem[(e, self.gen[e])] = self.es.enter_context(
                self.nc.semaphore(f"s_{e}_{self.gen[e]}"))
            self.nsem += 1
        ins = fn(self.eng[e])
        self.cnt[e] += 1
        g = self.gen[e]
        ins.then_inc(self.sem[(e, g)], 1)
        ev = ("E", e, g, self.cnt[e])
        self._commit(ev, reads, writes)
        return ins

    def dma(self, q, fn, sbt, reads=(), writes=(), disjoint=False):
        self._deps(q, reads, writes, disjoint)
        if q not in sbt.dsem:
            self.uid += 1
            if self.free_dsems.get(q):
                sbt.dsem[q], sbt.dcnt[q] = self.free_dsems[q].pop()
            else:
                sbt.dsem[q] = self.es.enter_context(self.nc.semaphore(f"d_{q}_{sbt.name}_{self.uid}"))
                sbt.dcnt[q] = 0
                self.nsem += 1
            if self.dsem_scopes:
                self.dsem_scopes[-1].append((sbt, q))
        ins = fn(self.eng[q])
        ins.then_inc(sbt.dsem[q], 16)
        sbt.dcnt[q] += 1
        ev = ("D", sbt, q)
        self._commit(ev, reads, writes, disjoint)
        return ins

    def load(self, q, dst, dst_ap, src, src_ap, disjoint=False, **kw):
        return self.dma(q, lambda e: e.dma_start(out=dst_ap, in_=src_ap, **kw), dst,
                        reads=[src], writes=[dst], disjoint=disjoint)

    def store(self, q, dst, dst_ap, src, src_ap, disjoint=True, **kw):
        return self.dma(q, lambda e: e.dma_start(out=dst_ap, in_=src_ap, **kw), src,
                        reads=[src], writes=[dst], disjoint=disjoint)

    def finish(self, tiles, e="sp"):
        for t in tiles:
            for ev in list(t.w.values()) + list(t.r.values()):
                self._wait(e, ev)


class Pool:
    def __init__(self, k, name, shape, dt, n, space="sb"):
        mk = k.sb if space == "sb" else k.ps
        self.t = [mk(f"{name}{i}", shape, dt) for i in range(n)]
        self.i = 0

    def next(self):
        t = self.t[self.i % len(self.t)]
        self.i += 1
        return t

class _Dummy:
    pass
    pass

S = 16384
D = 1024
NBLK = S // 512
NTILE = S // 128
INW = 5376
EPS = 1e-6
O_Q, O_K, O_V, O_HQ, O_ZF, O_ZB, O_HI, O_HG, O_GL = 0, 512, 640, 768, 1280, 1792, 2304, 2816, 3328
NEXP = 16
FF = 2048
CAP = 2048


def mm(k, out, oap, lt, lap, rt, rap, start, stop):
    k.op("pe", lambda e: e.matmul(oap, lhsT=lap, rhs=rap, start=start, stop=stop),
         [lt, rt], [out])


def tp(k, out, oap, it, iap, ident):
    k.op("pe", lambda e: e.transpose(oap, iap, ident[:]), [it, ident], [out])


def colload(k, name, src, ap):
    t = k.sb(name, [128, 8], F32)
    k.load("sp", t, t[:], src, ap.rearrange("o (c p) -> p (o c)", p=128),
           allow_slow_non_contiguous=True)
    return t


def rowload(k, name, src, ap, n):
    t = k.sb(name, [128, n], F32)
    k.load("sp", t, t[:], src, ap.partition_broadcast(128))
    return t


def phase_mod(k, T):
    k.push()
    ccol = colload(k, "ccol", T.c, T.c.ap())
    cact = k.sb("cact", [128, 8], F32)
    k.op("act", lambda e: e.activation(out=cact[:], in_=ccol[:], func=AF.Silu), [ccol], [cact])
    wpool = Pool(k, "adaw", [128, 8, 512], F32, 2)
    brow = k.sb("brow", [1, 6144], F32)
    mrow = k.sb("mrow", [1, 6144], F32)
    pp = Pool(k, "pmod", [1, 512], F32, 2, "ps")
    for l in range(2):
        k.load("sp", brow, brow[:], T.ada_b, T.ada_b.ap()[l:l + 1, :])
        for nb in range(12):
            w = wpool.next()
            k.load("sp", w, w[:], T.ada_w,
                   T.ada_w.ap()[l].rearrange("(kc p) n -> p kc n", p=128)[:, :, nb * 512:(nb + 1) * 512])
            ps = pp.next()
            for kc in range(8):
                mm(k, ps, ps[:], cact, cact[:, kc:kc + 1], w, w[:, kc, :], kc == 0, kc == 7)
            k.op("dve", lambda e: e.tensor_tensor(out=mrow[:, nb * 512:(nb + 1) * 512], in0=ps[:],
                                                  in1=brow[:, nb * 512:(nb + 1) * 512], op=ALU.add),
                 [ps, brow], [mrow])
        k.store("sp", T.modd, T.modd.ap()[l:l + 1, :], mrow, mrow[:], disjoint=False)
    k.pop()


def layer_consts(k, T, l):
    C = _Dummy()
    md = T.modd.ap()
    sh1 = colload(k, "sh1", T.modd, md[l:l + 1, 0:1024])
    sc1 = colload(k, "sc1", T.modd, md[l:l + 1, 1024:2048])
    nw1 = colload(k, "nw1", T.norm_mix_w, T.norm_mix_w.ap()[l:l + 1, :])
    C.a1 = k.sb("a1", [128, 8], F32)
    C.b1 = sh1
    k.op("dve", lambda e: e.scalar_tensor_tensor(out=C.a1[:], in0=sc1[:], scalar=1.0, in1=nw1[:],
                                                 op0=ALU.add, op1=ALU.mult), [sc1, nw1], [C.a1])
    C.a2 = k.sb("a2", [128, 1024], F32)
    k.push()
    sc2 = rowload(k, "sc2", T.modd, md[l:l + 1, 4096:5120], 1024)
    nw2 = rowload(k, "nw2", T.norm_ffn_w, T.norm_ffn_w.ap()[l:l + 1, :], 1024)
    k.op("dve", lambda e: e.scalar_tensor_tensor(out=C.a2[:], in0=sc2[:], scalar=1.0, in1=nw2[:],
                                                 op0=ALU.add, op1=ALU.mult), [sc2, nw2], [C.a2])
    k.pop()
    C.b2 = rowload(k, "sh2", T.modd, md[l:l + 1, 3072:4096], 1024)
    C.g1 = rowload(k, "g1", T.modd, md[l:l + 1, 2048:3072], 1024)
    C.g2 = rowload(k, "g2", T.modd, md[l:l + 1, 5120:6144], 1024)
    C.qkw = k.sb("qkw", [128, 10, 64], F32)
    for h in range(10):
        src = T.q_norm_w if h < 8 else T.k_norm_w
        k.load("sp", C.qkw, C.qkw[:, h, :], src, src.ap()[l:l + 1, :].partition_broadcast(128))
    C.hgw = k.sb("hgw", [128, 4, 128], F32)
    for h in range(4):
        k.load("sp", C.hgw, C.hgw[:, h, :], T.hg_norm_w, T.hg_norm_w.ap()[l:l + 1, :].partition_broadcast(128))
    C.lb = k.sb("lb", [128, 8], F32)
    C.oml = k.sb("oml", [128, 8], F32)
    C.noml = k.sb("noml", [128, 8], F32)
    if l == 0:
        k.op("dve", lambda e: e.memset(C.lb[:], 0.0), [], [C.lb])
    else:
        a0 = k.sb("lba0", [128, 8], F32)
        a1 = k.sb("lba1", [128, 8], F32)
        hb = T.hg_lower_bounds.ap()
        k.load("sp", a0, a0[:], T.hg_lower_bounds, hb[0].rearrange("r (h d) -> d (r h)", d=128),
               allow_slow_non_contiguous=True)
        k.load("sp", a1, a1[:], T.hg_lower_bounds, hb[1].rearrange("r (h d) -> d (r h)", d=128),
               allow_slow_non_contiguous=True)
        k.op("dve", lambda e: e.tensor_tensor(out=a0[:], in0=a1[:], in1=a0[:], op=ALU.subtract), [a0, a1], [a0])
        k.op("act", lambda e: e.activation(out=C.lb[:], in_=a0[:], func=AF.Sigmoid), [a0], [C.lb])
    k.op("dve", lambda e: e.tensor_scalar(out=C.oml[:], in0=C.lb[:], scalar1=-1.0, scalar2=1.0,
                                          op0=ALU.mult, op1=ALU.add), [C.lb], [C.oml])
    k.op("dve", lambda e: e.tensor_scalar(out=C.noml[:], in0=C.oml[:], scalar1=-1.0, scalar2=None,
                                          op0=ALU.mult), [C.oml], [C.noml])
    return C


def phase_proj(k, T, C, l, xsrc, xsrc_ap):
    k.push()
    win = k.sb("win", [128, 8, INW], BF16)
    for kc in range(8):
        for cb in range(3):
            k.load("pool", win, win[:, kc, cb * 1792:(cb + 1) * 1792], T.w_in,
                   T.w_in.ap()[l, kc * 128:(kc + 1) * 128, cb * 1792:(cb + 1) * 1792], disjoint=True)
    xp = Pool(k, "xt", [128, 4, 1024], F32, 1)
    xnp = Pool(k, "xn", [128, 4, 1024], BF16, 1)
    hTp = Pool(k, "hT", [128, 8, 512], BF16, 1)
    junk = k.sb("junk", [128, 1024], BF16)
    ssp = Pool(k, "ss", [128, 4], F32, 2)
    rsp = Pool(k, "rs", [128, 4], F32, 2)
    pb = Pool(k, "pb", [128, 512], F32, 6, "ps")
    ptr = Pool(k, "ptr", [128, 1024], BF16, 2, "ps")
    qkp = Pool(k, "qk", [128, 10, 64], F32, 2)
    sqp = Pool(k, "sq", [128, 10, 64], F32, 2)
    t1p = Pool(k, "t1", [128, 10, 64], F32, 2)
    s10p = Pool(k, "s10", [128, 10], F32, 2)
    csp = Pool(k, "cs", [128, 2, 64], F32, 2)
    qbp = Pool(k, "qb", [128, 5, 128], BF16, 2)
    qTp = Pool(k, "qTs", [128, 5, 512], BF16, 1)
    vtp = Pool(k, "vt", [128, 4, 256], BF16, 2)
    hip = Pool(k, "hi", [128, 4, 512], BF16, 1)
    sgp = Pool(k, "sg", [128, 4, 512], BF16, 1)
    glp = Pool(k, "gl", [128, 2048], F32, 1)
    fmp = Pool(k, "fm", [128, 512], BF16, 3)
    f32p = Pool(k, "f32", [128, 512], F32, 4)
    sigp = Pool(k, "sig", [128, 512], F32, 2)
    for B in range(NBLK):
        r0 = B * 512
        xt = xp.next()
        k.load("sp", xt, xt[:], xsrc, xsrc_ap[r0:r0 + 512, :].rearrange("(t p) d -> p t d", p=128))
        ss = ssp.next()
        rs = rsp.next()
        xn = xnp.next()
        for t in range(4):
            k.op("act", lambda e: e.activation(out=junk[:], in_=xt[:, t, :], func=AF.Square,
                                               accum_out=ss[:, t:t + 1]), [xt], [junk, ss])
        k.op("act", lambda e: e.activation(out=rs[:], in_=ss[:], func=AF.Sqrt, scale=1.0 / D, bias=EPS),
             [ss], [rs])
        k.op("dve", lambda e: e.reciprocal(out=rs[:], in_=rs[:]), [rs], [rs])
        for t in range(4):
            k.op("act", lambda e: e.activation(out=xn[:, t, :], in_=xt[:, t, :], func=AF.Copy,
                                               scale=rs[:, t:t + 1]), [xt, rs], [xn])
        hT = hTp.next()
        for kc in range(8):
            p = ptr.next()
            for t in range(4):
                tp(k, p, p[:, t * 128:(t + 1) * 128], xn, xn[:, t, kc * 128:(kc + 1) * 128], T.ident_bf)
            k.op("dve", lambda e: e.tensor_scalar(out=hT[:, kc, :], in0=p[:, 0:512],
                                                  scalar1=C.a1[:, kc:kc + 1], scalar2=C.b1[:, kc:kc + 1],
                                                  op0=ALU.mult, op1=ALU.add), [p, C.a1, C.b1], [hT])

        def proj_tm(ps, t, c0, n):
            for kc in range(8):
                mm(k, ps, ps[:, 0:n], hT, hT[:, kc, t * 128:(t + 1) * 128], win, win[:, kc, c0:c0 + n],
                   kc == 0, kc == 7)

        def proj_fm(ps, c0):
            for kc in range(8):
                mm(k, ps, ps[:], win, win[:, kc, c0:c0 + 128], hT, hT[:, kc, :], kc == 0, kc == 7)

        qT = qTp.next()
        vt = vtp.next()
        k.op("pool", lambda e: e.memset(vt[:], 0.0), [], [vt])
        k.op("pool", lambda e: e.memset(vt[:, :, 64:65], 1.0), [], [vt])
        k.op("pool", lambda e: e.memset(vt[:, :, 192:193], 1.0), [], [vt])
        hi = hip.next()
        sg = sgp.next()
        for t in range(4):
            tr0 = r0 + t * 128
            cs = csp.next()
            k.load("sp", cs, cs[:, 0, :], T.cos64, T.cos64.ap()[tr0:tr0 + 128, :])
            k.load("sp", cs, cs[:, 1, :], T.sin64, T.sin64.ap()[tr0:tr0 + 128, :])
            ps1 = pb.next()
            proj_tm(ps1, t, O_Q, 512)
            ps2 = pb.next()
            proj_tm(ps2, t, O_K, 256)
            qk = qkp.next()
            k.op("act", lambda e: e.copy(out=qk[:, 0:8, :], in_=ps1[:].rearrange("p (h d) -> p h d", d=64)),
                 [ps1], [qk])
            k.op("act", lambda e: e.copy(out=qk[:, 8:10, :], in_=ps2[:, 0:128].rearrange("p (h d) -> p h d", d=64)),
                 [ps2], [qk])
            k.op("act", lambda e: e.copy(out=vt[:, t, :].rearrange("p (g d) -> p g d", d=128)[:, :, 0:64],
                                         in_=ps2[:, 128:256].rearrange("p (g d) -> p g d", d=64)), [ps2], [vt])
            sq = sqp.next()
            s10 = s10p.next()
            k.op("dve", lambda e: e.tensor_tensor(out=sq[:], in0=qk[:], in1=qk[:], op=ALU.mult), [qk], [sq])
            k.op("dve", lambda e: e.tensor_reduce(out=s10[:], in_=sq[:], axis=AX.X, op=ALU.add), [sq], [s10])
            k.op("act", lambda e: e.activation(out=s10[:], in_=s10[:], func=AF.Sqrt, scale=1.0 / 64, bias=EPS),
                 [s10], [s10])
            k.op("dve", lambda e: e.reciprocal(out=s10[:], in_=s10[:]), [s10], [s10])
            k.op("dve", lambda e: e.tensor_tensor(out=qk[:], in0=qk[:],
                                                  in1=s10[:].unsqueeze(2).to_broadcast([128, 10, 64]),
                                                  op=ALU.mult), [qk, s10], [qk])
            k.op("pool", lambda e: e.tensor_tensor(out=qk[:], in0=qk[:], in1=C.qkw[:], op=ALU.mult),
                 [qk, C.qkw], [qk])
            t1 = t1p.next()
            k.op("pool", lambda e: e.tensor_tensor(out=t1[:], in0=qk[:],
                                                   in1=cs[:, 0:1, :].to_broadcast([128, 10, 64]), op=ALU.mult),
                 [qk, cs], [t1])
            qv = qk[:].rearrange("p h (a f j) -> p h a f j", a=2, f=2)
            sv = cs[:, 1, :].rearrange("p (a f j) -> p a f j", a=2, f=2)
            sqv = sq[:].rearrange("p h (a f j) -> p h a f j", a=2, f=2)
            for f in range(2):
                k.op("dve", lambda e: e.tensor_tensor(
                    out=sqv[:, :, :, f, :], in0=qv[:, :, :, 1 - f, :],
                    in1=sv[:, :, f, :].unsqueeze(1).to_broadcast([128, 10, 2, 16]), op=ALU.mult),
                    [qk, cs], [sq])
            qb = qbp.next()
            k.op("dve", lambda e: e.tensor_tensor(
                out=qb[:, 0:4, :].rearrange("p j (g d) -> p g j d", g=2),
                in0=t1[:, 0:8, :].rearrange("p (g j) d -> p g j d", g=2),
                in1=sq[:, 0:8, :].rearrange("p (g j) d -> p g j d", g=2), op=ALU.add), [t1, sq], [qb])
            k.op("pool", lambda e: e.tensor_tensor(
                out=qb[:, 4, :].rearrange("p (g d) -> p g d", g=2),
                in0=t1[:, 8:10, :], in1=sq[:, 8:10, :], op=ALU.add), [t1, sq], [qb])
            p = ptr.next()
            for j in range(5):
                tp(k, p, p[:, j * 128:(j + 1) * 128], qb, qb[:, j, :], T.ident_bf)
            k.op("act", lambda e: e.copy(out=qT[:, :, t * 128:(t + 1) * 128],
                                         in_=p[:, 0:640].rearrange("p (j q) -> p j q", q=128)), [p], [qT])
            ps = pb.next()
            proj_tm(ps, t, O_HI, 512)
            k.op("dve", lambda e: e.tensor_copy(out=hi[:, t, :], in_=ps[:]), [ps], [hi])
            ps = pb.next()
            proj_tm(ps, t, O_HG, 512)
            k.op("act", lambda e: e.activation(out=sg[:, t, :], in_=ps[:], func=AF.Silu), [ps], [sg])
            gl = glp.next()
            for c in range(4):
                ps = pb.next()
                proj_tm(ps, t, O_GL + c * 512, 512)
                k.op("act", lambda e: e.activation(out=gl[:, c * 512:(c + 1) * 512], in_=ps[:], func=AF.Sigmoid),
                     [ps], [gl])
            k.store("pool", T.gl_d, T.gl_d.ap()[tr0:tr0 + 128, :], gl, gl[:])
        k.store("pool", T.qT_d, T.qT_d.ap()[:, :, r0:r0 + 512], qT, qT[:, 0:4, :])
        k.store("pool", T.kT_d, T.kT_d.ap()[:, r0:r0 + 512], qT, qT[:, 4, :])
        k.store("pool", T.va_d, T.va_d.ap()[r0:r0 + 512, :].rearrange("(t p) c -> p t c", p=128), vt, vt[:])
        k.store("pool", T.vh_d, T.vh_d.ap()[r0:r0 + 512, :].rearrange("(t p) c -> p t c", p=128), hi, hi[:])
        k.store("pool", T.sg_d, T.sg_d.ap()[r0:r0 + 512, :].rearrange("(t p) c -> p t c", p=128), sg, sg[:])
        for h in range(4):
            ps = pb.next()
            proj_fm(ps, O_HQ + h * 128)
            o = fmp.next()
            k.op("act", lambda e: e.activation(out=o[:], in_=ps[:], func=AF.Silu), [ps], [o])
            k.store("pool", T.hq_d, T.hq_d.ap()[h, :, r0:r0 + 512], o, o[:])
        for dr in range(2):
            for h in range(4):
                ci = dr * 4 + h
                ps = pb.next()
                proj_fm(ps, (O_ZF if dr == 0 else O_ZB) + h * 128)
                sig = sigp.next()
                k.op("act", lambda e: e.activation(out=sig[:], in_=ps[:], func=AF.Sigmoid), [ps], [sig])
                f = f32p.next()
                k.op("dve", lambda e: e.tensor_scalar(out=f[:], in0=sig[:], scalar1=C.oml[:, ci:ci + 1],
                                                      scalar2=C.lb[:, ci:ci + 1], op0=ALU.mult, op1=ALU.add),
                     [sig, C.oml, C.lb], [f])
                k.op("pool", lambda e: e.tensor_scalar(out=f[:], in0=f[:], scalar1=1e-6, scalar2=None,
                                                       op0=ALU.max), [f], [f])
                k.op("act", lambda e: e.activation(out=f[:], in_=f[:], func=AF.Ln), [f], [f])
                k.store("pool", T.g_d, T.g_d.ap()[dr, h, :, r0:r0 + 512], f, f[:])
                kk = f32p.next()
                k.op("dve", lambda e: e.tensor_scalar(out=kk[:], in0=sig[:], scalar1=C.noml[:, ci:ci + 1],
                                                      scalar2=C.oml[:, ci:ci + 1], op0=ALU.mult, op1=ALU.add),
                     [sig, C.noml, C.oml], [kk])
                k.store("pool", T.kk_d, T.kk_d.ap()[dr, h, :, r0:r0 + 512], kk, kk[:])
    k.pop()


def phase_attn(k, T):
    k.push()
    kT = k.sb("kT", [128, S], BF16)
    va = k.sb("va", [128, NTILE, 256], BF16)
    for i in range(4):
        k.load("sp", kT, kT[:, i * 4096:(i + 1) * 4096], T.kT_d, T.kT_d.ap()[:, i * 4096:(i + 1) * 4096],
               disjoint=True)
        k.load("sp", va, va[:, i * 32:(i + 1) * 32, :], T.va_d,
               T.va_d.ap()[i * 4096:(i + 1) * 4096, :].rearrange("(t p) c -> p t c", p=128), disjoint=True)
    qz = [Pool(k, f"qz{g}", [128, 4, 512], BF16, 2) for g in range(2)]
    for g in range(2):
        for t_ in qz[g].t:
            k.op("pool", lambda e: e.memset(t_[:], 0.0), [], [t_])
    psc = Pool(k, "psc", [128, 512], F32, 3, "ps")
    pac = Pool(k, "pac", [128, 512], F32, 4, "ps")
    ppx = k.ps("ppx", [128, 8, 128], BF16)
    ptp = Pool(k, "pT", [128, 512], BF16, 4)
    ohp = Pool(k, "ohi", [65, 512], BF16, 2)
    olp = Pool(k, "olo", [65, 512], BF16, 2)
    otp = Pool(k, "otm", [128, 4, 65], F32, 2)
    rcp = Pool(k, "rc", [128, 4], F32, 2)
    oap = Pool(k, "oa", [128, 4, 512], BF16, 2)
    for B in range(NBLK):
        r0 = B * 512
        qzb = [qz[g].next() for g in range(2)]
        for g in range(2):
            k.load("sp", qzb[g], qzb[g][64 * g:64 * g + 64, :, :], T.qT_d, T.qT_d.ap()[64 * g:64 * g + 64, :, r0:r0 + 512])
        oa = oap.next()
        for g in range(2):
            acc = [pac.next() for _ in range(4)]
            items = [(kc, j) for kc in range(NTILE) for j in range(4)]
            LA = 2
            inflight = {}
            for n in range(len(items) + LA):
                if n < len(items):
                    kc, j = items[n]
                    ps = psc.next()
                    mm(k, ps, ps[:], kT, kT[:, kc * 128:(kc + 1) * 128], qzb[g], qzb[g][:, j, :], True, True)
                    inflight[n] = ps
                m = n - LA
                if m >= 0:
                    kc, j = items[m]
                    ps = inflight.pop(m)
                    pT = ptp.next()
                    k.op("act", lambda e: e.activation(out=pT[:], in_=ps[:], func=AF.Exp, scale=0.125), [ps], [pT])
                    mm(k, acc[j], acc[j][:], va, va[:, kc, g * 128:(g + 1) * 128], pT, pT[:],
                       kc == 0, kc == NTILE - 1)
            for j in range(4):
                h = g * 4 + j
                ohi = ohp.next(); olo = olp.next()
                k.op("act", lambda e: e.copy(out=ohi[:], in_=acc[j][0:65, :]), [acc[j]], [ohi])
                k.op("dve", lambda e: e.tensor_tensor(out=olo[:], in0=acc[j][0:65, :], in1=ohi[:], op=ALU.subtract),
                     [acc[j], ohi], [olo])
                for t in range(4):
                    tp(k, ppx, ppx[:, t, 0:65], ohi, ohi[:, t * 128:(t + 1) * 128], T.ident65)
                    tp(k, ppx, ppx[:, 4 + t, 0:65], olo, olo[:, t * 128:(t + 1) * 128], T.ident65)
                otm = otp.next()
                k.op("act", lambda e: e.copy(out=otm[:], in_=ppx[:, 0:4, 0:65]), [ppx], [otm])
                k.op("dve", lambda e: e.tensor_tensor(out=otm[:], in0=otm[:], in1=ppx[:, 4:8, 0:65], op=ALU.add),
                     [otm, ppx], [otm])
                rc = rcp.next()
                k.op("dve", lambda e: e.reciprocal(out=rc[:], in_=otm[:, :, 64]), [otm], [rc])
                k.op("dve", lambda e: e.tensor_tensor(out=oa[:, :, h * 64:(h + 1) * 64], in0=otm[:, :, 0:64],
                                                      in1=rc[:].unsqueeze(2).to_broadcast([128, 4, 64]),
                                                      op=ALU.mult), [otm, rc], [oa])
        k.store("pool", T.oatt_d, T.oatt_d.ap()[r0:r0 + 512, :].rearrange("(t p) c -> p t c", p=128), oa, oa[:])
    k.pop()


def phase_hgrn(k, T):
    k.push()
    HD = [(h, dr) for dr in range(2) for h in range(4)]
    S32 = {hd: k.sb(f"S32_{hd[0]}{hd[1]}", [128, 128], F32) for hd in HD}
    S16 = {hd: k.sb(f"S16_{hd[0]}{hd[1]}", [128, 128], BF16) for hd in HD}
    for hd in HD:
        k.op("pool", lambda e: e.memset(S32[hd][:], 0.0), [], [S32[hd]])
        k.op("pool", lambda e: e.memset(S16[hd][:], 0.0), [], [S16[hd]])
    N = 8
    hqp = Pool(k, "hq", [128, 128], BF16, 2 * N)
    gp = Pool(k, "g", [128, 128], F32, 2 * N)
    kkp = Pool(k, "kk", [128, 128], F32, 2 * N)
    vp = Pool(k, "v", [128, 128], BF16, 2 * N)
    for i_ in range(2 * N):
        for p_ in (gp, kkp, vp):
            p_.t[i_].dsem = hqp.t[i_].dsem
            p_.t[i_].dcnt = hqp.t[i_].dcnt
    bp = Pool(k, "b", [128, 128], F32, N)
    b2p = Pool(k, "b2", [128, 128], F32, N)
    ebp = Pool(k, "eb", [128, 128], F32, N)
    tmp = Pool(k, "tmp", [128, 128], F32, N)
    qtp = Pool(k, "qt", [128, 128], BF16, N)
    ktp = Pool(k, "kt", [128, 128], BF16, N)
    khtp = Pool(k, "kht", [128, 128], BF16, N)
    scp = Pool(k, "sc", [128, 128], BF16, N)
    osp = Pool(k, "os", [128, 128], F32, N)
    po = [k.ps(f"po{i}", [128, 4, 128], F32) for i in range(2)]
    psb = k.ps("psb", [128, 4, 128], F32)
    ptbs = [k.ps(f"ptb{i}", [128, 2, 4, 128], BF16) for i in range(2)]
    pub = [k.ps(f"pu{i}", [128, 4, 128], F32) for i in range(2)]
    qpp = Pool(k, "qpad", [128, 4, 128], BF16, N)
    khpp = Pool(k, "khpad", [128, 4, 128], BF16, N)
    khtpp = Pool(k, "khtpad", [128, 4, 128], BF16, N)
    for step in range(NTILE):
        st = {}
        for i, (h, dr) in enumerate(HD):
            Tt = step if dr == 0 else NTILE - 1 - step
            c0 = Tt * 128
            hq = hqp.next(); g = gp.next(); kk = kkp.next(); v = vp.next()
            k.load("sp", hq, hq[:], T.hq_d, T.hq_d.ap()[h, :, c0:c0 + 128])
            k.load("sp", g, g[:], T.g_d, T.g_d.ap()[dr, h, :, c0:c0 + 128])
            k.load("sp", kk, kk[:], T.kk_d, T.kk_d.ap()[dr, h, :, c0:c0 + 128])
            k.load("sp", v, v[:], T.vh_d, T.vh_d.ap()[c0:c0 + 128, h * 128:(h + 1) * 128])
            b = bp.next()
            k.op("dve", lambda e: e.tensor_tensor_scan(out=b[:], data0=T.rst[:], data1=g[:], initial=0.0,
                                                       op0=ALU.mult, op1=ALU.add), [T.rst, g], [b])
            b3 = b[:].rearrange("p (c i) -> p c i", i=32)
            if dr == 1:
                b2 = b2p.next()
                k.op("pool", lambda e: e.tensor_tensor(out=b2[:].rearrange("p (c i) -> p c i", i=32),
                                                       in0=b3[:, :, 31:32].to_broadcast([128, 4, 32]),
                                                       in1=b3, op=ALU.subtract), [b], [b2])
                k.op("pool", lambda e: e.tensor_tensor(out=b2[:], in0=b2[:], in1=g[:], op=ALU.add), [b2, g], [b2])
                b = b2
                b3 = b[:].rearrange("p (c i) -> p c i", i=32)
            le = 31 if dr == 0 else 0
            eb = ebp.next()
            k.op("act", lambda e: e.activation(out=eb[:], in_=b[:], func=AF.Exp), [b], [eb])
            qt = qtp.next()
            k.op("dve", lambda e: e.tensor_tensor(out=qt[:], in0=hq[:], in1=eb[:], op=ALU.mult), [hq, eb], [qt])
            t1 = tmp.next()
            k.op("act", lambda e: e.activation(out=t1[:], in_=b[:], func=AF.Exp, scale=-1.0), [b], [t1])
            kt = ktp.next()
            k.op("dve", lambda e: e.tensor_tensor(out=kt[:], in0=kk[:], in1=t1[:], op=ALU.mult), [kk, t1], [kt])
            t2 = tmp.next()
            k.op("pool", lambda e: e.tensor_tensor(out=t2[:].rearrange("p (c i) -> p c i", i=32),
                                                   in0=b3[:, :, le:le + 1].to_broadcast([128, 4, 32]),
                                                   in1=b3, op=ALU.subtract), [b], [t2])
            k.op("act", lambda e: e.activation(out=t2[:], in_=t2[:], func=AF.Exp), [t2], [t2])
            kht = khtp.next()
            k.op("pool", lambda e: e.tensor_tensor(out=kht[:], in0=kk[:], in1=t2[:], op=ALU.mult), [kk, t2], [kht])
            khtpad = khtpp.next()
            k.op("pool", lambda e: e.tensor_tensor(out=khtpad[:], in0=kht[:].unsqueeze(1).to_broadcast([128, 4, 128]),
                                                   in1=T.cmask[:], op=ALU.mult), [kht, T.cmask], [khtpad])
            ptb = ptbs[(i // 2) % 2]
            for c in range(4):
                tp(k, ptb, ptb[:, i % 2, c, :], khtpad, khtpad[:, c, :], T.ident_bf)
            kh = khpp.next()
            k.op("act", lambda e: e.copy(out=kh[:], in_=ptb[:, i % 2, :, :]), [ptb], [kh])
            qpad = qpp.next()
            k.op("dve", lambda e: e.tensor_tensor(out=qpad[:], in0=qt[:].unsqueeze(1).to_broadcast([128, 4, 128]),
                                                  in1=T.cmask[:], op=ALU.mult), [qt, T.cmask], [qpad])
            mm(k, psb, psb[:, i % 4, :], kt, kt[:], qt, qt[:], True, True)
            sc = scp.next()
            mk = T.mask_f if dr == 0 else T.mask_b
            k.op("dve", lambda e: e.tensor_tensor(out=sc[:], in0=psb[:, i % 4, :], in1=mk[:], op=ALU.mult),
                 [psb, mk], [sc])
            o = po[i // 4]
            k.op("pe", lambda e: e.matmul(o[:, i % 4, :], lhsT=sc[:], rhs=v[:], start=(i % 4 == 0), stop=False,
                                           skip_group_check=True), [sc, v], [o])
            st[(h, dr)] = (qpad, kh, v, eb, le, Tt)
        for ci in range(4):
            for i, (h, dr) in enumerate(HD):
                qt, kh, v, eb, le, Tt = st[(h, dr)]
                c = ci if dr == 0 else 3 - ci
                o = po[i // 4]
                k.op("pe", lambda e: e.matmul(o[:, i % 4, :], lhsT=qt[:, c, :],
                                               rhs=S16[(h, dr)][:], start=False, stop=(ci == 3),
                                               skip_group_check=True), [qt, S16[(h, dr)]], [o])
                pu = pub[i // 4]
                k.op("pe", lambda e: e.matmul(pu[:, i % 4, :], lhsT=kh[:, c, :],
                                               rhs=v[:], start=True, stop=True,
                                               skip_group_check=True), [kh, v], [pu])
                s32 = S32[(h, dr)]
                dcol = eb[:, 32 * c + le:32 * c + le + 1]
                k.op("dve", lambda e: e.scalar_tensor_tensor(out=s32[:], in0=s32[:], scalar=dcol,
                                                             in1=pu[:, i % 4, :], op0=ALU.mult, op1=ALU.add),
                     [s32, eb, pu], [s32])
                k.op("act", lambda e: e.copy(out=S16[(h, dr)][:], in_=s32[:]), [s32], [S16[(h, dr)]])
        for i, (h, dr) in enumerate(HD):
            Tt = st[(h, dr)][5]
            os_ = osp.next()
            k.op("act", lambda e: e.copy(out=os_[:], in_=po[i // 4][:, i % 4, :]), [po[i // 4]], [os_])
            dst = T.of_d if dr == 0 else T.ob_d
            k.store("pool", dst, dst.ap()[Tt * 128:(Tt + 1) * 128, h * 128:(h + 1) * 128], os_, os_[:])
    k.pop()


def phase_merge(k, T, C, l, xsrc, xsrc_ap, acc, acc_ap):
    import os
    CUT = float(os.environ.get('MCUT', '99'))
    k.push()
    wba = k.sb("wba", [128, 4, 1024], BF16)
    wbh = k.sb("wbh", [128, 4, 1024], BF16)
    wout = k.sb("wout", [128, 8, 1024], BF16)
    wr = k.sb("wr", [128, 8, 16], F32)
    stg = Pool(k, "stg", [128, 1024], F32, 2)
    for kc in range(4):
        k.load("pool", wba, wba[:, kc, :], T.w_branch_att, T.w_branch_att.ap()[l, kc * 128:(kc + 1) * 128, :], disjoint=True)
        k.load("pool", wbh, wbh[:, kc, :], T.w_branch_hg, T.w_branch_hg.ap()[l, kc * 128:(kc + 1) * 128, :], disjoint=True)
    for kc in range(8):
        s_ = stg.next()
        k.load("sp", s_, s_[:], T.w_out, T.w_out.ap()[l, kc * 128:(kc + 1) * 128, :])
        k.op("dve", lambda e: e.tensor_tensor(out=wout[:, kc, :], in0=s_[:], in1=C.g1[:], op=ALU.mult),
             [s_, C.g1], [wout])
    k.load("sp", wr, wr[:], T.w_router, T.w_router.ap()[l].rearrange("(kc p) e -> p kc e", p=128))
    wr3 = [k.sb(f"wr3_{i}", [128, 8, 16], BF16) for i in range(3)]
    wrr = k.sb("wrr", [128, 8, 16], F32)
    k.op("act", lambda e: e.copy(out=wr3[0][:], in_=wr[:]), [wr], [wr3[0]])
    k.op("dve", lambda e: e.tensor_tensor(out=wrr[:], in0=wr[:], in1=wr3[0][:], op=ALU.subtract), [wr, wr3[0]], [wrr])
    k.op("act", lambda e: e.copy(out=wr3[1][:], in_=wrr[:]), [wrr], [wr3[1]])
    k.op("dve", lambda e: e.tensor_tensor(out=wrr[:], in0=wrr[:], in1=wr3[1][:], op=ALU.subtract), [wrr, wr3[1]], [wrr])
    k.op("act", lambda e: e.copy(out=wr3[2][:], in_=wrr[:]), [wrr], [wr3[2]])
    oap = Pool(k, "oat", [128, 512], BF16, 2)
    ofp = Pool(k, "of", [128, 4, 128], F32, 2)
    obp = Pool(k, "ob", [128, 4, 128], F32, 2)
    sgp = Pool(k, "sgm", [128, 512], BF16, 2)
    glp = Pool(k, "glm", [128, 2048], F32, 2)
    xp = Pool(k, "xm", [128, 1024], F32, 2)
    sqp = Pool(k, "sqm", [128, 4, 128], F32, 2)
    s4p = Pool(k, "s4", [128, 4], F32, 2)
    ohp = Pool(k, "ohb", [128, 512], BF16, 2)
    lTp = Pool(k, "lT", [128, 8, 128], BF16, 2)
    m1p = Pool(k, "m1", [128, 1024], F32, 2)
    m2p = Pool(k, "m2", [128, 1024], F32, 2)
    mbp = Pool(k, "mb", [128, 1024], BF16, 2)
    mTp = Pool(k, "mT", [128, 8, 128], BF16, 2)
    x1p = Pool(k, "x1", [128, 1024], F32, 2)
    junk = k.sb("junkm", [128, 1024], F32)
    s1p = Pool(k, "s1", [128, 1], F32, 4)
    h2p = Pool(k, "h2", [128, 1024], F32, 2)
    h2bp = Pool(k, "h2b", [128, 8, 128], BF16, 6)
    hbp = [Pool(k, f"hb{i}", [128, 1024], BF16, 2 if i == 0 else 1) for i in range(3)]
    r1p = Pool(k, "r1", [128, 1024], F32, 1)
    lgp = Pool(k, "lg", [128, 16], F32, 2)
    pbk = Pool(k, "pbk", [128, 512], F32, 6, "ps")
    ptb = k.ps("ptbm", [128, 1024], BF16)
    plg = k.ps("plg", [128, 16], F32)
    for Tt in range(int(os.environ.get('MTILES', NTILE))):
        r0 = Tt * 128
        oat = oap.next(); of = ofp.next(); ob = obp.next(); sg = sgp.next(); gl = glp.next(); xm = xp.next()
        k.load("sp", oat, oat[:], T.oatt_d, T.oatt_d.ap()[r0:r0 + 128, :])
        k.load("sp", of, of[:], T.of_d, T.of_d.ap()[r0:r0 + 128, :].rearrange("p (h e) -> p h e", e=128))
        k.load("sp", ob, ob[:], T.ob_d, T.ob_d.ap()[r0:r0 + 128, :].rearrange("p (h e) -> p h e", e=128))
        k.load("sp", sg, sg[:], T.sg_d, T.sg_d.ap()[r0:r0 + 128, :])
        k.load("sp", gl, gl[:], T.gl_d, T.gl_d.ap()[r0:r0 + 128, :])
        k.load("sp", xm, xm[:], xsrc, xsrc_ap[r0:r0 + 128, :])
        k.op("pool", lambda e: e.tensor_tensor(out=of[:], in0=of[:], in1=ob[:], op=ALU.add), [of, ob], [of])
        sq = sqp.next(); s4 = s4p.next()
        k.op("dve", lambda e: e.tensor_tensor(out=sq[:], in0=of[:], in1=of[:], op=ALU.mult), [of], [sq])
        k.op("dve", lambda e: e.tensor_reduce(out=s4[:], in_=sq[:], axis=AX.X, op=ALU.add), [sq], [s4])
        k.op("act", lambda e: e.activation(out=s4[:], in_=s4[:], func=AF.Sqrt, scale=1.0 / 128, bias=EPS), [s4], [s4])
        k.op("dve", lambda e: e.reciprocal(out=s4[:], in_=s4[:]), [s4], [s4])
        k.op("dve", lambda e: e.tensor_tensor(out=of[:], in0=of[:], in1=s4[:].unsqueeze(2).to_broadcast([128, 4, 128]),
                                              op=ALU.mult), [of, s4], [of])
        k.op("pool", lambda e: e.tensor_tensor(out=of[:], in0=of[:], in1=C.hgw[:], op=ALU.mult), [of, C.hgw], [of])
        ohb = ohp.next()
        k.op("dve", lambda e: e.tensor_tensor(out=ohb[:].rearrange("p (h e) -> p h e", e=128), in0=of[:],
                                              in1=sg[:].rearrange("p (h e) -> p h e", e=128), op=ALU.mult),
             [of, sg], [ohb])
        if CUT < 1:
            continue
        for j in range(4):
            tp(k, ptb, ptb[:, j * 128:(j + 1) * 128], oat, oat[:, j * 128:(j + 1) * 128], T.ident_bf)
            tp(k, ptb, ptb[:, (4 + j) * 128:(5 + j) * 128], ohb, ohb[:, j * 128:(j + 1) * 128], T.ident_bf)
        lT = lTp.next()
        k.op("act", lambda e: e.copy(out=lT[:], in_=ptb[:].rearrange("p (j q) -> p j q", q=128)), [ptb], [lT])
        if CUT < 2:
            continue
        m1 = m1p.next(); m2 = m2p.next()
        for hf in range(2):
            pa = pbk.next()
            for kc in range(4):
                mm(k, pa, pa[:], lT, lT[:, kc, :], wba, wba[:, kc, hf * 512:(hf + 1) * 512], kc == 0, kc == 3)
            k.op("dve", lambda e: e.tensor_tensor(out=m1[:, hf * 512:(hf + 1) * 512], in0=pa[:],
                                                  in1=gl[:, hf * 512:(hf + 1) * 512], op=ALU.mult), [pa, gl], [m1])
            ph = pbk.next()
            for kc in range(4):
                mm(k, ph, ph[:], lT, lT[:, 4 + kc, :], wbh, wbh[:, kc, hf * 512:(hf + 1) * 512], kc == 0, kc == 3)
            k.op("dve", lambda e: e.tensor_tensor(out=m2[:, hf * 512:(hf + 1) * 512], in0=ph[:],
                                                  in1=gl[:, 1024 + hf * 512:1024 + (hf + 1) * 512], op=ALU.mult),
                 [ph, gl], [m2])
        mb = mbp.next()
        k.op("pool", lambda e: e.tensor_tensor(out=mb[:], in0=m1[:], in1=m2[:], op=ALU.add), [m1, m2], [mb])
        for j in range(8):
            tp(k, ptb, ptb[:, j * 128:(j + 1) * 128], mb, mb[:, j * 128:(j + 1) * 128], T.ident_bf)
        mT = mTp.next()
        k.op("act", lambda e: e.copy(out=mT[:], in_=ptb[:].rearrange("p (j q) -> p j q", q=128)), [ptb], [mT])
        x1 = x1p.next()
        for hf in range(2):
            po = pbk.next()
            for kc in range(8):
                mm(k, po, po[:], mT, mT[:, kc, :], wout, wout[:, kc, hf * 512:(hf + 1) * 512], kc == 0, kc == 7)
            k.op("dve", lambda e: e.tensor_tensor(out=x1[:, hf * 512:(hf + 1) * 512], in0=po[:],
                                                  in1=xm[:, hf * 512:(hf + 1) * 512], op=ALU.add), [po, xm], [x1])
        if CUT < 3:
            continue
        k.store("pool", T.xs1, T.xs1.ap()[r0:r0 + 128, :], x1, x1[:])
        k.store("pool", acc, acc_ap[r0:r0 + 128, :], x1, x1[:])
        if CUT < 3.1:
            continue
        s1 = s1p.next()
        k.op("act", lambda e: e.activation(out=junk[:], in_=x1[:], func=AF.Square, accum_out=s1[:]), [x1], [junk, s1])
        if CUT < 3.2:
            continue
        k.op("act", lambda e: e.activation(out=s1[:], in_=s1[:], func=AF.Sqrt, scale=1.0 / D, bias=EPS), [s1], [s1])
        k.op("dve", lambda e: e.reciprocal(out=s1[:], in_=s1[:]), [s1], [s1])
        if CUT < 3.3:
            continue
        h2 = h2p.next()
        k.op("act", lambda e: e.activation(out=h2[:], in_=x1[:], func=AF.Copy, scale=s1[:]), [x1, s1], [h2])
        if CUT < 3.4:
            continue
        k.op("dve", lambda e: e.tensor_tensor(out=h2[:], in0=h2[:], in1=C.a2[:], op=ALU.mult), [h2, C.a2], [h2])
        if CUT < 3.5:
            continue
        k.op("dve", lambda e: e.tensor_tensor(out=h2[:], in0=h2[:], in1=C.b2[:], op=ALU.add), [h2, C.b2], [h2])
        if CUT < 4:
            continue
        hb = [hbp[i].next() for i in range(3)]
        r1 = r1p.next()
        k.op("act", lambda e: e.copy(out=hb[0][:], in_=h2[:]), [h2], [hb[0]])
        k.op("dve", lambda e: e.tensor_tensor(out=r1[:], in0=h2[:], in1=hb[0][:], op=ALU.subtract), [h2, hb[0]], [r1])
        k.op("act", lambda e: e.copy(out=hb[1][:], in_=r1[:]), [r1], [hb[1]])
        k.op("dve", lambda e: e.tensor_tensor(out=r1[:], in0=r1[:], in1=hb[1][:], op=ALU.subtract), [r1, hb[1]], [r1])
        k.op("act", lambda e: e.copy(out=hb[2][:], in_=r1[:]), [r1], [hb[2]])
        hT3 = [h2bp.next() for _ in range(3)]
        for i in range(3):
            for j in range(8):
                tp(k, ptb, ptb[:, j * 128:(j + 1) * 128], hb[i], hb[i][:, j * 128:(j + 1) * 128], T.ident_bf)
            k.op("act" if i != 1 else "dve", lambda e: e.tensor_copy(out=hT3[i][:], in_=ptb[:].rearrange("p (j q) -> p j q", q=128))
                 if i == 1 else e.copy(out=hT3[i][:], in_=ptb[:].rearrange("p (j q) -> p j q", q=128)), [ptb], [hT3[i]])
        k.store("pool", T.h2b_d, T.h2b_d.ap()[r0:r0 + 128, :], hb[0], hb[0][:])
        if CUT < 5:
            continue
        terms = [(0, 0), (0, 1), (1, 0), (0, 2), (2, 0), (1, 1)]
        for ti, (a_, b_) in enumerate(terms):
            for kc in range(8):
                mm(k, plg, plg[:], hT3[a_], hT3[a_][:, kc, :], wr3[b_], wr3[b_][:, kc, :],
                   ti == 0 and kc == 0, ti == len(terms) - 1 and kc == 7)
        if CUT < 6:
            continue
        lg = lgp.next()
        mx = s1p.next(); se = s1p.next()
        k.op("dve", lambda e: e.tensor_reduce(out=mx[:], in_=plg[:], axis=AX.X, op=ALU.max), [plg], [mx])
        k.op("dve", lambda e: e.tensor_scalar(out=mx[:], in0=mx[:], scalar1=-1.0, scalar2=None, op0=ALU.mult), [mx], [mx])
        k.op("act", lambda e: e.activation(out=lg[:], in_=plg[:], func=AF.Exp, bias=mx[:], accum_out=se[:]),
             [plg, mx], [lg, se])
        k.op("dve", lambda e: e.reciprocal(out=se[:], in_=se[:]), [se], [se])
        k.op("dve", lambda e: e.tensor_scalar(out=lg[:], in0=lg[:], scalar1=se[:], scalar2=None, op0=ALU.mult),
             [lg, se], [lg])
        k.store("pool", T.aff_d, T.aff_d.ap()[r0:r0 + 128, :], lg, lg[:])
    k.pop()


def phase_route(k, T, idx_all):
    k.push()
    for nm, shp, dt in (("iota", [128, CAP], F32), ("rst16", [128, CAP], F32), ("U_bf", [128, 128], BF16), ("tcol", [128, 2], BF16)):
        t = k.sb(nm, shp, dt)
        src = getattr(T, "c_" + nm)
        k.load("sp", t, t[:], src, src.ap())
        setattr(T, nm, t)
    aff = k.sb("affall", [128, 128, NEXP], F32)
    k.load("sp", aff, aff[:], T.aff_d, T.aff_d.ap().rearrange("(t p) e -> t p e", p=128))
    cmp = k.sb("cmp", [128, 128, NEXP], F32)
    ones = k.sb("ones", [128, 128], F32)
    k.op("pool", lambda e: e.memset(ones[:], 1.0), [], [ones])
    lo = k.sb("lo", [128, NEXP], F32); hi = k.sb("hi", [128, NEXP], F32)
    mid = k.sb("mid", [128, NEXP], F32); cnt = k.sb("cnt", [128, NEXP], F32)
    ge = k.sb("ge", [128, NEXP], F32); d1 = k.sb("d1", [128, NEXP], F32)
    pt = k.ps("ptot", [128, NEXP], F32)
    k.op("dve", lambda e: e.memset(lo[:], 0.0), [], [lo])
    k.op("dve", lambda e: e.memset(hi[:], 1.0), [], [hi])
    for it in range(40):
        k.op("dve", lambda e: e.tensor_tensor(out=mid[:], in0=lo[:], in1=hi[:], op=ALU.add), [lo, hi], [mid])
        k.op("dve", lambda e: e.tensor_scalar(out=mid[:], in0=mid[:], scalar1=0.5, scalar2=None, op0=ALU.mult), [mid], [mid])
        k.op("dve", lambda e: e.tensor_tensor(out=cmp[:], in0=aff[:],
                                              in1=mid[:].unsqueeze(1).to_broadcast([128, 128, NEXP]),
                                              op=ALU.is_gt), [aff, mid], [cmp])
        k.op("dve", lambda e: e.tensor_reduce(out=cnt[:], in_=cmp[:].rearrange("t p e -> t e p"), axis=AX.X,
                                              op=ALU.add), [cmp], [cnt])
        mm(k, pt, pt[:], ones, ones[:], cnt, cnt[:], True, True)
        k.op("dve", lambda e: e.tensor_scalar(out=ge[:], in0=pt[:], scalar1=float(CAP) - 0.5, scalar2=None,
                                              op0=ALU.is_ge), [pt], [ge])
        k.op("dve", lambda e: e.tensor_tensor(out=d1[:], in0=mid[:], in1=lo[:], op=ALU.subtract), [mid, lo], [d1])
        k.op("dve", lambda e: e.tensor_tensor(out=d1[:], in0=d1[:], in1=ge[:], op=ALU.mult), [d1, ge], [d1])
        k.op("dve", lambda e: e.tensor_tensor(out=lo[:], in0=lo[:], in1=d1[:], op=ALU.add), [lo, d1], [lo])
        k.op("dve", lambda e: e.tensor_tensor(out=d1[:], in0=hi[:], in1=mid[:], op=ALU.subtract), [hi, mid], [d1])
        k.op("dve", lambda e: e.tensor_tensor(out=d1[:], in0=d1[:], in1=ge[:], op=ALU.mult), [d1, ge], [d1])
        k.op("dve", lambda e: e.tensor_tensor(out=hi[:], in0=mid[:], in1=d1[:], op=ALU.add), [mid, d1], [hi])
    msk = k.sb("msk", [128, NEXP, 128], F32)
    scn = k.sb("scn", [128, NEXP, 128], F32)
    scb = k.sb("scb", [128, NEXP, 129], BF16)
    k.op("dve", lambda e: e.tensor_tensor(out=msk[:].rearrange("t e p -> t p e"), in0=aff[:],
                                          in1=lo[:].unsqueeze(1).to_broadcast([128, 128, NEXP]), op=ALU.is_gt),
         [aff, lo], [msk])
    k.op("dve", lambda e: e.tensor_tensor_scan(out=scn[:].rearrange("t e p -> t (e p)"),
                                               data0=T.rst16[:], data1=msk[:].rearrange("t e p -> t (e p)"),
                                               initial=0.0, op0=ALU.mult, op1=ALU.add), [T.rst16, msk], [scn])
    k.op("act", lambda e: e.copy(out=scb[:, :, 0:128], in_=scn[:]), [scn], [scb])
    k.op("act", lambda e: e.copy(out=scb[:, :, 128], in_=T.tcol[:, 0:1].to_broadcast([128, NEXP])), [T.tcol], [scb])
    totb = k.sb("totb", [128, NEXP], BF16)
    tot = k.sb("tot", [128, NEXP], F32)
    k.op("dve", lambda e: e.tensor_copy(out=totb[:], in_=scn[:, :, 127]), [scn], [totb])
    k.op("dve", lambda e: e.tensor_copy(out=tot[:], in_=scn[:, :, 127]), [scn], [tot])
    pof = k.ps("pof", [128, NEXP], F32)
    mm(k, pof, pof[:], T.U_bf, T.U_bf[:], totb, totb[:], True, True)
    offs = k.sb("offs", [128, NEXP], F32)
    incl = k.sb("incl", [128, NEXP], F32)
    k.op("dve", lambda e: e.tensor_copy(out=offs[:], in_=pof[:]), [pof], [offs])
    k.op("dve", lambda e: e.tensor_tensor(out=incl[:], in0=offs[:], in1=tot[:], op=ALU.add), [offs, tot], [incl])
    t1p = Pool(k, "rt1", [128, CAP], F32, 2)
    ohp = Pool(k, "oh", [128, CAP], BF16, 2)
    wp = Pool(k, "wrk", [128, CAP], BF16, 2)
    pa = Pool(k, "pa", [128, 129], F32, 2, "ps")
    pr = Pool(k, "pr", [128, 1], F32, 2, "ps")
    rcp = Pool(k, "rcol", [128, 1], F32, 4)
    fnp = Pool(k, "fine", [128, 1], F32, 4)
    jk = k.sb("jk", [128, 128], F32)
    idxf = k.sb("idxf", [128, NEXP, 16], F32)
    for ex in range(NEXP):
        t1 = t1p.next(); oh = ohp.next(); w_ = wp.next(); t2 = t1p.next()
        k.op("dve", lambda e: e.tensor_scalar(out=t1[:], in0=T.iota[:], scalar1=offs[:, ex:ex + 1], scalar2=None,
                                              op0=ALU.is_ge), [T.iota, offs], [t1])
        k.op("dve", lambda e: e.scalar_tensor_tensor(out=oh[:], in0=T.iota[:], scalar=incl[:, ex:ex + 1], in1=t1[:],
                                                     op0=ALU.is_lt, op1=ALU.mult), [T.iota, incl, t1], [oh])
        k.op("dve", lambda e: e.tensor_scalar(out=t2[:], in0=T.iota[:], scalar1=offs[:, ex:ex + 1], scalar2=None,
                                              op0=ALU.subtract), [T.iota, offs], [t2])
        k.op("dve", lambda e: e.tensor_tensor(out=w_[:], in0=t2[:], in1=oh[:], op=ALU.mult), [t2, oh], [w_])
        for kq in range(16):
            pA = pa.next(); pR = pr.next()
            mm(k, pA, pA[:], oh, oh[:, kq * 128:(kq + 1) * 128], scb, scb[:, ex, :], True, True)
            mm(k, pR, pR[:], w_, w_[:, kq * 128:(kq + 1) * 128], T.tcol, T.tcol[:, 1:2], True, True)
            rc = rcp.next(); fn = fnp.next()
            k.op("act", lambda e: e.copy(out=rc[:], in_=pR[:]), [pR], [rc])
            k.op("dve", lambda e: e.tensor_scalar(out=jk[:], in0=pA[:, 0:128], scalar1=rc[:], scalar2=0.0,
                                                  op0=ALU.is_le, op1=ALU.add, accum_out=fn[:]), [pA, rc], [jk, fn])
            k.op("dve", lambda e: e.scalar_tensor_tensor(out=idxf[:, ex, kq:kq + 1], in0=pA[:, 128:129], scalar=128.0,
                                                         in1=fn[:], op0=ALU.mult, op1=ALU.add), [pA, fn], [idxf])
    k.op("dve", lambda e: e.tensor_copy(out=idx_all[:], in_=idxf[:]), [idxf], [idx_all])
    k.pop()


def phase_ffn(k, T, C, l, idx_all, acc, acc_ap):
    k.push()
    wg = k.sb("wg", [128, 8, FF], BF16)
    wu = k.sb("wu", [128, 8, FF], BF16)
    wd = k.sb("wd", [128, 16, 1024], BF16)
    stg = Pool(k, "stgd", [128, 1024], F32, 2)
    xep = Pool(k, "xe", [128, 1024], BF16, 3)
    twp = Pool(k, "tw", [128, NEXP], F32, 8)
    hTp = Pool(k, "xeT", [128, 8, 512], BF16, 2)
    hid = k.sb("hid", [128, 16, 512], BF16)
    sgp = Pool(k, "sgf", [128, 512], F32, 2)
    yop = Pool(k, "yo", [128, 1024], F32, 3)
    pgu = Pool(k, "pgu", [128, 512], F32, 4, "ps")
    pdn = Pool(k, "pdn", [128, 512], F32, 2, "ps")
    ptx = Pool(k, "ptx", [128, 1024], BF16, 2, "ps")
    for ex in range(NEXP):
        for kc in range(8):
            k.load("pool", wg, wg[:, kc, :], T.w_exp_gate, T.w_exp_gate.ap()[l, ex, kc * 128:(kc + 1) * 128, :])
            k.load("pool", wu, wu[:, kc, :], T.w_exp_up, T.w_exp_up.ap()[l, ex, kc * 128:(kc + 1) * 128, :])
        for fc in range(16):
            s_ = stg.next()
            k.load("sp", s_, s_[:], T.w_exp_down, T.w_exp_down.ap()[l, ex, fc * 128:(fc + 1) * 128, :])
            k.op("dve", lambda e: e.tensor_tensor(out=wd[:, fc, :], in0=s_[:], in1=C.g2[:], op=ALU.mult),
                 [s_, C.g2], [wd])
        for B in range(CAP // 512):
            hT = hTp.next()
            tws = []
            for t in range(4):
                kq = B * 4 + t
                xe = xep.next(); tw = twp.next()
                ix = idx_all[:, ex, kq:kq + 1]
                k.dma("pool", lambda e: e.indirect_dma_start(out=xe[:], out_offset=None, in_=T.h2b_d.ap(),
                                                              in_offset=bass.IndirectOffsetOnAxis(ap=ix, axis=0)),
                      xe, reads=[T.h2b_d, idx_all], writes=[xe])
                k.dma("pool", lambda e: e.indirect_dma_start(out=tw[:], out_offset=None, in_=T.aff_d.ap(),
                                                              in_offset=bass.IndirectOffsetOnAxis(ap=ix, axis=0)),
                      tw, reads=[T.aff_d, idx_all], writes=[tw])
                tws.append(tw)
                px = ptx.next()
                for j in range(8):
                    tp(k, px, px[:, j * 128:(j + 1) * 128], xe, xe[:, j * 128:(j + 1) * 128], T.ident_bf)
                k.op("act", lambda e: e.copy(out=hT[:, :, t * 128:(t + 1) * 128],
                                             in_=px[:].rearrange("p (j q) -> p j q", q=128)), [px], [hT])
            for fc in range(16):
                pg = pgu.next()
                for kc in range(8):
                    mm(k, pg, pg[:], wg, wg[:, kc, fc * 128:(fc + 1) * 128], hT, hT[:, kc, :], kc == 0, kc == 7)
                pu = pgu.next()
                for kc in range(8):
                    mm(k, pu, pu[:], wu, wu[:, kc, fc * 128:(fc + 1) * 128], hT, hT[:, kc, :], kc == 0, kc == 7)
                sg = sgp.next()
                k.op("act", lambda e: e.activation(out=sg[:], in_=pg[:], func=AF.Silu), [pg], [sg])
                k.op("dve", lambda e: e.tensor_tensor(out=hid[:, fc, :], in0=sg[:], in1=pu[:], op=ALU.mult),
                     [sg, pu], [hid])
            for t in range(4):
                kq = B * 4 + t
                yo = yop.next()
                for hf in range(2):
                    pd = pdn.next()
                    for fc in range(16):
                        mm(k, pd, pd[:], hid, hid[:, fc, t * 128:(t + 1) * 128], wd, wd[:, fc, hf * 512:(hf + 1) * 512],
                           fc == 0, fc == 15)
                    k.op("act", lambda e: e.activation(out=yo[:, hf * 512:(hf + 1) * 512], in_=pd[:], func=AF.Copy,
                                                       scale=tws[t][:, ex:ex + 1]), [pd, tws[t]], [yo])
                ix = idx_all[:, ex, kq:kq + 1]
                k.dma("pool", lambda e: e.indirect_dma_start(out=acc_ap, out_offset=bass.IndirectOffsetOnAxis(ap=ix, axis=0),
                                                              in_=yo[:], in_offset=None, compute_op=ALU.add),
                      yo, reads=[yo, idx_all], writes=[acc])
    k.pop()


def build(stop_after=99, dbg=(), nlayers=2, skip=()):
    nc = bass.Bass("TRN2", target_bir_lowering=False)
    k = K(nc)
    T = _Dummy()

    def inp(name, shape, dt=F32):
        setattr(T, name, k.dram(name, shape, dt, kind="ExternalInput"))

    inp("x", [1, S, D]); inp("c", [1, D]); inp("ada_w", [2, D, 6 * D]); inp("ada_b", [2, 6 * D])
    inp("norm_mix_w", [2, D]); inp("norm_ffn_w", [2, D]); inp("w_in", [2, D, INW])
    inp("q_norm_w", [2, 64]); inp("k_norm_w", [2, 64]); inp("hg_lower_bounds", [2, 2, 512])
    inp("hg_norm_w", [2, 128]); inp("w_branch_att", [2, 512, D]); inp("w_branch_hg", [2, 512, D])
    inp("w_out", [2, D, D]); inp("w_router", [2, D, NEXP])
    if stop_after >= 7:
        inp("w_exp_gate", [2, NEXP, D, FF]); inp("w_exp_up", [2, NEXP, D, FF]); inp("w_exp_down", [2, NEXP, FF, D])
    inp("cos64", [S, 64]); inp("sin64", [S, 64])
    inp("c_ident_bf", [128, 128], BF16); inp("c_ident_f", [128, 128]); inp("c_mask_f", [128, 128], BF16)
    inp("c_mask_b", [128, 128], BF16); inp("c_rst", [128, 128]); inp("c_cmask", [128, 4, 128], BF16)
    inp("c_iota", [128, CAP]); inp("c_rst16", [128, CAP]); inp("c_U_bf", [128, 128], BF16); inp("c_tcol", [128, 2], BF16)
    T.y = k.dram("y", [S, D], F32, kind="ExternalOutput")

    def scr(name, shape, dt):
        setattr(T, name, k.dram(name, shape, dt, kind="ExternalOutput" if name in dbg else "Internal"))

    scr("modd", [2, 6 * D], F32)
    scr("qT_d", [128, 4, S], BF16); scr("kT_d", [128, S], BF16); scr("va_d", [S, 256], BF16)
    scr("vh_d", [S, 512], BF16); scr("sg_d", [S, 512], BF16); scr("gl_d", [S, 2048], F32)
    scr("hq_d", [4, 128, S], BF16); scr("g_d", [2, 4, 128, S], F32); scr("kk_d", [2, 4, 128, S], F32)
    scr("oatt_d", [S, 512], BF16); scr("of_d", [S, 512], F32); scr("ob_d", [S, 512], F32)
    scr("xs1", [S, D], F32); scr("xs2", [S, D], F32); scr("h2T_d", [128, 8, S], BF16)
    scr("aff_d", [S, NEXP], F32); scr("h2b_d", [S, D], BF16)
    for nm, dt in (("ident_bf", BF16), ("ident_f", F32), ("mask_f", BF16), ("mask_b", BF16), ("rst", F32)):
        t = k.sb(nm, [128, 128], dt)
        src = getattr(T, "c_" + nm)
        k.load("sp", t, t[:], src, src.ap())
        setattr(T, nm, t)
    T.cmask = k.sb("cmask", [128, 4, 128], BF16)
    k.load("sp", T.cmask, T.cmask[:], T.c_cmask, T.c_cmask.ap())
    T.ident65 = k.sb("ident65", [65, 65], BF16)
    k.load("sp", T.ident65, T.ident65[:], T.c_ident_bf, T.c_ident_bf.ap()[0:65, 0:65])
    phase_mod(k, T)
    if stop_after >= 1:
        for l in range(nlayers):
            k.push()
            C = layer_consts(k, T, l)
            idx_all = k.sb("idx_all", [128, NEXP, 16], I32)
            xsrc, xap = (T.x, T.x.ap()[0]) if l == 0 else (T.xs2, T.xs2.ap())
            acc, aap = (T.xs2, T.xs2.ap()) if l == 0 else (T.y, T.y.ap())
            if 2 not in skip:
                phase_proj(k, T, C, l, xsrc, xap)
            if stop_after >= 3 and 3 not in skip:
                phase_attn(k, T)
            if stop_after >= 4 and 4 not in skip:
                phase_hgrn(k, T)
            if stop_after >= 5 and 5 not in skip:
                phase_merge(k, T, C, l, xsrc, xap, acc, aap)
            if stop_after >= 6:
                phase_route(k, T, idx_all)
            if stop_after >= 7:
                phase_ffn(k, T, C, l, idx_all, acc, aap)
            k.pop()
            if stop_after < 7:
                break
    k.barrier()
    k.close()
    return nc


def host_consts():
    bf = ml_dtypes.bfloat16
    t = np.arange(S)
    inv = (10000.0 ** (-np.arange(0, 32, 2, dtype=np.float32) / 32)).astype(np.float32)
    ang_r = ((t // 64).astype(np.float32)[:, None] * inv).astype(np.float32)
    ang_c = ((t % 64).astype(np.float32)[:, None] * inv).astype(np.float32)
    cr, sr, cc, sc = np.cos(ang_r), np.sin(ang_r), np.cos(ang_c), np.sin(ang_c)
    cos64 = np.concatenate([cr, cr, cc, cc], 1).astype(np.float32)
    sin64 = np.concatenate([-sr, sr, -sc, sc], 1).astype(np.float32)
    j = np.arange(128)[:, None]
    i = np.arange(128)[None, :]
    same = (j // 32) == (i // 32)
    return {
        "cos64": cos64, "sin64": sin64,
        "c_ident_bf": np.eye(128, dtype=np.float32).astype(bf), "c_ident_f": np.eye(128, dtype=np.float32),
        "c_mask_f": (same & (j <= i)).astype(np.float32).astype(bf),
        "c_mask_b": (same & (j >= i)).astype(np.float32).astype(bf),
        "c_cmask": np.broadcast_to(((np.arange(128)[None, :] // 32) == np.arange(4)[:, None]).astype(np.float32),
                                   (128, 4, 128)).astype(bf).copy(),
        "c_iota": np.broadcast_to(np.arange(CAP, dtype=np.float32), (128, CAP)).copy(),
        "c_rst16": np.broadcast_to(((np.arange(CAP) % 128) != 0).astype(np.float32), (128, CAP)).copy(),
        "c_U_bf": (np.arange(128)[:, None] < np.arange(128)[None, :]).astype(np.float32).astype(bf),
        "c_tcol": np.stack([np.arange(128, dtype=np.float32), np.ones(128, np.float32)], 1).astype(bf),
        "c_rst": np.broadcast_to(((np.arange(128) % 32) != 0).astype(np.float32), (128, 128)).copy(),
    }


def kernel(**inputs):
    nc = build()
    m = {kk_: np.ascontiguousarray(np.asarray(v, dtype=np.float32)) for kk_, v in inputs.items()}
    m.update(host_consts())
    res = run_bass_kernel_spmd(nc, [m], core_ids=[0])
    return np.asarray(res.results[0]["y"], dtype=np.float32).reshape(1, S, D)
```

```python
import numpy as np
import ml_dtypes
from concourse.bass_utils import run_bass_kernel_spmd

from contextlib import ExitStack
import concourse.bass as bass
import concourse.mybir as mybir

F32 = mybir.dt.float32
BF16 = mybir.dt.bfloat16
I32 = mybir.dt.int32
U32 = mybir.dt.uint32
AF = mybir.ActivationFunctionType
ALU = mybir.AluOpType
AX = mybir.AxisListType

SEM_ROT = 30000
SAME_ENGINE_SYNC = True


class Tl:
    def __init__(self, k, t, name, space):
        self.k = k
        self.t = t
        self.name = name
        self.space = space
        self.w = {}
        self.r = {}
        self.dsem = {}
        self.dcnt = {}
        k.tiles.append(self)

    def __getitem__(self, idx):
        return self.t[idx]

    def ap(self):
        return self.t.ap() if hasattr(self.t, "ap") else self.t[:]


class K:
    ENG = ("pe", "dve", "act", "pool", "sp")

    def __init__(self, nc):
        self.nc = nc
        self.es = ExitStack()
        self.eng = {"pe": nc.tensor, "dve": nc.vector, "act": nc.scalar,
                    "pool": nc.gpsimd, "sp": nc.sync}
        self.gen = {e: 0 for e in self.ENG}
        self.sem = {}
        self.cnt = {e: 0 for e in self.ENG}
        for e in self.ENG:
            self.sem[(e, 0)] = self.es.enter_context(nc.semaphore(f"s_{e}_0"))
        self.seen = {e: {} for e in self.ENG}
        self.seenD = {e: {} for e in self.ENG}
        self.nsem = len(self.ENG)
        self.uid = 0
        self.tiles = []
        self.stacks = []
        self.dsem_scopes = []
        self.free_dsems = {}

    def push(self):
        self.stacks.append(ExitStack())
        self.dsem_scopes.append([])

    def pop(self):
        self.barrier()
        for t, q in self.dsem_scopes.pop():
            if q in t.dsem:
                self.free_dsems.setdefault(q, []).append((t.dsem.pop(q), t.dcnt.pop(q)))
        for e in self.ENG:
            self.seenD[e] = {}
        self.stacks.pop().close()

    def _st(self):
        return self.stacks[-1] if self.stacks else self.es

    def sb(self, name, shape, dt):
        self.uid += 1
        t = self._st().enter_context(self.nc.sbuf_tensor(f"{name}_{self.uid}", list(shape), dt))
        return Tl(self, t, name, "sb")

    def ps(self, name, shape, dt=F32):
        self.uid += 1
        t = self._st().enter_context(self.nc.psum_tensor(f"{name}_{self.uid}", list(shape), dt))
        return Tl(self, t, name, "ps")

    def dram(self, name, shape, dt, kind="Internal"):
        t = self.nc.dram_tensor(name, list(shape), dt, kind=kind)
        return Tl(self, t, name, "dram")

    def close(self):
        while self.stacks:
            self.stacks.pop().close()
        self.es.close()


    def _wait(self, e, ev, raw=True):
        if ev is None:
            return
        if ev[0] == "E":
            _, src, gen, c = ev
            if src == e and (e == "pe" or not SAME_ENGINE_SYNC or not raw):
                return
            key = (src, gen)
            if self.seen[e].get(key, 0) >= c:
                return
            self.eng[e].wait_ge(self.sem[key], c)
            self.seen[e][key] = c
        else:
            _, tl, q = ev
            if q not in tl.dsem:
                return
            tgt = tl.dcnt[q] * 16
            if self.seenD[e].get((id(tl), q), 0) >= tgt:
                return
            self.eng[e].wait_ge(tl.dsem[q], tgt)
            self.seenD[e][(id(tl), q)] = tgt

    @staticmethod
    def _key(ev):
        return ev[:3] if ev[0] == "E" else ("D", id(ev[1]), ev[2])

    def _deps(self, e, reads, writes, disjoint=False):
        for t in reads:
            for ev in t.w.values():
                self._wait(e, ev)
        if disjoint:
            return
        for t in writes:
            for ev in t.w.values():
                self._wait(e, ev, raw=False)
            for ev in t.r.values():
                self._wait(e, ev, raw=False)

    def _commit(self, ev, reads, writes, disjoint=False):
        for t in reads:
            if t in writes:
                continue
            t.r[self._key(ev)] = ev
        for t in writes:
            if disjoint:
                t.w[self._key(ev)] = ev
            else:
                t.w = {self._key(ev): ev}
                t.r = {}

    def barrier(self):
        for t in self.tiles:
            for ev in list(t.w.values()) + list(t.r.values()):
                self._wait("sp", ev)
            for q in list(t.dsem):
                self._wait("sp", ("D", t, q))
        for e in self.ENG:
            if e != "sp" and self.cnt[e] > 0:
                self._wait("sp", ("E", e, self.gen[e], self.cnt[e]))
        self.op("sp", lambda e: e.nop())
        ev = ("E", "sp", self.gen["sp"], self.cnt["sp"])
        for e in self.ENG:
            if e != "sp":
                self._wait(e, ev)
        for t in self.tiles:
            t.w = {}
            t.r = {}

    def op(self, e, fn, reads=(), writes=()):
        self._deps(e, reads, writes)
        if self.cnt[e] >= SEM_ROT:
            self.gen[e] += 1
            self.cnt[e] = 0
            self.sem[(e, self.gen[e])] = self.es.enter_context(
                self.nc.semaphore(f"s_{e}_{self.gen[e]}"))
            self.nsem += 1
        ins = fn(self.eng[e])
        self.cnt[e] += 1
        g = self.gen[e]
        ins.then_inc(self.sem[(e, g)], 1)
        ev = ("E", e, g, self.cnt[e])
        self._commit(ev, reads, writes)
        return ins

    def dma(self, q, fn, sbt, reads=(), writes=(), disjoint=False):
        self._deps(q, reads, writes, disjoint)
        if q not in sbt.dsem:
            self.uid += 1
            if self.free_dsems.get(q):
                sbt.dsem[q], sbt.dcnt[q] = self.free_dsems[q].pop()
            else:
                sbt.dsem[q] = self.es.enter_context(self.nc.semaphore(f"d_{q}_{sbt.name}_{self.uid}"))
                sbt.dcnt[q] = 0
                self.nsem += 1
            if self.dsem_scopes:
                self.dsem_scopes[-1].append((sbt, q))
        ins = fn(self.eng[q])
        ins.then_inc(sbt.dsem[q], 16)
        sbt.dcnt[q] += 1
        ev = ("D", sbt, q)
        self._commit(ev, reads, writes, disjoint)
        return ins

    def load(self, q, dst, dst_ap, src, src_ap, disjoint=False, **kw):
        return self.dma(q, lambda e: e.dma_start(out=dst_ap, in_=src_ap, **kw), dst,
                        reads=[src], writes=[dst], disjoint=disjoint)

    def store(self, q, dst, dst_ap, src, src_ap, disjoint=True, **kw):
        return self.dma(q, lambda e: e.dma_start(out=dst_ap, in_=src_ap, **kw), src,
                        reads=[src], writes=[dst], disjoint=disjoint)

    def finish(self, tiles, e="sp"):
        for t in tiles:
            for ev in list(t.w.values()) + list(t.r.values()):
                self._wait(e, ev)


class Pool:
    def __init__(self, k, name, shape, dt, n, space="sb"):
        mk = k.sb if space == "sb" else k.ps
        self.t = [mk(f"{name}{i}", shape, dt) for i in range(n)]
        self.i = 0

    def next(self):
        t = self.t[self.i % len(self.t)]
        self.i += 1
        return t

class _Dummy:
    pass
    pass

S = 16384
D = 1024
NBLK = S // 512
NTILE = S // 128
INW = 5376
EPS = 1e-6
O_Q, O_K, O_V, O_HQ, O_ZF, O_ZB, O_HI, O_HG, O_GL = 0, 512, 640, 768, 1280, 1792, 2304, 2816, 3328
NEXP = 16
FF = 2048
CAP = 2048


def mm(k, out, oap, lt, lap, rt, rap, start, stop):
    k.op("pe", lambda e: e.matmul(oap, lhsT=lap, rhs=rap, start=start, stop=stop),
         [lt, rt], [out])


def tp(k, out, oap, it, iap, ident):
    k.op("pe", lambda e: e.transpose(oap, iap, ident[:]), [it, ident], [out])


def colload(k, name, src, ap):
    t = k.sb(name, [128, 8], F32)
    k.load("sp", t, t[:], src, ap.rearrange("o (c p) -> p (o c)", p=128),
           allow_slow_non_contiguous=True)
    return t


def rowload(k, name, src, ap, n):
    t = k.sb(name, [128, n], F32)
    k.load("sp", t, t[:], src, ap.partition_broadcast(128))
    return t


def phase_mod(k, T):
    k.push()
    ccol = colload(k, "ccol", T.c, T.c.ap())
    cact = k.sb("cact", [128, 8], F32)
    k.op("act", lambda e: e.activation(out=cact[:], in_=ccol[:], func=AF.Silu), [ccol], [cact])
    wpool = Pool(k, "adaw", [128, 8, 512], F32, 2)
    brow = k.sb("brow", [1, 6144], F32)
    mrow = k.sb("mrow", [1, 6144], F32)
    pp = Pool(k, "pmod", [1, 512], F32, 2, "ps")
    for l in range(2):
        k.load("sp", brow, brow[:], T.ada_b, T.ada_b.ap()[l:l + 1, :])
        for nb in range(12):
            w = wpool.next()
            k.load("sp", w, w[:], T.ada_w,
                   T.ada_w.ap()[l].rearrange("(kc p) n -> p kc n", p=128)[:, :, nb * 512:(nb + 1) * 512])
            ps = pp.next()
            for kc in range(8):
                mm(k, ps, ps[:], cact, cact[:, kc:kc + 1], w, w[:, kc, :], kc == 0, kc == 7)
            k.op("dve", lambda e: e.tensor_tensor(out=mrow[:, nb * 512:(nb + 1) * 512], in0=ps[:],
                                                  in1=brow[:, nb * 512:(nb + 1) * 512], op=ALU.add),
                 [ps, brow], [mrow])
        k.store("sp", T.modd, T.modd.ap()[l:l + 1, :], mrow, mrow[:], disjoint=False)
    k.pop()


def layer_consts(k, T, l):
    C = _Dummy()
    md = T.modd.ap()
    sh1 = colload(k, "sh1", T.modd, md[l:l + 1, 0:1024])
    sc1 = colload(k, "sc1", T.modd, md[l:l + 1, 1024:2048])
    nw1 = colload(k, "nw1", T.norm_mix_w, T.norm_mix_w.ap()[l:l + 1, :])
    C.a1 = k.sb("a1", [128, 8], F32)
    C.b1 = sh1
    k.op("dve", lambda e: e.scalar_tensor_tensor(out=C.a1[:], in0=sc1[:], scalar=1.0, in1=nw1[:],
                                                 op0=ALU.add, op1=ALU.mult), [sc1, nw1], [C.a1])
    C.a2 = k.sb("a2", [128, 1024], F32)
    k.push()
    sc2 = rowload(k, "sc2", T.modd, md[l:l + 1, 4096:5120], 1024)
    nw2 = rowload(k, "nw2", T.norm_ffn_w, T.norm_ffn_w.ap()[l:l + 1, :], 1024)
    k.op("dve", lambda e: e.scalar_tensor_tensor(out=C.a2[:], in0=sc2[:], scalar=1.0, in1=nw2[:],
                                                 op0=ALU.add, op1=ALU.mult), [sc2, nw2], [C.a2])
    k.pop()
    C.b2 = rowload(k, "sh2", T.modd, md[l:l + 1, 3072:4096], 1024)
    C.g1 = rowload(k, "g1", T.modd, md[l:l + 1, 2048:3072], 1024)
    C.g2 = rowload(k, "g2", T.modd, md[l:l + 1, 5120:6144], 1024)
    C.qkw = k.sb("qkw", [128, 10, 64], F32)
    for h in range(10):
        src = T.q_norm_w if h < 8 else T.k_norm_w
        k.load("sp", C.qkw, C.qkw[:, h, :], src, src.ap()[l:l + 1, :].partition_broadcast(128))
    C.hgw = k.sb("hgw", [128, 4, 128], F32)
    for h in range(4):
        k.load("sp", C.hgw, C.hgw[:, h, :], T.hg_norm_w, T.hg_norm_w.ap()[l:l + 1, :].partition_broadcast(128))
    C.lb = k.sb("lb", [128, 8], F32)
    C.oml = k.sb("oml", [128, 8], F32)
    C.noml = k.sb("noml", [128, 8], F32)
    if l == 0:
        k.op("dve", lambda e: e.memset(C.lb[:], 0.0), [], [C.lb])
    else:
        a0 = k.sb("lba0", [128, 8], F32)
        a1 = k.sb("lba1", [128, 8], F32)
        hb = T.hg_lower_bounds.ap()
        k.load("sp", a0, a0[:], T.hg_lower_bounds, hb[0].rearrange("r (h d) -> d (r h)", d=128),
               allow_slow_non_contiguous=True)
        k.load("sp", a1, a1[:], T.hg_lower_bounds, hb[1].rearrange("r (h d) -> d (r h)", d=128),
               allow_slow_non_contiguous=True)
        k.op("dve", lambda e: e.tensor_tensor(out=a0[:], in0=a1[:], in1=a0[:], op=ALU.subtract), [a0, a1], [a0])
        k.op("act", lambda e: e.activation(out=C.lb[:], in_=a0[:], func=AF.Sigmoid), [a0], [C.lb])
    k.op("dve", lambda e: e.tensor_scalar(out=C.oml[:], in0=C.lb[:], scalar1=-1.0, scalar2=1.0,
                                          op0=ALU.mult, op1=ALU.add), [C.lb], [C.oml])
    k.op("dve", lambda e: e.tensor_scalar(out=C.noml[:], in0=C.oml[:], scalar1=-1.0, scalar2=None,
                                          op0=ALU.mult), [C.oml], [C.noml])
    return C


def phase_proj(k, T, C, l, xsrc, xsrc_ap):
    k.push()
    win = k.sb("win", [128, 8, INW], BF16)
    for kc in range(8):
        for cb in range(3):
            k.load("pool", win, win[:, kc, cb * 1792:(cb + 1) * 1792], T.w_in,
                   T.w_in.ap()[l, kc * 128:(kc + 1) * 128, cb * 1792:(cb + 1) * 1792], disjoint=True)
    xp = Pool(k, "xt", [128, 4, 1024], F32, 1)
    xnp = Pool(k, "xn", [128, 4, 1024], BF16, 1)
    hTp = Pool(k, "hT", [128, 8, 512], BF16, 1)
    junk = k.sb("junk", [128, 1024], BF16)
    ssp = Pool(k, "ss", [128, 4], F32, 2)
    rsp = Pool(k, "rs", [128, 4], F32, 2)
    pb = Pool(k, "pb", [128, 512], F32, 6, "ps")
    ptr = Pool(k, "ptr", [128, 1024], BF16, 2, "ps")
    qkp = Pool(k, "qk", [128, 10, 64], F32, 2)
    sqp = Pool(k, "sq", [128, 10, 64], F32, 2)
    t1p = Pool(k, "t1", [128, 10, 64], F32, 2)
    s10p = Pool(k, "s10", [128, 10], F32, 2)
    csp = Pool(k, "cs", [128, 2, 64], F32, 2)
    qbp = Pool(k, "qb", [128, 5, 128], BF16, 2)
    qTp = Pool(k, "qTs", [128, 5, 512], BF16, 1)
    vtp = Pool(k, "vt", [128, 4, 256], BF16, 2)
    hip = Pool(k, "hi", [128, 4, 512], BF16, 1)
    sgp = Pool(k, "sg", [128, 4, 512], BF16, 1)
    glp = Pool(k, "gl", [128, 2048], F32, 1)
    fmp = Pool(k, "fm", [128, 512], BF16, 3)
    f32p = Pool(k, "f32", [128, 512], F32, 4)
    sigp = Pool(k, "sig", [128, 512], F32, 2)
    for B in range(NBLK):
        r0 = B * 512
        xt = xp.next()
        k.load("sp", xt, xt[:], xsrc, xsrc_ap[r0:r0 + 512, :].rearrange("(t p) d -> p t d", p=128))
        ss = ssp.next()
        rs = rsp.next()
        xn = xnp.next()
        for t in range(4):
            k.op("act", lambda e: e.activation(out=junk[:], in_=xt[:, t, :], func=AF.Square,
                                               accum_out=ss[:, t:t + 1]), [xt], [junk, ss])
        k.op("act", lambda e: e.activation(out=rs[:], in_=ss[:], func=AF.Sqrt, scale=1.0 / D, bias=EPS),
             [ss], [rs])
        k.op("dve", lambda e: e.reciprocal(out=rs[:], in_=rs[:]), [rs], [rs])
        for t in range(4):
            k.op("act", lambda e: e.activation(out=xn[:, t, :], in_=xt[:, t, :], func=AF.Copy,
                                               scale=rs[:, t:t + 1]), [xt, rs], [xn])
        hT = hTp.next()
        for kc in range(8):
            p = ptr.next()
            for t in range(4):
                tp(k, p, p[:, t * 128:(t + 1) * 128], xn, xn[:, t, kc * 128:(kc + 1) * 128], T.ident_bf)
            k.op("dve", lambda e: e.tensor_scalar(out=hT[:, kc, :], in0=p[:, 0:512],
                                                  scalar1=C.a1[:, kc:kc + 1], scalar2=C.b1[:, kc:kc + 1],
                                                  op0=ALU.mult, op1=ALU.add), [p, C.a1, C.b1], [hT])

        def proj_tm(ps, t, c0, n):
            for kc in range(8):
                mm(k, ps, ps[:, 0:n], hT, hT[:, kc, t * 128:(t + 1) * 128], win, win[:, kc, c0:c0 + n],
                   kc == 0, kc == 7)

        def proj_fm(ps, c0):
            for kc in range(8):
                mm(k, ps, ps[:], win, win[:, kc, c0:c0 + 128], hT, hT[:, kc, :], kc == 0, kc == 7)

        qT = qTp.next()
        vt = vtp.next()
        k.op("pool", lambda e: e.memset(vt[:], 0.0), [], [vt])
        k.op("pool", lambda e: e.memset(vt[:, :, 64:65], 1.0), [], [vt])
        k.op("pool", lambda e: e.memset(vt[:, :, 192:193], 1.0), [], [vt])
        hi = hip.next()
        sg = sgp.next()
        for t in range(4):
            tr0 = r0 + t * 128
            cs = csp.next()
            k.load("sp", cs, cs[:, 0, :], T.cos64, T.cos64.ap()[tr0:tr0 + 128, :])
            k.load("sp", cs, cs[:, 1, :], T.sin64, T.sin64.ap()[tr0:tr0 + 128, :])
            ps1 = pb.next()
            proj_tm(ps1, t, O_Q, 512)
            ps2 = pb.next()
            proj_tm(ps2, t, O_K, 256)
            qk = qkp.next()
            k.op("act", lambda e: e.copy(out=qk[:, 0:8, :], in_=ps1[:].rearrange("p (h d) -> p h d", d=64)),
                 [ps1], [qk])
            k.op("act", lambda e: e.copy(out=qk[:, 8:10, :], in_=ps2[:, 0:128].rearrange("p (h d) -> p h d", d=64)),
                 [ps2], [qk])
            k.op("act", lambda e: e.copy(out=vt[:, t, :].rearrange("p (g d) -> p g d", d=128)[:, :, 0:64],
                                         in_=ps2[:, 128:256].rearrange("p (g d) -> p g d", d=64)), [ps2], [vt])
            sq = sqp.next()
            s10 = s10p.next()
            k.op("dve", lambda e: e.tensor_tensor(out=sq[:], in0=qk[:], in1=qk[:], op=ALU.mult), [qk], [sq])
            k.op("dve", lambda e: e.tensor_reduce(out=s10[:], in_=sq[:], axis=AX.X, op=ALU.add), [sq], [s10])
            k.op("act", lambda e: e.activation(out=s10[:], in_=s10[:], func=AF.Sqrt, scale=1.0 / 64, bias=EPS),
                 [s10], [s10])
            k.op("dve", lambda e: e.reciprocal(out=s10[:], in_=s10[:]), [s10], [s10])
            k.op("dve", lambda e: e.tensor_tensor(out=qk[:], in0=qk[:],
                                                  in1=s10[:].unsqueeze(2).to_broadcast([128, 10, 64]),
                                                  op=ALU.mult), [qk, s10], [qk])
            k.op("pool", lambda e: e.tensor_tensor(out=qk[:], in0=qk[:], in1=C.qkw[:], op=ALU.mult),
                 [qk, C.qkw], [qk])
            t1 = t1p.next()
            k.op("pool", lambda e: e.tensor_tensor(out=t1[:], in0=qk[:],
                                                   in1=cs[:, 0:1, :].to_broadcast([128, 10, 64]), op=ALU.mult),
                 [qk, cs], [t1])
            qv = qk[:].rearrange("p h (a f j) -> p h a f j", a=2, f=2)
            sv = cs[:, 1, :].rearrange("p (a f j) -> p a f j", a=2, f=2)
            sqv = sq[:].rearrange("p h (a f j) -> p h a f j", a=2, f=2)
            for f in range(2):
                k.op("dve", lambda e: e.tensor_tensor(
                    out=sqv[:, :, :, f, :], in0=qv[:, :, :, 1 - f, :],
                    in1=sv[:, :, f, :].unsqueeze(1).to_broadcast([128, 10, 2, 16]), op=ALU.mult),
                    [qk, cs], [sq])
            qb = qbp.next()
            k.op("dve", lambda e: e.tensor_tensor(
                out=qb[:, 0:4, :].rearrange("p j (g d) -> p g j d", g=2),
                in0=t1[:, 0:8, :].rearrange("p (g j) d -> p g j d", g=2),
                in1=sq[:, 0:8, :].rearrange("p (g j) d -> p g j d", g=2), op=ALU.add), [t1, sq], [qb])
            k.op("pool", lambda e: e.tensor_tensor(
                out=qb[:, 4, :].rearrange("p (g d) -> p g d", g=2),
                in0=t1[:, 8:10, :], in1=sq[:, 8:10, :], op=ALU.add), [t1, sq], [qb])
            p = ptr.next()
            for j in range(5):
                tp(k, p, p[:, j * 128:(j + 1) * 128], qb, qb[:, j, :], T.ident_bf)
            k.op("act", lambda e: e.copy(out=qT[:, :, t * 128:(t + 1) * 128],
                                         in_=p[:, 0:640].rearrange("p (j q) -> p j q", q=128)), [p], [qT])
            ps = pb.next()
            proj_tm(ps, t, O_HI, 512)
            k.op("dve", lambda e: e.tensor_copy(out=hi[:, t, :], in_=ps[:]), [ps], [hi])
            ps = pb.next()
            proj_tm(ps, t, O_HG, 512)
            k.op("act", lambda e: e.activation(out=sg[:, t, :], in_=ps[:], func=AF.Silu), [ps], [sg])
            gl = glp.next()
            for c in range(4):
                ps = pb.next()
                proj_tm(ps, t, O_GL + c * 512, 512)
                k.op("act", lambda e: e.activation(out=gl[:, c * 512:(c + 1) * 512], in_=ps[:], func=AF.Sigmoid),
                     [ps], [gl])
            k.store("pool", T.gl_d, T.gl_d.ap()[tr0:tr0 + 128, :], gl, gl[:])
        k.store("pool", T.qT_d, T.qT_d.ap()[:, :, r0:r0 + 512], qT, qT[:, 0:4, :])
        k.store("pool", T.kT_d, T.kT_d.ap()[:, r0:r0 + 512], qT, qT[:, 4, :])
        k.store("pool", T.va_d, T.va_d.ap()[r0:r0 + 512, :].rearrange("(t p) c -> p t c", p=128), vt, vt[:])
        k.store("pool", T.vh_d, T.vh_d.ap()[r0:r0 + 512, :].rearrange("(t p) c -> p t c", p=128), hi, hi[:])
        k.store("pool", T.sg_d, T.sg_d.ap()[r0:r0 + 512, :].rearrange("(t p) c -> p t c", p=128), sg, sg[:])
        for h in range(4):
            ps = pb.next()
            proj_fm(ps, O_HQ + h * 128)
            o = fmp.next()
            k.op("act", lambda e: e.activation(out=o[:], in_=ps[:], func=AF.Silu), [ps], [o])
            k.store("pool", T.hq_d, T.hq_d.ap()[h, :, r0:r0 + 512], o, o[:])
        for dr in range(2):
            for h in range(4):
                ci = dr * 4 + h
                ps = pb.next()
                proj_fm(ps, (O_ZF if dr == 0 else O_ZB) + h * 128)
                sig = sigp.next()
                k.op("act", lambda e: e.activation(out=sig[:], in_=ps[:], func=AF.Sigmoid), [ps], [sig])
                f = f32p.next()
                k.op("dve", lambda e: e.tensor_scalar(out=f[:], in0=sig[:], scalar1=C.oml[:, ci:ci + 1],
                                                      scalar2=C.lb[:, ci:ci + 1], op0=ALU.mult, op1=ALU.add),
                     [sig, C.oml, C.lb], [f])
                k.op("pool", lambda e: e.tensor_scalar(out=f[:], in0=f[:], scalar1=1e-6, scalar2=None,
                                                       op0=ALU.max), [f], [f])
                k.op("act", lambda e: e.activation(out=f[:], in_=f[:], func=AF.Ln), [f], [f])
                k.store("pool", T.g_d, T.g_d.ap()[dr, h, :, r0:r0 + 512], f, f[:])
                kk = f32p.next()
                k.op("dve", lambda e: e.tensor_scalar(out=kk[:], in0=sig[:], scalar1=C.noml[:, ci:ci + 1],
                                                      scalar2=C.oml[:, ci:ci + 1], op0=ALU.mult, op1=ALU.add),
                     [sig, C.noml, C.oml], [kk])
                k.store("pool", T.kk_d, T.kk_d.ap()[dr, h, :, r0:r0 + 512], kk, kk[:])
    k.pop()


def phase_attn(k, T):
    k.push()
    kT = k.sb("kT", [128, S], BF16)
    va = k.sb("va", [128, NTILE, 256], BF16)
    for i in range(4):
        k.load("sp", kT, kT[:, i * 4096:(i + 1) * 4096], T.kT_d, T.kT_d.ap()[:, i * 4096:(i + 1) * 4096],
               disjoint=True)
        k.load("sp", va, va[:, i * 32:(i + 1) * 32, :], T.va_d,
               T.va_d.ap()[i * 4096:(i + 1) * 4096, :].rearrange("(t p) c -> p t c", p=128), disjoint=True)
    qz = [Pool(k, f"qz{g}", [128, 4, 512], BF16, 2) for g in range(2)]
    for g in range(2):
        for t_ in qz[g].t:
            k.op("pool", lambda e: e.memset(t_[:], 0.0), [], [t_])
    psc = Pool(k, "psc", [128, 512], F32, 3, "ps")
    pac = Pool(k, "pac", [128, 512], F32, 4, "ps")
    ppx = k.ps("ppx", [128, 8, 128], BF16)
    ptp = Pool(k, "pT", [128, 512], BF16, 4)
    ohp = Pool(k, "ohi", [65, 512], BF16, 2)
    olp = Pool(k, "olo", [65, 512], BF16, 2)
    otp = Pool(k, "otm", [128, 4, 65], F32, 2)
    rcp = Pool(k, "rc", [128, 4], F32, 2)
    oap = Pool(k, "oa", [128, 4, 512], BF16, 2)
    for B in range(NBLK):
        r0 = B * 512
        qzb = [qz[g].next() for g in range(2)]
        for g in range(2):
            k.load("sp", qzb[g], qzb[g][64 * g:64 * g + 64, :, :], T.qT_d, T.qT_d.ap()[64 * g:64 * g + 64, :, r0:r0 + 512])
        oa = oap.next()
        for g in range(2):
            acc = [pac.next() for _ in range(4)]
            items = [(kc, j) for kc in range(NTILE) for j in range(4)]
            LA = 2
            inflight = {}
            for n in range(len(items) + LA):
                if n < len(items):
                    kc, j = items[n]
                    ps = psc.next()
                    mm(k, ps, ps[:], kT, kT[:, kc * 128:(kc + 1) * 128], qzb[g], qzb[g][:, j, :], True, True)
                    inflight[n] = ps
                m = n - LA
                if m >= 0:
                    kc, j = items[m]
                    ps = inflight.pop(m)
                    pT = ptp.next()
                    k.op("act", lambda e: e.activation(out=pT[:], in_=ps[:], func=AF.Exp, scale=0.125), [ps], [pT])
                    mm(k, acc[j], acc[j][:], va, va[:, kc, g * 128:(g + 1) * 128], pT, pT[:],
                       kc == 0, kc == NTILE - 1)
            for j in range(4):
                h = g * 4 + j
                ohi = ohp.next(); olo = olp.next()
                k.op("act", lambda e: e.copy(out=ohi[:], in_=acc[j][0:65, :]), [acc[j]], [ohi])
                k.op("dve", lambda e: e.tensor_tensor(out=olo[:], in0=acc[j][0:65, :], in1=ohi[:], op=ALU.subtract),
                     [acc[j], ohi], [olo])
                for t in range(4):
                    tp(k, ppx, ppx[:, t, 0:65], ohi, ohi[:, t * 128:(t + 1) * 128], T.ident65)
                    tp(k, ppx, ppx[:, 4 + t, 0:65], olo, olo[:, t * 128:(t + 1) * 128], T.ident65)
                otm = otp.next()
                k.op("act", lambda e: e.copy(out=otm[:], in_=ppx[:, 0:4, 0:65]), [ppx], [otm])
                k.op("dve", lambda e: e.tensor_tensor(out=otm[:], in0=otm[:], in1=ppx[:, 4:8, 0:65], op=ALU.add),
                     [otm, ppx], [otm])
                rc = rcp.next()
                k.op("dve", lambda e: e.reciprocal(out=rc[:], in_=otm[:, :, 64]), [otm], [rc])
                k.op("dve", lambda e: e.tensor_tensor(out=oa[:, :, h * 64:(h + 1) * 64], in0=otm[:, :, 0:64],
                                                      in1=rc[:].unsqueeze(2).to_broadcast([128, 4, 64]),
                                                      op=ALU.mult), [otm, rc], [oa])
        k.store("pool", T.oatt_d, T.oatt_d.ap()[r0:r0 + 512, :].rearrange("(t p) c -> p t c", p=128), oa, oa[:])
    k.pop()


def phase_hgrn(k, T):
    k.push()
    HD = [(h, dr) for dr in range(2) for h in range(4)]
    S32 = {hd: k.sb(f"S32_{hd[0]}{hd[1]}", [128, 128], F32) for hd in HD}
    S16 = {hd: k.sb(f"S16_{hd[0]}{hd[1]}", [128, 128], BF16) for hd in HD}
    for hd in HD:
        k.op("pool", lambda e: e.memset(S32[hd][:], 0.0), [], [S32[hd]])
        k.op("pool", lambda e: e.memset(S16[hd][:], 0.0), [], [S16[hd]])
    N = 8
    hqp = Pool(k, "hq", [128, 128], BF16, 2 * N)
    gp = Pool(k, "g", [128, 128], F32, 2 * N)
    kkp = Pool(k, "kk", [128, 128], F32, 2 * N)
    vp = Pool(k, "v", [128, 128], BF16, 2 * N)
    for i_ in range(2 * N):
        for p_ in (gp, kkp, vp):
            p_.t[i_].dsem = hqp.t[i_].dsem
            p_.t[i_].dcnt = hqp.t[i_].dcnt
    bp = Pool(k, "b", [128, 128], F32, N)
    b2p = Pool(k, "b2", [128, 128], F32, N)
    ebp = Pool(k, "eb", [128, 128], F32, N)
    tmp = Pool(k, "tmp", [128, 128], F32, N)
    qtp = Pool(k, "qt", [128, 128], BF16, N)
    ktp = Pool(k, "kt", [128, 128], BF16, N)
    khtp = Pool(k, "kht", [128, 128], BF16, N)
    scp = Pool(k, "sc", [128, 128], BF16, N)
    osp = Pool(k, "os", [128, 128], F32, N)
    po = [k.ps(f"po{i}", [128, 4, 128], F32) for i in range(2)]
    psb = k.ps("psb", [128, 4, 128], F32)
    ptbs = [k.ps(f"ptb{i}", [128, 2, 4, 128], BF16) for i in range(2)]
    pub = [k.ps(f"pu{i}", [128, 4, 128], F32) for i in range(2)]
    qpp = Pool(k, "qpad", [128, 4, 128], BF16, N)
    khpp = Pool(k, "khpad", [128, 4, 128], BF16, N)
    khtpp = Pool(k, "khtpad", [128, 4, 128], BF16, N)
    t2p = Pool(k, "tmp2", [128, 128], F32, N)
    for step in range(NTILE):
        st = {}
        cx = []
        for i, (h, dr) in enumerate(HD):
            Tt = step if dr == 0 else NTILE - 1 - step
            c0 = Tt * 128
            c = _Dummy()
            c.i, c.h, c.dr, c.Tt = i, h, dr, Tt
            c.le = 31 if dr == 0 else 0
            c.hq = hqp.next(); c.g = gp.next(); c.kk = kkp.next(); c.v = vp.next()
            k.load("sp", c.hq, c.hq[:], T.hq_d, T.hq_d.ap()[h, :, c0:c0 + 128])
            k.load("sp", c.g, c.g[:], T.g_d, T.g_d.ap()[dr, h, :, c0:c0 + 128])
            k.load("sp", c.kk, c.kk[:], T.kk_d, T.kk_d.ap()[dr, h, :, c0:c0 + 128])
            k.load("sp", c.v, c.v[:], T.vh_d, T.vh_d.ap()[c0:c0 + 128, h * 128:(h + 1) * 128])
            cx.append(c)
        for c in cx:
            c.b = bp.next()
            k.op("dve", lambda e: e.tensor_tensor_scan(out=c.b[:], data0=T.rst[:], data1=c.g[:], initial=0.0,
                                                       op0=ALU.mult, op1=ALU.add), [T.rst, c.g], [c.b])
        for c in cx:
            if c.dr == 1:
                b3 = c.b[:].rearrange("p (c i) -> p c i", i=32)
                b2 = b2p.next()
                k.op("pool", lambda e: e.tensor_tensor(out=b2[:].rearrange("p (c i) -> p c i", i=32),
                                                       in0=b3[:, :, 31:32].to_broadcast([128, 4, 32]),
                                                       in1=b3, op=ALU.subtract), [c.b], [b2])
                k.op("pool", lambda e: e.tensor_tensor(out=b2[:], in0=b2[:], in1=c.g[:], op=ALU.add), [b2, c.g], [b2])
                c.b = b2
        for c in cx:
            c.eb = ebp.next(); c.t1 = tmp.next()
            k.op("act", lambda e: e.activation(out=c.eb[:], in_=c.b[:], func=AF.Exp), [c.b], [c.eb])
            k.op("act", lambda e: e.activation(out=c.t1[:], in_=c.b[:], func=AF.Exp, scale=-1.0), [c.b], [c.t1])
        for c in cx:
            b3 = c.b[:].rearrange("p (c i) -> p c i", i=32)
            c.t2 = t2p.next()
            k.op("pool", lambda e: e.tensor_tensor(out=c.t2[:].rearrange("p (c i) -> p c i", i=32),
                                                   in0=b3[:, :, c.le:c.le + 1].to_broadcast([128, 4, 32]),
                                                   in1=b3, op=ALU.subtract), [c.b], [c.t2])
        for c in cx:
            k.op("act", lambda e: e.activation(out=c.t2[:], in_=c.t2[:], func=AF.Exp), [c.t2], [c.t2])
        for c in cx:
            c.qt = qtp.next(); c.kt = ktp.next()
            k.op("dve", lambda e: e.tensor_tensor(out=c.qt[:], in0=c.hq[:], in1=c.eb[:], op=ALU.mult), [c.hq, c.eb], [c.qt])
            k.op("dve", lambda e: e.tensor_tensor(out=c.kt[:], in0=c.kk[:], in1=c.t1[:], op=ALU.mult), [c.kk, c.t1], [c.kt])
        for c in cx:
            c.kht = khtp.next()
            k.op("pool", lambda e: e.tensor_tensor(out=c.kht[:], in0=c.kk[:], in1=c.t2[:], op=ALU.mult), [c.kk, c.t2], [c.kht])
        for c in cx:
            c.khtpad = khtpp.next()
            k.op("pool", lambda e: e.tensor_tensor(out=c.khtpad[:], in0=c.kht[:].unsqueeze(1).to_broadcast([128, 4, 128]),
                                                   in1=T.cmask[:], op=ALU.mult), [c.kht, T.cmask], [c.khtpad])
            c.qpad = qpp.next()
            k.op("dve", lambda e: e.tensor_tensor(out=c.qpad[:], in0=c.qt[:].unsqueeze(1).to_broadcast([128, 4, 128]),
                                                  in1=T.cmask[:], op=ALU.mult), [c.qt, T.cmask], [c.qpad])
        for c in cx:
            i = c.i
            ptb = ptbs[(i // 2) % 2]
            for cc in range(4):
                tp(k, ptb, ptb[:, i % 2, cc, :], c.khtpad, c.khtpad[:, cc, :], T.ident_bf)
            c.kh = khpp.next()
            k.op("act", lambda e: e.copy(out=c.kh[:], in_=ptb[:, i % 2, :, :]), [ptb], [c.kh])
            mm(k, psb, psb[:, i % 4, :], c.kt, c.kt[:], c.qt, c.qt[:], True, True)
            c.sc = scp.next()
            mk = T.mask_f if c.dr == 0 else T.mask_b
            k.op("dve", lambda e: e.tensor_tensor(out=c.sc[:], in0=psb[:, i % 4, :], in1=mk[:], op=ALU.mult),
                 [psb, mk], [c.sc])
        for c in cx:
            i = c.i
            o = po[i // 4]
            k.op("pe", lambda e: e.matmul(o[:, i % 4, :], lhsT=c.sc[:], rhs=c.v[:], start=(i % 4 == 0), stop=False,
                                           skip_group_check=True), [c.sc, c.v], [o])
            st[(c.h, c.dr)] = (c.qpad, c.kh, c.v, c.eb, c.le, c.Tt)
        for ci in range(4):
            for i, (h, dr) in enumerate(HD):
                qt, kh, v, eb, le, Tt = st[(h, dr)]
                c = ci if dr == 0 else 3 - ci
                o = po[i // 4]
                k.op("pe", lambda e: e.matmul(o[:, i % 4, :], lhsT=qt[:, c, :],
                                               rhs=S16[(h, dr)][:], start=False, stop=(ci == 3),
                                               skip_group_check=True), [qt, S16[(h, dr)]], [o])
                pu = pub[i // 4]
                k.op("pe", lambda e: e.matmul(pu[:, i % 4, :], lhsT=kh[:, c, :],
                                               rhs=v[:], start=True, stop=True,
                                               skip_group_check=True), [kh, v], [pu])
                s32 = S32[(h, dr)]
                dcol = eb[:, 32 * c + le:32 * c + le + 1]
                k.op("dve", lambda e: e.scalar_tensor_tensor(out=s32[:], in0=s32[:], scalar=dcol,
                                                             in1=pu[:, i % 4, :], op0=ALU.mult, op1=ALU.add),
                     [s32, eb, pu], [s32])
                k.op("act", lambda e: e.copy(out=S16[(h, dr)][:], in_=s32[:]), [s32], [S16[(h, dr)]])
        for i, (h, dr) in enumerate(HD):
            Tt = st[(h, dr)][5]
            os_ = osp.next()
            k.op("act", lambda e: e.copy(out=os_[:], in_=po[i // 4][:, i % 4, :]), [po[i // 4]], [os_])
            dst = T.of_d if dr == 0 else T.ob_d
            k.store("pool", dst, dst.ap()[Tt * 128:(Tt + 1) * 128, h * 128:(h + 1) * 128], os_, os_[:])
    k.pop()


def phase_merge(k, T, C, l, xsrc, xsrc_ap, acc, acc_ap):
    import os
    CUT = float(os.environ.get('MCUT', '99'))
    k.push()
    wba = k.sb("wba", [128, 4, 1024], BF16)
    wbh = k.sb("wbh", [128, 4, 1024], BF16)
    wout = k.sb("wout", [128, 8, 1024], BF16)
    wr = k.sb("wr", [128, 8, 16], F32)
    stg = Pool(k, "stg", [128, 1024], F32, 2)
    for kc in range(4):
        k.load("pool", wba, wba[:, kc, :], T.w_branch_att, T.w_branch_att.ap()[l, kc * 128:(kc + 1) * 128, :], disjoint=True)
        k.load("pool", wbh, wbh[:, kc, :], T.w_branch_hg, T.w_branch_hg.ap()[l, kc * 128:(kc + 1) * 128, :], disjoint=True)
    for kc in range(8):
        s_ = stg.next()
        k.load("sp", s_, s_[:], T.w_out, T.w_out.ap()[l, kc * 128:(kc + 1) * 128, :])
        k.op("dve", lambda e: e.tensor_tensor(out=wout[:, kc, :], in0=s_[:], in1=C.g1[:], op=ALU.mult),
             [s_, C.g1], [wout])
    k.load("sp", wr, wr[:], T.w_router, T.w_router.ap()[l].rearrange("(kc p) e -> p kc e", p=128))
    wr3 = [k.sb(f"wr3_{i}", [128, 8, 16], BF16) for i in range(3)]
    wrr = k.sb("wrr", [128, 8, 16], F32)
    k.op("act", lambda e: e.copy(out=wr3[0][:], in_=wr[:]), [wr], [wr3[0]])
    k.op("dve", lambda e: e.tensor_tensor(out=wrr[:], in0=wr[:], in1=wr3[0][:], op=ALU.subtract), [wr, wr3[0]], [wrr])
    k.op("act", lambda e: e.copy(out=wr3[1][:], in_=wrr[:]), [wrr], [wr3[1]])
    k.op("dve", lambda e: e.tensor_tensor(out=wrr[:], in0=wrr[:], in1=wr3[1][:], op=ALU.subtract), [wrr, wr3[1]], [wrr])
    k.op("act", lambda e: e.copy(out=wr3[2][:], in_=wrr[:]), [wrr], [wr3[2]])
    oap = Pool(k, "oat", [128, 512], BF16, 2)
    ofp = Pool(k, "of", [128, 4, 128], F32, 2)
    obp = Pool(k, "ob", [128, 4, 128], F32, 2)
    sgp = Pool(k, "sgm", [128, 512], BF16, 2)
    glp = Pool(k, "glm", [128, 2048], F32, 2)
    xp = Pool(k, "xm", [128, 1024], F32, 2)
    sqp = Pool(k, "sqm", [128, 4, 128], F32, 2)
    s4p = Pool(k, "s4", [128, 4], F32, 2)
    ohp = Pool(k, "ohb", [128, 512], BF16, 2)
    lTp = Pool(k, "lT", [128, 8, 128], BF16, 2)
    m1p = Pool(k, "m1", [128, 1024], F32, 2)
    m2p = Pool(k, "m2", [128, 1024], F32, 2)
    mbp = Pool(k, "mb", [128, 1024], BF16, 2)
    mTp = Pool(k, "mT", [128, 8, 128], BF16, 2)
    x1p = Pool(k, "x1", [128, 1024], F32, 2)
    junk = k.sb("junkm", [128, 1024], F32)
    s1p = Pool(k, "s1", [128, 1], F32, 4)
    h2p = Pool(k, "h2", [128, 1024], F32, 2)
    h2bp = Pool(k, "h2b", [128, 8, 128], BF16, 6)
    hbp = [Pool(k, f"hb{i}", [128, 1024], BF16, 2 if i == 0 else 1) for i in range(3)]
    r1p = Pool(k, "r1", [128, 1024], F32, 1)
    lgp = Pool(k, "lg", [128, 16], F32, 2)
    pbk = Pool(k, "pbk", [128, 512], F32, 6, "ps")
    ptb = k.ps("ptbm", [128, 1024], BF16)
    plg = k.ps("plg", [128, 16], F32)
    for Tt in range(int(os.environ.get('MTILES', NTILE))):
        r0 = Tt * 128
        oat = oap.next(); of = ofp.next(); ob = obp.next(); sg = sgp.next(); gl = glp.next(); xm = xp.next()
        k.load("sp", oat, oat[:], T.oatt_d, T.oatt_d.ap()[r0:r0 + 128, :])
        k.load("sp", of, of[:], T.of_d, T.of_d.ap()[r0:r0 + 128, :].rearrange("p (h e) -> p h e", e=128))
        k.load("sp", ob, ob[:], T.ob_d, T.ob_d.ap()[r0:r0 + 128, :].rearrange("p (h e) -> p h e", e=128))
        k.load("sp", sg, sg[:], T.sg_d, T.sg_d.ap()[r0:r0 + 128, :])
        k.load("sp", gl, gl[:], T.gl_d, T.gl_d.ap()[r0:r0 + 128, :])
        k.load("sp", xm, xm[:], xsrc, xsrc_ap[r0:r0 + 128, :])
        k.op("pool", lambda e: e.tensor_tensor(out=of[:], in0=of[:], in1=ob[:], op=ALU.add), [of, ob], [of])
        sq = sqp.next(); s4 = s4p.next()
        k.op("dve", lambda e: e.tensor_tensor(out=sq[:], in0=of[:], in1=of[:], op=ALU.mult), [of], [sq])
        k.op("dve", lambda e: e.tensor_reduce(out=s4[:], in_=sq[:], axis=AX.X, op=ALU.add), [sq], [s4])
        k.op("act", lambda e: e.activation(out=s4[:], in_=s4[:], func=AF.Sqrt, scale=1.0 / 128, bias=EPS), [s4], [s4])
        k.op("dve", lambda e: e.reciprocal(out=s4[:], in_=s4[:]), [s4], [s4])
        k.op("dve", lambda e: e.tensor_tensor(out=of[:], in0=of[:], in1=s4[:].unsqueeze(2).to_broadcast([128, 4, 128]),
                                              op=ALU.mult), [of, s4], [of])
        k.op("pool", lambda e: e.tensor_tensor(out=of[:], in0=of[:], in1=C.hgw[:], op=ALU.mult), [of, C.hgw], [of])
        ohb = ohp.next()
        k.op("dve", lambda e: e.tensor_tensor(out=ohb[:].rearrange("p (h e) -> p h e", e=128), in0=of[:],
                                              in1=sg[:].rearrange("p (h e) -> p h e", e=128), op=ALU.mult),
             [of, sg], [ohb])
        if CUT < 1:
            continue
        for j in range(4):
            tp(k, ptb, ptb[:, j * 128:(j + 1) * 128], oat, oat[:, j * 128:(j + 1) * 128], T.ident_bf)
            tp(k, ptb, ptb[:, (4 + j) * 128:(5 + j) * 128], ohb, ohb[:, j * 128:(j + 1) * 128], T.ident_bf)
        lT = lTp.next()
        k.op("act", lambda e: e.copy(out=lT[:], in_=ptb[:].rearrange("p (j q) -> p j q", q=128)), [ptb], [lT])
        if CUT < 2:
            continue
        m1 = m1p.next(); m2 = m2p.next()
        for hf in range(2):
            pa = pbk.next()
            for kc in range(4):
                mm(k, pa, pa[:], lT, lT[:, kc, :], wba, wba[:, kc, hf * 512:(hf + 1) * 512], kc == 0, kc == 3)
            k.op("dve", lambda e: e.tensor_tensor(out=m1[:, hf * 512:(hf + 1) * 512], in0=pa[:],
                                                  in1=gl[:, hf * 512:(hf + 1) * 512], op=ALU.mult), [pa, gl], [m1])
            ph = pbk.next()
            for kc in range(4):
                mm(k, ph, ph[:], lT, lT[:, 4 + kc, :], wbh, wbh[:, kc, hf * 512:(hf + 1) * 512], kc == 0, kc == 3)
            k.op("dve", lambda e: e.tensor_tensor(out=m2[:, hf * 512:(hf + 1) * 512], in0=ph[:],
                                                  in1=gl[:, 1024 + hf * 512:1024 + (hf + 1) * 512], op=ALU.mult),
                 [ph, gl], [m2])
        mb = mbp.next()
        k.op("pool", lambda e: e.tensor_tensor(out=mb[:], in0=m1[:], in1=m2[:], op=ALU.add), [m1, m2], [mb])
        for j in range(8):
            tp(k, ptb, ptb[:, j * 128:(j + 1) * 128], mb, mb[:, j * 128:(j + 1) * 128], T.ident_bf)
        mT = mTp.next()
        k.op("act", lambda e: e.copy(out=mT[:], in_=ptb[:].rearrange("p (j q) -> p j q", q=128)), [ptb], [mT])
        x1 = x1p.next()
        for hf in range(2):
            po = pbk.next()
            for kc in range(8):
                mm(k, po, po[:], mT, mT[:, kc, :], wout, wout[:, kc, hf * 512:(hf + 1) * 512], kc == 0, kc == 7)
            k.op("dve", lambda e: e.tensor_tensor(out=x1[:, hf * 512:(hf + 1) * 512], in0=po[:],
                                                  in1=xm[:, hf * 512:(hf + 1) * 512], op=ALU.add), [po, xm], [x1])
        if CUT < 3:
            continue
        k.store("pool", T.xs1, T.xs1.ap()[r0:r0 + 128, :], x1, x1[:])
        k.store("pool", acc, acc_ap[r0:r0 + 128, :], x1, x1[:])
        if CUT < 3.1:
            continue
        s1 = s1p.next()
        k.op("act", lambda e: e.activation(out=junk[:], in_=x1[:], func=AF.Square, accum_out=s1[:]), [x1], [junk, s1])
        if CUT < 3.2:
            continue
        k.op("act", lambda e: e.activation(out=s1[:], in_=s1[:], func=AF.Sqrt, scale=1.0 / D, bias=EPS), [s1], [s1])
        k.op("dve", lambda e: e.reciprocal(out=s1[:], in_=s1[:]), [s1], [s1])
        if CUT < 3.3:
            continue
        h2 = h2p.next()
        k.op("act", lambda e: e.activation(out=h2[:], in_=x1[:], func=AF.Copy, scale=s1[:]), [x1, s1], [h2])
        if CUT < 3.4:
            continue
        k.op("dve", lambda e: e.tensor_tensor(out=h2[:], in0=h2[:], in1=C.a2[:], op=ALU.mult), [h2, C.a2], [h2])
        if CUT < 3.5:
            continue
        k.op("dve", lambda e: e.tensor_tensor(out=h2[:], in0=h2[:], in1=C.b2[:], op=ALU.add), [h2, C.b2], [h2])
        if CUT < 4:
            continue
        hb = [hbp[i].next() for i in range(3)]
        r1 = r1p.next()
        k.op("act", lambda e: e.copy(out=hb[0][:], in_=h2[:]), [h2], [hb[0]])
        k.op("dve", lambda e: e.tensor_tensor(out=r1[:], in0=h2[:], in1=hb[0][:], op=ALU.subtract), [h2, hb[0]], [r1])
        k.op("act", lambda e: e.copy(out=hb[1][:], in_=r1[:]), [r1], [hb[1]])
        k.op("dve", lambda e: e.tensor_tensor(out=r1[:], in0=r1[:], in1=hb[1][:], op=ALU.subtract), [r1, hb[1]], [r1])
        k.op("act", lambda e: e.copy(out=hb[2][:], in_=r1[:]), [r1], [hb[2]])
        hT3 = [h2bp.next() for _ in range(3)]
        for i in range(3):
            for j in range(8):
                tp(k, ptb, ptb[:, j * 128:(j + 1) * 128], hb[i], hb[i][:, j * 128:(j + 1) * 128], T.ident_bf)
            k.op("act" if i != 1 else "dve", lambda e: e.tensor_copy(out=hT3[i][:], in_=ptb[:].rearrange("p (j q) -> p j q", q=128))
                 if i == 1 else e.copy(out=hT3[i][:], in_=ptb[:].rearrange("p (j q) -> p j q", q=128)), [ptb], [hT3[i]])
        k.store("pool", T.h2b_d, T.h2b_d.ap()[r0:r0 + 128, :], hb[0], hb[0][:])
        if CUT < 5:
            continue
        terms = [(0, 0), (0, 1), (1, 0), (0, 2), (2, 0), (1, 1)]
        for ti, (a_, b_) in enumerate(terms):
            for kc in range(8):
                mm(k, plg, plg[:], hT3[a_], hT3[a_][:, kc, :], wr3[b_], wr3[b_][:, kc, :],
                   ti == 0 and kc == 0, ti == len(terms) - 1 and kc == 7)
        if CUT < 6:
            continue
        lg = lgp.next()
        mx = s1p.next(); se = s1p.next()
        k.op("dve", lambda e: e.tensor_reduce(out=mx[:], in_=plg[:], axis=AX.X, op=ALU.max), [plg], [mx])
        k.op("dve", lambda e: e.tensor_scalar(out=mx[:], in0=mx[:], scalar1=-1.0, scalar2=None, op0=ALU.mult), [mx], [mx])
        k.op("act", lambda e: e.activation(out=lg[:], in_=plg[:], func=AF.Exp, bias=mx[:], accum_out=se[:]),
             [plg, mx], [lg, se])
        k.op("dve", lambda e: e.reciprocal(out=se[:], in_=se[:]), [se], [se])
        k.op("dve", lambda e: e.tensor_scalar(out=lg[:], in0=lg[:], scalar1=se[:], scalar2=None, op0=ALU.mult),
             [lg, se], [lg])
        k.store("pool", T.aff_d, T.aff_d.ap()[r0:r0 + 128, :], lg, lg[:])
    k.pop()


def phase_route(k, T, idx_all):
    k.push()
    for nm, shp, dt in (("iota", [128, CAP], F32), ("rst16", [128, CAP], F32), ("U_bf", [128, 128], BF16), ("tcol", [128, 2], BF16)):
        t = k.sb(nm, shp, dt)
        src = getattr(T, "c_" + nm)
        k.load("sp", t, t[:], src, src.ap())
        setattr(T, nm, t)
    aff = k.sb("affall", [128, 128, NEXP], F32)
    k.load("sp", aff, aff[:], T.aff_d, T.aff_d.ap().rearrange("(t p) e -> t p e", p=128))
    cmp = k.sb("cmp", [128, 128, NEXP], F32)
    ones = k.sb("ones", [128, 128], F32)
    k.op("pool", lambda e: e.memset(ones[:], 1.0), [], [ones])
    lo = k.sb("lo", [128, NEXP], F32); hi = k.sb("hi", [128, NEXP], F32)
    mid = k.sb("mid", [128, NEXP], F32); cnt = k.sb("cnt", [128, NEXP], F32)
    ge = k.sb("ge", [128, NEXP], F32); d1 = k.sb("d1", [128, NEXP], F32)
    pt = k.ps("ptot", [128, NEXP], F32)
    k.op("dve", lambda e: e.memset(lo[:], 0.0), [], [lo])
    k.op("dve", lambda e: e.memset(hi[:], 1.0), [], [hi])
    for it in range(40):
        k.op("dve", lambda e: e.tensor_tensor(out=mid[:], in0=lo[:], in1=hi[:], op=ALU.add), [lo, hi], [mid])
        k.op("dve", lambda e: e.tensor_scalar(out=mid[:], in0=mid[:], scalar1=0.5, scalar2=None, op0=ALU.mult), [mid], [mid])
        k.op("dve", lambda e: e.tensor_tensor(out=cmp[:], in0=aff[:],
                                              in1=mid[:].unsqueeze(1).to_broadcast([128, 128, NEXP]),
                                              op=ALU.is_gt), [aff, mid], [cmp])
        k.op("dve", lambda e: e.tensor_reduce(out=cnt[:], in_=cmp[:].rearrange("t p e -> t e p"), axis=AX.X,
                                              op=ALU.add), [cmp], [cnt])
        mm(k, pt, pt[:], ones, ones[:], cnt, cnt[:], True, True)
        k.op("dve", lambda e: e.tensor_scalar(out=ge[:], in0=pt[:], scalar1=float(CAP) - 0.5, scalar2=None,
                                              op0=ALU.is_ge), [pt], [ge])
        k.op("dve", lambda e: e.tensor_tensor(out=d1[:], in0=mid[:], in1=lo[:], op=ALU.subtract), [mid, lo], [d1])
        k.op("dve", lambda e: e.tensor_tensor(out=d1[:], in0=d1[:], in1=ge[:], op=ALU.mult), [d1, ge], [d1])
        k.op("dve", lambda e: e.tensor_tensor(out=lo[:], in0=lo[:], in1=d1[:], op=ALU.add), [lo, d1], [lo])
        k.op("dve", lambda e: e.tensor_tensor(out=d1[:], in0=hi[:], in1=mid[:], op=ALU.subtract), [hi, mid], [d1])
        k.op("dve", lambda e: e.tensor_tensor(out=d1[:], in0=d1[:], in1=ge[:], op=ALU.mult), [d1, ge], [d1])
        k.op("dve", lambda e: e.tensor_tensor(out=hi[:], in0=mid[:], in1=d1[:], op=ALU.add), [mid, d1], [hi])
    msk = k.sb("msk", [128, NEXP, 128], F32)
    scn = k.sb("scn", [128, NEXP, 128], F32)
    scb = k.sb("scb", [128, NEXP, 129], BF16)
    k.op("dve", lambda e: e.tensor_tensor(out=msk[:].rearrange("t e p -> t p e"), in0=aff[:],
                                          in1=lo[:].unsqueeze(1).to_broadcast([128, 128, NEXP]), op=ALU.is_gt),
         [aff, lo], [msk])
    k.op("dve", lambda e: e.tensor_tensor_scan(out=scn[:].rearrange("t e p -> t (e p)"),
                                               data0=T.rst16[:], data1=msk[:].rearrange("t e p -> t (e p)"),
                                               initial=0.0, op0=ALU.mult, op1=ALU.add), [T.rst16, msk], [scn])
    k.op("act", lambda e: e.copy(out=scb[:, :, 0:128], in_=scn[:]), [scn], [scb])
    k.op("act", lambda e: e.copy(out=scb[:, :, 128], in_=T.tcol[:, 0:1].to_broadcast([128, NEXP])), [T.tcol], [scb])
    totb = k.sb("totb", [128, NEXP], BF16)
    tot = k.sb("tot", [128, NEXP], F32)
    k.op("dve", lambda e: e.tensor_copy(out=totb[:], in_=scn[:, :, 127]), [scn], [totb])
    k.op("dve", lambda e: e.tensor_copy(out=tot[:], in_=scn[:, :, 127]), [scn], [tot])
    pof = k.ps("pof", [128, NEXP], F32)
    mm(k, pof, pof[:], T.U_bf, T.U_bf[:], totb, totb[:], True, True)
    offs = k.sb("offs", [128, NEXP], F32)
    incl = k.sb("incl", [128, NEXP], F32)
    k.op("dve", lambda e: e.tensor_copy(out=offs[:], in_=pof[:]), [pof], [offs])
    k.op("dve", lambda e: e.tensor_tensor(out=incl[:], in0=offs[:], in1=tot[:], op=ALU.add), [offs, tot], [incl])
    t1p = Pool(k, "rt1", [128, CAP], F32, 2)
    ohp = Pool(k, "oh", [128, CAP], BF16, 2)
    wp = Pool(k, "wrk", [128, CAP], BF16, 2)
    pa = Pool(k, "pa", [128, 129], F32, 2, "ps")
    pr = Pool(k, "pr", [128, 1], F32, 2, "ps")
    rcp = Pool(k, "rcol", [128, 1], F32, 4)
    fnp = Pool(k, "fine", [128, 1], F32, 4)
    jk = k.sb("jk", [128, 128], F32)
    idxf = k.sb("idxf", [128, NEXP, 16], F32)
    for ex in range(NEXP):
        t1 = t1p.next(); oh = ohp.next(); w_ = wp.next(); t2 = t1p.next()
        k.op("dve", lambda e: e.tensor_scalar(out=t1[:], in0=T.iota[:], scalar1=offs[:, ex:ex + 1], scalar2=None,
                                              op0=ALU.is_ge), [T.iota, offs], [t1])
        k.op("dve", lambda e: e.scalar_tensor_tensor(out=oh[:], in0=T.iota[:], scalar=incl[:, ex:ex + 1], in1=t1[:],
                                                     op0=ALU.is_lt, op1=ALU.mult), [T.iota, incl, t1], [oh])
        k.op("dve", lambda e: e.tensor_scalar(out=t2[:], in0=T.iota[:], scalar1=offs[:, ex:ex + 1], scalar2=None,
                                              op0=ALU.subtract), [T.iota, offs], [t2])
        k.op("dve", lambda e: e.tensor_tensor(out=w_[:], in0=t2[:], in1=oh[:], op=ALU.mult), [t2, oh], [w_])
        for kq in range(16):
            pA = pa.next(); pR = pr.next()
            mm(k, pA, pA[:], oh, oh[:, kq * 128:(kq + 1) * 128], scb, scb[:, ex, :], True, True)
            mm(k, pR, pR[:], w_, w_[:, kq * 128:(kq + 1) * 128], T.tcol, T.tcol[:, 1:2], True, True)
            rc = rcp.next(); fn = fnp.next()
            k.op("act", lambda e: e.copy(out=rc[:], in_=pR[:]), [pR], [rc])
            k.op("dve", lambda e: e.tensor_scalar(out=jk[:], in0=pA[:, 0:128], scalar1=rc[:], scalar2=0.0,
                                                  op0=ALU.is_le, op1=ALU.add, accum_out=fn[:]), [pA, rc], [jk, fn])
            k.op("dve", lambda e: e.scalar_tensor_tensor(out=idxf[:, ex, kq:kq + 1], in0=pA[:, 128:129], scalar=128.0,
                                                         in1=fn[:], op0=ALU.mult, op1=ALU.add), [pA, fn], [idxf])
    k.op("dve", lambda e: e.tensor_copy(out=idx_all[:], in_=idxf[:]), [idxf], [idx_all])
    k.pop()


def phase_ffn(k, T, C, l, idx_all, acc, acc_ap):
    k.push()
    wg = k.sb("wg", [128, 8, FF], BF16)
    wu = k.sb("wu", [128, 8, FF], BF16)
    wd = k.sb("wd", [128, 16, 1024], BF16)
    stg = Pool(k, "stgd", [128, 1024], F32, 2)
    xep = Pool(k, "xe", [128, 1024], BF16, 3)
    twp = Pool(k, "tw", [128, NEXP], F32, 8)
    hTp = Pool(k, "xeT", [128, 8, 512], BF16, 2)
    hid = k.sb("hid", [128, 16, 512], BF16)
    sgp = Pool(k, "sgf", [128, 512], F32, 2)
    yop = Pool(k, "yo", [128, 1024], F32, 3)
    pgu = Pool(k, "pgu", [128, 512], F32, 4, "ps")
    pdn = Pool(k, "pdn", [128, 512], F32, 2, "ps")
    ptx = Pool(k, "ptx", [128, 1024], BF16, 2, "ps")
    for ex in range(NEXP):
        for kc in range(8):
            k.load("pool", wg, wg[:, kc, :], T.w_exp_gate, T.w_exp_gate.ap()[l, ex, kc * 128:(kc + 1) * 128, :])
            k.load("pool", wu, wu[:, kc, :], T.w_exp_up, T.w_exp_up.ap()[l, ex, kc * 128:(kc + 1) * 128, :])
        for fc in range(16):
            s_ = stg.next()
            k.load("sp", s_, s_[:], T.w_exp_down, T.w_exp_down.ap()[l, ex, fc * 128:(fc + 1) * 128, :])
            k.op("dve", lambda e: e.tensor_tensor(out=wd[:, fc, :], in0=s_[:], in1=C.g2[:], op=ALU.mult),
                 [s_, C.g2], [wd])
        for B in range(CAP // 512):
            hT = hTp.next()
            tws = []
            for t in range(4):
                kq = B * 4 + t
                xe = xep.next(); tw = twp.next()
                ix = idx_all[:, ex, kq:kq + 1]
                k.dma("pool", lambda e: e.indirect_dma_start(out=xe[:], out_offset=None, in_=T.h2b_d.ap(),
                                                              in_offset=bass.IndirectOffsetOnAxis(ap=ix, axis=0)),
                      xe, reads=[T.h2b_d, idx_all], writes=[xe])
                k.dma("pool", lambda e: e.indirect_dma_start(out=tw[:], out_offset=None, in_=T.aff_d.ap(),
                                                              in_offset=bass.IndirectOffsetOnAxis(ap=ix, axis=0)),
                      tw, reads=[T.aff_d, idx_all], writes=[tw])
                tws.append(tw)
                px = ptx.next()
                for j in range(8):
                    tp(k, px, px[:, j * 128:(j + 1) * 128], xe, xe[:, j * 128:(j + 1) * 128], T.ident_bf)
                k.op("act", lambda e: e.copy(out=hT[:, :, t * 128:(t + 1) * 128],
                                             in_=px[:].rearrange("p (j q) -> p j q", q=128)), [px], [hT])
            for fc in range(16):
                pg = pgu.next()
                for kc in range(8):
                    mm(k, pg, pg[:], wg, wg[:, kc, fc * 128:(fc + 1) * 128], hT, hT[:, kc, :], kc == 0, kc == 7)
                pu = pgu.next()
                for kc in range(8):
                    mm(k, pu, pu[:], wu, wu[:, kc, fc * 128:(fc + 1) * 128], hT, hT[:, kc, :], kc == 0, kc == 7)
                sg = sgp.next()
                k.op("act", lambda e: e.activation(out=sg[:], in_=pg[:], func=AF.Silu), [pg], [sg])
                k.op("dve", lambda e: e.tensor_tensor(out=hid[:, fc, :], in0=sg[:], in1=pu[:], op=ALU.mult),
                     [sg, pu], [hid])
            for t in range(4):
                kq = B * 4 + t
                yo = yop.next()
                for hf in range(2):
                    pd = pdn.next()
                    for fc in range(16):
                        mm(k, pd, pd[:], hid, hid[:, fc, t * 128:(t + 1) * 128], wd, wd[:, fc, hf * 512:(hf + 1) * 512],
                           fc == 0, fc == 15)
                    k.op("act", lambda e: e.activation(out=yo[:, hf * 512:(hf + 1) * 512], in_=pd[:], func=AF.Copy,
                                                       scale=tws[t][:, ex:ex + 1]), [pd, tws[t]], [yo])
                ix = idx_all[:, ex, kq:kq + 1]
                k.dma("pool", lambda e: e.indirect_dma_start(out=acc_ap, out_offset=bass.IndirectOffsetOnAxis(ap=ix, axis=0),
                                                              in_=yo[:], in_offset=None, compute_op=ALU.add),
                      yo, reads=[yo, idx_all], writes=[acc])
    k.pop()


def build(stop_after=99, dbg=(), nlayers=2, skip=()):
    nc = bass.Bass("TRN2", target_bir_lowering=False)
    k = K(nc)
    T = _Dummy()

    def inp(name, shape, dt=F32):
        setattr(T, name, k.dram(name, shape, dt, kind="ExternalInput"))

    inp("x", [1, S, D]); inp("c", [1, D]); inp("ada_w", [2, D, 6 * D]); inp("ada_b", [2, 6 * D])
    inp("norm_mix_w", [2, D]); inp("norm_ffn_w", [2, D]); inp("w_in", [2, D, INW])
    inp("q_norm_w", [2, 64]); inp("k_norm_w", [2, 64]); inp("hg_lower_bounds", [2, 2, 512])
    inp("hg_norm_w", [2, 128]); inp("w_branch_att", [2, 512, D]); inp("w_branch_hg", [2, 512, D])
    inp("w_out", [2, D, D]); inp("w_router", [2, D, NEXP])
    if stop_after >= 7:
        inp("w_exp_gate", [2, NEXP, D, FF]); inp("w_exp_up", [2, NEXP, D, FF]); inp("w_exp_down", [2, NEXP, FF, D])
    inp("cos64", [S, 64]); inp("sin64", [S, 64])
    inp("c_ident_bf", [128, 128], BF16); inp("c_ident_f", [128, 128]); inp("c_mask_f", [128, 128], BF16)
    inp("c_mask_b", [128, 128], BF16); inp("c_rst", [128, 128]); inp("c_cmask", [128, 4, 128], BF16)
    inp("c_iota", [128, CAP]); inp("c_rst16", [128, CAP]); inp("c_U_bf", [128, 128], BF16); inp("c_tcol", [128, 2], BF16)
    T.y = k.dram("y", [S, D], F32, kind="ExternalOutput")

    def scr(name, shape, dt):
        setattr(T, name, k.dram(name, shape, dt, kind="ExternalOutput" if name in dbg else "Internal"))

    scr("modd", [2, 6 * D], F32)
    scr("qT_d", [128, 4, S], BF16); scr("kT_d", [128, S], BF16); scr("va_d", [S, 256], BF16)
    scr("vh_d", [S, 512], BF16); scr("sg_d", [S, 512], BF16); scr("gl_d", [S, 2048], F32)
    scr("hq_d", [4, 128, S], BF16); scr("g_d", [2, 4, 128, S], F32); scr("kk_d", [2, 4, 128, S], F32)
    scr("oatt_d", [S, 512], BF16); scr("of_d", [S, 512], F32); scr("ob_d", [S, 512], F32)
    scr("xs1", [S, D], F32); scr("xs2", [S, D], F32); scr("h2T_d", [128, 8, S], BF16)
    scr("aff_d", [S, NEXP], F32); scr("h2b_d", [S, D], BF16)
    for nm, dt in (("ident_bf", BF16), ("ident_f", F32), ("mask_f", BF16), ("mask_b", BF16), ("rst", F32)):
        t = k.sb(nm, [128, 128], dt)
        src = getattr(T, "c_" + nm)
        k.load("sp", t, t[:], src, src.ap())
        setattr(T, nm, t)
    T.cmask = k.sb("cmask", [128, 4, 128], BF16)
    k.load("sp", T.cmask, T.cmask[:], T.c_cmask, T.c_cmask.ap())
    T.ident65 = k.sb("ident65", [65, 65], BF16)
    k.load("sp", T.ident65, T.ident65[:], T.c_ident_bf, T.c_ident_bf.ap()[0:65, 0:65])
    phase_mod(k, T)
    if stop_after >= 1:
        for l in range(nlayers):
            k.push()
            C = layer_consts(k, T, l)
            idx_all = k.sb("idx_all", [128, NEXP, 16], I32)
            xsrc, xap = (T.x, T.x.ap()[0]) if l == 0 else (T.xs2, T.xs2.ap())
            acc, aap = (T.xs2, T.xs2.ap()) if l == 0 else (T.y, T.y.ap())
            if 2 not in skip:
                phase_proj(k, T, C, l, xsrc, xap)
            if stop_after >= 3 and 3 not in skip:
                phase_attn(k, T)
            if stop_after >= 4 and 4 not in skip:
                phase_hgrn(k, T)
            if stop_after >= 5 and 5 not in skip:
                phase_merge(k, T, C, l, xsrc, xap, acc, aap)
            if stop_after >= 6:
                phase_route(k, T, idx_all)
            if stop_after >= 7:
                phase_ffn(k, T, C, l, idx_all, acc, aap)
            k.pop()
            if stop_after < 7:
                break
    k.barrier()
    k.close()
    return nc


def host_consts():
    bf = ml_dtypes.bfloat16
    t = np.arange(S)
    inv = (10000.0 ** (-np.arange(0, 32, 2, dtype=np.float32) / 32)).astype(np.float32)
    ang_r = ((t // 64).astype(np.float32)[:, None] * inv).astype(np.float32)
    ang_c = ((t % 64).astype(np.float32)[:, None] * inv).astype(np.float32)
    cr, sr, cc, sc = np.cos(ang_r), np.sin(ang_r), np.cos(ang_c), np.sin(ang_c)
    cos64 = np.concatenate([cr, cr, cc, cc], 1).astype(np.float32)
    sin64 = np.concatenate([-sr, sr, -sc, sc], 1).astype(np.float32)
    j = np.arange(128)[:, None]
    i = np.arange(128)[None, :]
    same = (j // 32) == (i // 32)
    return {
        "cos64": cos64, "sin64": sin64,
        "c_ident_bf": np.eye(128, dtype=np.float32).astype(bf), "c_ident_f": np.eye(128, dtype=np.float32),
        "c_mask_f": (same & (j <= i)).astype(np.float32).astype(bf),
        "c_mask_b": (same & (j >= i)).astype(np.float32).astype(bf),
        "c_cmask": np.broadcast_to(((np.arange(128)[None, :] // 32) == np.arange(4)[:, None]).astype(np.float32),
                                   (128, 4, 128)).astype(bf).copy(),
        "c_iota": np.broadcast_to(np.arange(CAP, dtype=np.float32), (128, CAP)).copy(),
        "c_rst16": np.broadcast_to(((np.arange(CAP) % 128) != 0).astype(np.float32), (128, CAP)).copy(),
        "c_U_bf": (np.arange(128)[:, None] < np.arange(128)[None, :]).astype(np.float32).astype(bf),
        "c_tcol": np.stack([np.arange(128, dtype=np.float32), np.ones(128, np.float32)], 1).astype(bf),
        "c_rst": np.broadcast_to(((np.arange(128) % 32) != 0).astype(np.float32), (128, 128)).copy(),
    }


def kernel(**inputs):
    nc = build()
    m = {kk_: np.ascontiguousarray(np.asarray(v, dtype=np.float32)) for kk_, v in inputs.items()}
    m.update(host_consts())
    res = run_bass_kernel_spmd(nc, [m], core_ids=[0])
    return np.asarray(res.results[0]["y"], dtype=np.float32).reshape(1, S, D)
```
